# Optimizing a Trainium2 kernel written in Bass

```python
import math
import jax
import jax.numpy as jnp
from jax import lax
import numpy as np

D_MODEL = 1024
BATCH = 4
SEQ = 8192
DEPTH = 1

GRID_W = 64
CTX_LEN = 256
RG_WIDTH = 1024
RG_HEADS = 16
RG_HEAD_DIM = RG_WIDTH // RG_HEADS
RG_CONV_W = 4
RG_CONV_LEFT = 2
RG_C = 8.0
HY_WIDTH = 1024
HY_CONV_W = 3
HY_CONV_LEFT = 1
HY_SEQ_BANDS = 16
HY_COL_BANDS = 8
HY_EMB_DIM = 1 + 2 * HY_SEQ_BANDS + 1 + 2 * HY_COL_BANDS
HY_FILTER_HIDDEN = 64
HY_DECAY_TARGET = 1e-2
HY_FAST_DECAY = 0.3
HY_SLOW_DECAY = 1.5
N_GROUPS = 4
EXPERTS_PER_GROUP = 8
N_EXPERTS = N_GROUPS * EXPERTS_PER_GROUP
TOP_K = 2
D_EXPERT = 512
MOE_BLOCK = 256
N_MOD = 6
EPS = 1e-6
IN_COLS = 2 * RG_WIDTH + 3 * HY_WIDTH + 2 * D_MODEL
IN_SPLITS = (RG_WIDTH, 2 * RG_WIDTH, 2 * RG_WIDTH + 3 * HY_WIDTH,
             2 * RG_WIDTH + 3 * HY_WIDTH + D_MODEL)

kernel_name = 'hybrid_rglru_hyena_hmoe_dit_block'


def rmsnorm(x, g):
    xf = x.astype(jnp.float32)
    y = xf * lax.rsqrt(jnp.mean(xf * xf, axis=-1, keepdims=True) + EPS)
    return (y * g.astype(jnp.float32)).astype(x.dtype)


def modulate(h, shift, scale):
    return h * (1.0 + scale[..., None, :]) + shift[..., None, :]


def dwconv_centred(x, w, b, left):
    k, L = w.shape[0], x.shape[1]
    xp = jnp.pad(x, ((0, 0), (left, k - 1 - left), (0, 0)))
    y = b + xp[:, 0:L] * w[0]
    for j in range(1, k):
        y = y + xp[:, j:j + L] * w[j]
    return y


def rglru_coeffs(xc, wa, ba, wx, bx, lam):
    B, L, W = xc.shape
    xh = xc.reshape(B, L, RG_HEADS, RG_HEAD_DIM)
    r = jax.nn.sigmoid(jnp.einsum('blhd,hde->blhe', xh, wa).reshape(B, L, W) + ba)
    i = jax.nn.sigmoid(jnp.einsum('blhd,hde->blhe', xh, wx).reshape(B, L, W) + bx)
    log_a = -RG_C * r.astype(jnp.float32) * jax.nn.softplus(-lam.astype(jnp.float32))
    a = jnp.exp(log_a)
    b = jnp.sqrt(-jnp.expm1(2.0 * log_a)) * (i * xc).astype(jnp.float32)
    return a, b


def _scan_op(left, right):
    a_l, b_l = left
    a_r, b_r = right
    return a_r * a_l, a_r * b_l + b_r


def linear_scan(a, b, h0, reverse):
    idx = -1 if reverse else 0
    b = b.at[:, idx].add(a[:, idx] * h0)
    _, h = lax.associative_scan(_scan_op, (a, b), reverse=reverse, axis=1)
    return h


def bidir_rglru(xc, h0_f, h0_b, rg_f, rg_b):
    a_f, b_f = rglru_coeffs(xc, *rg_f)
    a_b, b_b = rglru_coeffs(xc, *rg_b)
    h_f = linear_scan(a_f, b_f, h0_f, False)
    h_b = linear_scan(a_b, b_b, h0_b, True)
    return h_f, h_b


def hyena_filter(L, rows, w1, b1, w2, b2, freq, w3):
    f32 = jnp.float32
    s = jnp.arange(L, dtype=jnp.int32)
    sf = s.astype(f32)
    t_norm = sf / max(L - 1, 1)
    seq_bands = jnp.linspace(1e-4, HY_SEQ_BANDS - 1, HY_SEQ_BANDS, dtype=f32)
    ang = (2.0 * math.pi / L) * sf[:, None] * seq_bands[None, :]
    if rows is None:
        grid = jnp.zeros((L, 1 + 2 * HY_COL_BANDS), f32)
    else:
        row_lag = (s // GRID_W).astype(f32) / rows
        col_bands = jnp.arange(1, HY_COL_BANDS + 1, dtype=f32)
        col_ang = (2.0 * math.pi / GRID_W) * (s % GRID_W).astype(f32)[:, None] * col_bands[None, :]
        grid = jnp.concatenate([row_lag[:, None], jnp.cos(col_ang), jnp.sin(col_ang)], axis=-1)
    feats = jnp.concatenate([t_norm[:, None], jnp.cos(ang), jnp.sin(ang), grid], axis=-1)
    fr = freq.astype(f32)
    z = jnp.sin(fr * (feats @ w1.astype(f32) + b1.astype(f32)))
    z = jnp.sin(fr * (z @ w2.astype(f32) + b2.astype(f32)))
    k = z @ w3.astype(f32)
    max_decay = math.log(HY_DECAY_TARGET) / HY_FAST_DECAY
    min_decay = math.log(HY_DECAY_TARGET) / HY_SLOW_DECAY
    deltas = jnp.abs(jnp.linspace(min_decay, max_decay, HY_WIDTH, dtype=f32))
    decay = jnp.exp(-t_norm[:, None] * deltas[None, :])
    k_fwd = k[:, :HY_WIDTH] * decay
    k_bwd = k[:, HY_WIDTH:] * decay
    return jnp.concatenate([k_fwd, jnp.zeros((1, HY_WIDTH), f32), k_bwd[:0:-1]], axis=0)


def bidir_fftconv(u, k2, skip):
    L = u.shape[1]
    uf = u.astype(jnp.float32)
    U = jnp.fft.rfft(uf, n=2 * L, axis=1)
    K = jnp.fft.rfft(k2, n=2 * L, axis=0)
    y = jnp.fft.irfft(U * K[None], n=2 * L, axis=1)[:, :L]
    return (y + uf * skip.astype(jnp.float32)).astype(u.dtype)


def hyena_mix(p_hy, conv_w, conv_b, k2, skip):
    z = dwconv_centred(p_hy, conv_w, conv_b, HY_CONV_LEFT)
    x0, x1, v = jnp.split(z, 3, axis=-1)
    return bidir_fftconv(v * x1, k2, skip) * x0


def token_mixer(h, rows, h0_f, h0_b, w_in, rg_conv_w, rg_conv_b, rg_f, rg_b, rg_proj,
                hy_conv_w, hy_conv_b, hy_filt, hy_skip, hy_proj, w_out):
    L = h.shape[1]
    p = h @ w_in
    p_rx, p_rg, p_hy, p_ga, p_gb = jnp.split(p, IN_SPLITS, axis=-1)
    xc = dwconv_centred(p_rx, rg_conv_w, rg_conv_b, RG_CONV_LEFT)
    h_f, h_b = bidir_rglru(xc, h0_f, h0_b, rg_f, rg_b)
    y_rg = (h_f + h_b).astype(h.dtype) * jax.nn.gelu(p_rg)
    k2 = hyena_filter(L, rows, *hy_filt)
    y_hy = hyena_mix(p_hy, hy_conv_w, hy_conv_b, k2, hy_skip)
    merged = jax.nn.sigmoid(p_ga) * (y_rg @ rg_proj) + jax.nn.sigmoid(p_gb) * (y_hy @ hy_proj)
    return merged @ w_out, h_f, h_b


def context_scan_states(hc, w_in_rx, rg_conv_w, rg_conv_b, rg_f, rg_b):
    xc = dwconv_centred(hc @ w_in_rx, rg_conv_w, rg_conv_b, RG_CONV_LEFT)
    zero = jnp.zeros((hc.shape[0], RG_WIDTH), jnp.float32)
    h_f, h_b = bidir_rglru(xc, zero, zero, rg_f, rg_b)
    return h_f[:, -1], h_b[:, 0]


def hier_moe(h, wg, bg, we, be, w1, w3, w2):
    B, L, D = h.shape
    N = B * L
    xs = h.reshape(N, D)
    p_group = jax.nn.softmax((xs @ wg + bg).astype(jnp.float32), axis=-1)
    grp = jnp.argmax(p_group, axis=-1).astype(jnp.int32)
    p_g = jnp.max(p_group, axis=-1)
    e_logits = (xs @ we + be).astype(jnp.float32).reshape(N, N_GROUPS, EXPERTS_PER_GROUP)
    e_in = e_logits[jnp.arange(N), grp]
    top_val, top_idx = lax.top_k(e_in, TOP_K)
    p_k = jax.nn.softmax(top_val, axis=-1)
    eid = (grp[:, None] * EXPERTS_PER_GROUP + top_idx).reshape(-1)
    wt = (p_g[:, None] * p_k).reshape(-1)
    tok = jnp.repeat(jnp.arange(N, dtype=jnp.int32), TOP_K)
    A = N * TOP_K
    order = jnp.argsort(eid)
    e_s, t_s, w_s = eid[order], tok[order], wt[order]
    counts = jax.ops.segment_sum(jnp.ones((A,), jnp.int32), eid, num_segments=N_EXPERTS)
    padded = ((counts + MOE_BLOCK - 1) // MOE_BLOCK) * MOE_BLOCK
    start = jnp.cumsum(counts) - counts
    pend = jnp.cumsum(padded)
    pstart = pend - padded
    dest = pstart[e_s] + (jnp.arange(A, dtype=jnp.int32) - start[e_s])
    NB = (A + N_EXPERTS * (MOE_BLOCK - 1)) // MOE_BLOCK
    P = NB * MOE_BLOCK
    row_tok = jnp.full((P,), N, jnp.int32).at[dest].set(t_s)
    row_wt = jnp.zeros((P,), jnp.float32).at[dest].set(w_s)
    blk_exp = jnp.minimum(jnp.searchsorted(pend, jnp.arange(NB, dtype=jnp.int32) * MOE_BLOCK,
                                           side='right'), N_EXPERTS - 1)
    xs_pad = jnp.concatenate([xs, jnp.zeros((1, D), xs.dtype)], axis=0)
    xb = xs_pad[row_tok].reshape(NB, MOE_BLOCK, D)

    def expert_block(args):
        xblk, e = args
        hid = jax.nn.silu(xblk @ w1[e]) * (xblk @ w3[e])
        return hid @ w2[e]

    yb = lax.map(expert_block, (xb, blk_exp)).reshape(P, D)
    out = jnp.zeros((N + 1, D), h.dtype).at[row_tok].add(yb * row_wt[:, None].astype(h.dtype))
    return out[:N].reshape(B, L, D)


def setup_inputs(seed: int = 0) -> dict:
    key = jax.random.key(seed)
    ks = iter(jax.random.split(key, 48))

    def nrm(shape, s):
        return jax.random.normal(next(ks), shape, jnp.float32) * s

    def lam_init():
        u = jax.random.uniform(next(ks), (DEPTH, RG_WIDTH), jnp.float32, minval=0.9, maxval=0.999)
        sg = u ** (1.0 / RG_C)
        return jnp.log(sg) - jnp.log1p(-sg)

    D = D_MODEL
    return {
        'x': nrm((BATCH, SEQ, D), 1.0),
        'c': nrm((BATCH, D), 1.0),
        'ctx': nrm((BATCH, CTX_LEN, D), 1.0),
        'c_ctx': nrm((D,), 1.0),
        'ada_w': nrm((DEPTH, D, N_MOD * D), D ** -0.5),
        'ada_b': nrm((DEPTH, N_MOD * D), 0.02),
        'norm1_g': 1.0 + nrm((DEPTH, D), 0.05),
        'norm2_g': 1.0 + nrm((DEPTH, D), 0.05),
        'final_g': 1.0 + nrm((D,), 0.05),
        'w_in': nrm((DEPTH, D, IN_COLS), D ** -0.5),
        'rg_conv_w': nrm((DEPTH, RG_CONV_W, RG_WIDTH), 0.5),
        'rg_conv_b': nrm((DEPTH, RG_WIDTH), 0.02),
        'rg_wa_f': nrm((DEPTH, RG_HEADS, RG_HEAD_DIM, RG_HEAD_DIM), RG_HEAD_DIM ** -0.5),
        'rg_ba_f': nrm((DEPTH, RG_WIDTH), 0.1),
        'rg_wx_f': nrm((DEPTH, RG_HEADS, RG_HEAD_DIM, RG_HEAD_DIM), RG_HEAD_DIM ** -0.5),
        'rg_bx_f': nrm((DEPTH, RG_WIDTH), 0.1),
        'rg_lam_f': lam_init(),
        'rg_wa_b': nrm((DEPTH, RG_HEADS, RG_HEAD_DIM, RG_HEAD_DIM), RG_HEAD_DIM ** -0.5),
        'rg_ba_b': nrm((DEPTH, RG_WIDTH), 0.1),
        'rg_wx_b': nrm((DEPTH, RG_HEADS, RG_HEAD_DIM, RG_HEAD_DIM), RG_HEAD_DIM ** -0.5),
        'rg_bx_b': nrm((DEPTH, RG_WIDTH), 0.1),
        'rg_lam_b': lam_init(),
        'rg_proj': nrm((DEPTH, RG_WIDTH, D), RG_WIDTH ** -0.5),
        'hy_conv_w': nrm((DEPTH, HY_CONV_W, 3 * HY_WIDTH), HY_CONV_W ** -0.5),
        'hy_conv_b': nrm((DEPTH, 3 * HY_WIDTH), 0.02),
        'hy_pos_w1': nrm((DEPTH, HY_EMB_DIM, HY_FILTER_HIDDEN), HY_EMB_DIM ** -0.5),
        'hy_pos_b1': nrm((DEPTH, HY_FILTER_HIDDEN), 0.1),
        'hy_pos_w2': nrm((DEPTH, HY_FILTER_HIDDEN, HY_FILTER_HIDDEN), HY_FILTER_HIDDEN ** -0.5),
        'hy_pos_b2': nrm((DEPTH, HY_FILTER_HIDDEN), 0.1),
        'hy_freq': 1.0 + nrm((DEPTH, HY_FILTER_HIDDEN), 0.1),
        'hy_pos_w3': nrm((DEPTH, HY_FILTER_HIDDEN, 2 * HY_WIDTH), 0.003),
        'hy_skip': nrm((DEPTH, HY_WIDTH), 1.0),
        'hy_proj': nrm((DEPTH, HY_WIDTH, D), HY_WIDTH ** -0.5),
        'w_out': nrm((DEPTH, D, D), D ** -0.5),
        'moe_wg': nrm((DEPTH, D, N_GROUPS), D ** -0.5),
        'moe_bg': nrm((DEPTH, N_GROUPS), 0.01),
        'moe_we': nrm((DEPTH, D, N_EXPERTS), D ** -0.5),
        'moe_be': nrm((DEPTH, N_EXPERTS), 0.01),
        'moe_w1': nrm((DEPTH, N_EXPERTS, D, D_EXPERT), D ** -0.5),
        'moe_w3': nrm((DEPTH, N_EXPERTS, D, D_EXPERT), D ** -0.5),
        'moe_w2': nrm((DEPTH, N_EXPERTS, D_EXPERT, D), D_EXPERT ** -0.5),
    }


def reference(x, c, ctx, c_ctx, ada_w, ada_b, norm1_g, norm2_g, final_g, w_in,
              rg_conv_w, rg_conv_b, rg_wa_f, rg_ba_f, rg_wx_f, rg_bx_f, rg_lam_f,
              rg_wa_b, rg_ba_b, rg_wx_b, rg_bx_b, rg_lam_b, rg_proj,
              hy_conv_w, hy_conv_b, hy_pos_w1, hy_pos_b1, hy_pos_w2, hy_pos_b2, hy_freq,
              hy_pos_w3, hy_skip, hy_proj, w_out,
              moe_wg, moe_bg, moe_we, moe_be, moe_w1, moe_w3, moe_w2):
    B, L, D = x.shape
    rows = L // GRID_W
    for l in range(DEPTH):
        rg_f = (rg_wa_f[l], rg_ba_f[l], rg_wx_f[l], rg_bx_f[l], rg_lam_f[l])
        rg_b = (rg_wa_b[l], rg_ba_b[l], rg_wx_b[l], rg_bx_b[l], rg_lam_b[l])
        hy_filt = (hy_pos_w1[l], hy_pos_b1[l], hy_pos_w2[l], hy_pos_b2[l], hy_freq[l], hy_pos_w3[l])
        sh1, sc1, g1, sh2, sc2, g2 = jnp.split(jax.nn.silu(c) @ ada_w[l] + ada_b[l], N_MOD, axis=-1)
        if l == DEPTH - 1:
            csh1, csc1 = jnp.split(jax.nn.silu(c_ctx) @ ada_w[l][:, :2 * D] + ada_b[l][:2 * D], 2, axis=-1)
            hc = modulate(rmsnorm(ctx, norm1_g[l]), csh1, csc1)
            hcf, hcb = context_scan_states(hc, w_in[l][:, :RG_WIDTH], rg_conv_w[l], rg_conv_b[l], rg_f, rg_b)
        else:
            csh1, csc1, cg1, csh2, csc2, cg2 = jnp.split(
                jax.nn.silu(c_ctx) @ ada_w[l] + ada_b[l], N_MOD, axis=-1)
            hc = modulate(rmsnorm(ctx, norm1_g[l]), csh1, csc1)
            zero = jnp.zeros((B, RG_WIDTH), jnp.float32)
            out_c, hcf_seq, hcb_seq = token_mixer(
                hc, None, zero, zero, w_in[l], rg_conv_w[l], rg_conv_b[l], rg_f, rg_b, rg_proj[l],
                hy_conv_w[l], hy_conv_b[l], hy_filt, hy_skip[l], hy_proj[l], w_out[l])
            hcf, hcb = hcf_seq[:, -1], hcb_seq[:, 0]
            ctx = ctx + cg1[None, None, :] * out_c
            hc2 = modulate(rmsnorm(ctx, norm2_g[l]), csh2, csc2)
            ctx = ctx + cg2[None, None, :] * hier_moe(hc2, moe_wg[l], moe_bg[l], moe_we[l], moe_be[l],
                                                      moe_w1[l], moe_w3[l], moe_w2[l])
        hx = modulate(rmsnorm(x, norm1_g[l]), sh1, sc1)
        out_x, _, _ = token_mixer(
            hx, rows, hcf, hcb, w_in[l], rg_conv_w[l], rg_conv_b[l], rg_f, rg_b, rg_proj[l],
            hy_conv_w[l], hy_conv_b[l], hy_filt, hy_skip[l], hy_proj[l], w_out[l])
        x = x + g1[:, None, :] * out_x
        hx2 = modulate(rmsnorm(x, norm2_g[l]), sh2, sc2)
        x = x + g2[:, None, :] * hier_moe(hx2, moe_wg[l], moe_bg[l], moe_we[l], moe_be[l],
                                          moe_w1[l], moe_w3[l], moe_w2[l])
    return rmsnorm(x, final_g)
```

```python
import numpy as np
import ml_dtypes
import concourse.bass as bass
import concourse.mybir as mybir
from concourse.bass_utils import run_bass_kernel_spmd

F32 = mybir.dt.float32
BF16 = mybir.dt.bfloat16
I32 = mybir.dt.int32
ALU = mybir.AluOpType
AF = mybir.ActivationFunctionType
AX = mybir.AxisListType

L = 8192
D = 1024
NCH = 8
NTT = L // 128
CTX = 256
NE = 32
DE = 512
NFFT = 16384
EPS = 1e-6
NCORES = 8
MOE_BS = 512
MOE_NB = 2 * 4096 // MOE_BS + 32
TOWN = 4096


class T:
    __slots__ = ("w", "r")

    def __init__(self):
        self.w = {}
        self.r = {}


class Eng:
    def __init__(self, name, key, sem):
        self.name = name
        self.key = key
        self.sem = sem
        self.count = 0
        self.waited = {}
        self.prog = []
        self.dsems = []
        self.dnext = 0


class Planner:
    def __init__(self, nc, ndma=8):
        self.nc = nc
        self.sems = []
        self.totals = []
        self.E = {}
        for name in ("pe", "act", "dve", "pool", "sp"):
            k = self._newsem(name)
            self.E[name] = Eng(name, k, self.sems[k])
        for q in ("sp", "pool", "act"):
            for i in range({"sp": 16, "pool": 24, "act": 2}[q]):
                self.E[q].dsems.append(self._newsem(f"d_{q}{i}"))

    def _newsem(self, name):
        self.sems.append(self.nc.alloc_semaphore(name=name))
        self.totals.append(0)
        return len(self.sems) - 1

    def _need(self, reads, writes):
        need = {}
        for t in reads:
            for k, v in t.w.items():
                if need.get(k, 0) < v:
                    need[k] = v
        for t in writes:
            for k, v in t.w.items():
                if need.get(k, 0) < v:
                    need[k] = v
            for k, v in t.r.items():
                if need.get(k, 0) < v:
                    need[k] = v
        return need

    def _emit_waits(self, E, need, skip_self=False):
        for k, v in need.items():
            if skip_self and k == E.key:
                continue
            if E.waited.get(k, 0) >= v:
                continue
            E.waited[k] = v
            E.prog.append(("w", k, v))

    def op(self, ename, fn, reads=(), writes=()):
        E = self.E[ename]
        need = self._need(reads, writes)
        self._emit_waits(E, need, skip_self=(ename == "pe"))
        E.count += 1
        E.prog.append(("o", fn, E.key))
        for t in reads:
            t.r[E.key] = E.count
        for t in writes:
            t.w[E.key] = E.count

    def dma(self, q, out, in_, reads=(), writes=()):
        E = self.E[q]
        need = self._need(reads, writes)
        k = E.dsems[E.dnext]
        E.dnext = (E.dnext + 1) % len(E.dsems)
        if self.totals[k] > 0:
            need[k] = max(need.get(k, 0), self.totals[k])
        self._emit_waits(E, need)
        self.totals[k] += 16
        E.prog.append(("d", out, in_, k))
        for t in reads:
            t.r[k] = self.totals[k]
        for t in writes:
            t.w[k] = self.totals[k]

    def idma(self, q, out, in_, out_off, in_off, reads=(), writes=(), bound=None):
        E = self.E[q]
        need = self._need(reads, writes)
        k = E.dsems[E.dnext]
        E.dnext = (E.dnext + 1) % len(E.dsems)
        if self.totals[k] > 0:
            need[k] = max(need.get(k, 0), self.totals[k])
        self._emit_waits(E, need)
        self.totals[k] += 16
        E.prog.append(("i", out, in_, out_off, in_off, k, bound))
        for t in reads:
            t.r[k] = self.totals[k]
        for t in writes:
            t.w[k] = self.totals[k]

    def barrier(self):
        need = {}
        for E in self.E.values():
            if E.count:
                need[E.key] = E.count
            for k in E.dsems:
                if self.totals[k]:
                    need[k] = self.totals[k]
        for E in self.E.values():
            n2 = {k: v for k, v in need.items() if k != E.key}
            self._emit_waits(E, n2)

    def emit(self):
        sems = self.sems

        def replay(E, h):
            regs = {}
            for it in E.prog:
                if it[0] == "w":
                    h.wait_ge(sems[it[1]], it[2])
                elif it[0] == "o":
                    it[1](h).then_inc(sems[it[2]], 1)
                elif it[0] == "i":
                    oo = None if it[3] is None else bass.IndirectOffsetOnAxis(ap=it[3], axis=0)
                    io = None if it[4] is None else bass.IndirectOffsetOnAxis(ap=it[4], axis=0)
                    if it[6] is None:
                        h.indirect_dma_start(out=it[1], out_offset=oo, in_=it[2], in_offset=io).then_inc(sems[it[5]], 16)
                    else:
                        if it[6] not in regs:
                            regs[it[6]] = h.to_reg(it[6])
                        h.indirect_dma_start(out=it[1], out_offset=oo, in_=it[2], in_offset=io, bounds_check=regs[it[6]], oob_is_err=False).then_inc(sems[it[5]], 16)
                else:
                    h.dma_start(out=it[1], in_=it[2]).then_inc(sems[it[3]], 16)

        with self.nc.Block() as block:
            @block.tensor
            def _(h):
                replay(self.E["pe"], h)

            @block.scalar
            def _(h):
                replay(self.E["act"], h)

            @block.vector
            def _(h):
                replay(self.E["dve"], h)

            @block.gpsimd
            def _(h):
                replay(self.E["pool"], h)

            @block.sync
            def _(h):
                replay(self.E["sp"], h)


def _bf(a):
    return np.ascontiguousarray(a.astype(np.float32)).astype(ml_dtypes.bfloat16)


_CONST = None


def host_consts():
    global _CONST
    if _CONST is not None:
        return _CONST
    N = NFFT
    c = {}
    c["ident"] = np.eye(128, dtype=np.float32)
    c["identb"] = _bf(np.eye(128))
    c["ones"] = np.ones((128, 128), np.float32)
    n1 = np.arange(128)[:, None].astype(np.float64)
    k1 = np.arange(128)[None, :].astype(np.float64)
    th = -2 * np.pi * n1 * (2 * k1 + 1) / 256.0
    c["F1"] = _bf(np.concatenate([np.cos(th), np.sin(th)], axis=1))
    n2 = np.arange(128)[:, None, None].astype(np.float64)
    kk1 = np.arange(128)[None, :, None].astype(np.float64)
    kk2 = np.arange(64)[None, None, :].astype(np.float64)
    ph = -2 * np.pi * n2 * (kk1 + 0.5 + 128 * kk2) / N
    Gre, Gim = np.cos(ph), np.sin(ph)
    c["L1"] = _bf(np.concatenate([Gre, Gim], axis=2).reshape(128, 128 * 128))
    c["L2"] = _bf(np.concatenate([-Gim, Gre], axis=2).reshape(128, 128 * 128))
    k2 = np.arange(64)[:, None].astype(np.float64)
    nn2 = np.arange(128)[None, :].astype(np.float64)
    cp = 2 * np.pi * k2 * nn2 / 128.0
    Cre, Cim = np.cos(cp), np.sin(cp)
    c["R1"] = _bf(np.concatenate([np.concatenate([Cre, Cim], 1), np.concatenate([-Cim, Cre], 1)], 0))
    c["R2"] = _bf(np.concatenate([np.concatenate([-Cim, Cre], 1), np.concatenate([Cre, Cim], 1)], 0))
    hk1 = np.arange(128)[:, None, None].astype(np.float64)
    hn2 = np.arange(128)[None, :, None].astype(np.float64)
    hn1 = np.arange(TOWN // 128)[None, None, :].astype(np.float64)
    hp = 2 * np.pi * (hk1 + 0.5) * (128 * hn1 + hn2) / N
    c["HR"] = _bf((2.0 / N) * np.cos(hp).reshape(128, 128 * (TOWN // 128)))
    c["HI"] = _bf(-(2.0 / N) * np.sin(hp).reshape(128, 128 * (TOWN // 128)))
    PA = np.zeros((128, 128)); PC = np.zeros((128, 128))
    for m in range(64):
        PA[m, m] = 1; PA[m, m + 64] = 1
        PC[64 + m, m] = 1; PC[64 + m, 64 + m] = -1
    c["PA"] = _bf(PA); c["PC"] = _bf(PC)
    n = np.arange(N)
    s = np.where(n <= 8192, n, N - n).astype(np.int64)
    s = np.where(n == 8192, 0, s)
    sf = s.astype(np.float32)
    t_norm = (sf / np.float32(L - 1)).astype(np.float32)
    bands = np.linspace(1e-4, 15, 16, dtype=np.float32)
    ang = (np.float32(2.0 * np.pi / L) * sf[:, None] * bands[None, :]).astype(np.float32)
    rows = L // 64
    row_lag = (s // 64).astype(np.float32) / np.float32(rows)
    colb = np.arange(1, 9, dtype=np.float32)
    cang = (np.float32(2.0 * np.pi / 64) * (s % 64).astype(np.float32)[:, None] * colb[None, :]).astype(np.float32)
    feats = np.concatenate([t_norm[:, None], np.cos(ang), np.sin(ang), row_lag[:, None],
                            np.cos(cang), np.sin(cang)], axis=-1).astype(np.float32)
    c["featsT"] = np.ascontiguousarray(feats.T)
    tn = t_norm.copy()
    tn[8192] = 1e4
    c["ntn2"] = np.ascontiguousarray((-tn).reshape(128, 128))
    mxd = np.log(1e-2) / 0.3
    mnd = np.log(1e-2) / 1.5
    deltas = np.abs(np.linspace(mnd, mxd, 1024, dtype=np.float32))
    c["DLT"] = np.ascontiguousarray(np.broadcast_to(deltas[None, :], (128, 1024))).astype(np.float32)
    dec = np.exp(-tn.astype(np.float32)[:, None] * deltas[None, :]).astype(np.float32)
    dec = dec.reshape(128, 128, 32, 32).transpose(2, 0, 1, 3).reshape(32, 128, 4096)
    c["DEC"] = _bf(dec)
    c["tri"] = np.triu(np.ones((128, 128), np.float32), k=1)
    pp = np.arange(128, dtype=np.float32)[:, None]
    c["pkw"] = np.ascontiguousarray(pp + 128.0 * np.arange(8, dtype=np.float32)[None, :])
    c["pkw2"] = np.ascontiguousarray(pp + 128.0 * np.arange(4, dtype=np.float32)[None, :])
    c["jbs"] = np.ascontiguousarray(np.broadcast_to((MOE_BS * np.arange(MOE_NB, dtype=np.float32))[None, :], (128, MOE_NB)))
    _CONST = c
    return c


def fm(v, nch=None):
    v = np.asarray(v, np.float32)
    return np.ascontiguousarray(v.reshape(-1, 128).T)


def build(debug=(), stop_after=None):
    nc = bass.Bass("TRN2", target_bir_lowering=False)
    P = Planner(nc)
    dbg = set(debug)

    def din(name, shape, dt=F32):
        return nc.dram_tensor(name, list(shape), dt, kind="ExternalInput").ap()

    def dscr(name, shape, dt):
        kind = "ExternalOutput" if name in dbg else "Internal"
        return nc.dram_tensor(name, list(shape), dt, kind=kind).ap()

    x_d = din("x", [L, D])
    ctx_d = din("ctx", [CTX, D])
    cT_d = din("cT", [128, NCH, 2])
    adaw_d = din("ada_w", [D, 6 * D])
    adab_d = din("ada_bT", [128, 48])
    g1n_d = din("g1nT", [128, NCH])
    g2n_d = din("g2nT", [128, NCH])
    fg_d = din("final_g", [1, D])
    win_d = din("w_in", [D, 7 * D])
    rgcw_d = din("rg_cwT", [128, NCH, 5])
    rgcb_d = din("rg_cbT", [128, NCH])
    rgbd_d = din("rg_bd", [128, 4, NCH, 128])
    rgbias_d = din("rg_biasT", [128, 4, NCH])
    rglam_d = din("rg_lamT", [128, 2, NCH])
    rgp_d = din("rg_proj", [D, D])
    hycw_d = din("hy_cw96", [96, 11, 3, 3])
    hycb_d = din("hy_cb96", [96, 11, 3])
    hyw1_d = din("hy_w1", [50, 64])
    hyb1_d = din("hy_b1T", [64, 1])
    hyw2_d = din("hy_w2", [64, 64])
    hyb2_d = din("hy_b2T", [64, 1])
    hyfr_d = din("hy_frT", [64, 1])
    hyw3_d = din("hy_w3", [64, 2048])
    hyw3z_d = din("hy_w3z", [64, 1024])
    hysk_d = din("hy_sk96", [96, 11])
    hyp_d = din("hy_proj", [D, D])
    wout_d = din("w_out", [D, D])
    wge_d = din("moe_wge", [D, 36])
    bge_d = din("moe_bge", [1, 36])
    mw1_d = din("moe_w1", [NE, D, DE])
    mw3_d = din("moe_w3", [NE, D, DE])
    mw2_d = din("moe_w2", [NE, DE, D])
    C = {}
    hc = host_consts()
    for nm, arr in hc.items():
        C[nm] = din("k_" + nm, arr.shape, BF16 if arr.dtype == ml_dtypes.bfloat16 else F32)
    out_d = nc.dram_tensor("out", [TOWN, D], F32, kind="ExternalOutput").ap()

    PT_d = dscr("PT", [7 * D, L], BF16)
    YRG_d = dscr("YRG", [D, TOWN], BF16)
    YHY_d = dscr("YHY", [D, TOWN], BF16)
    X1_d = dscr("X1", [TOWN, D], F32)
    KAC_d = dscr("KAC", [32, 128, 2, 4096], BF16)
    tPT, tYRG, tYHY, tX1, tKAC = T(), T(), T(), T(), T()

    banks = []
    for i in range(8):
        banks.append((nc.alloc_psum_tensor(f"ps{i}", [128, 512], F32)[:, :], T()))
    bstate = [0]

    def bank():
        b = banks[bstate[0] % 8]
        bstate[0] += 1
        return b

    ARENA = 196608 - 2048
    arena = nc.alloc_sbuf_tensor("arena", [128, ARENA // 2], BF16)
    apos = [0]

    def alloc(shape, dt):
        esz = 4 if dt in (F32, I32) else 2
        n = int(np.prod(shape[1:]))
        nbytes = (n * esz + 63) // 64 * 64
        off = apos[0]
        apos[0] += nbytes
        assert apos[0] <= ARENA, f"SBUF overflow {apos[0]}"
        ap = arena[0:shape[0], off // 2: off // 2 + n * esz // 2]
        if esz == 4:
            ap = ap.bitcast(dt)
        if len(shape) > 2:
            names = " ".join(f"a{i}" for i in range(len(shape) - 1))
            kw = {f"a{i}": int(shape[i + 1]) for i in range(len(shape) - 1)}
            ap = ap.rearrange(f"p ({names}) -> p {names}", **kw)
        return ap

    def reset_arena(to=0):
        P.barrier()
        apos[0] = to

    def act(out, in_, func, r, w, bias=None, scale=None, accum=None, eng="act"):
        kw = {}
        if bias is not None:
            kw["bias"] = bias
        if scale is not None:
            kw["scale"] = scale
        if accum is not None:
            kw["accum_out"] = accum
        P.op("act", lambda h: h.activation(out=out, in_=in_, func=func, **kw), r, w)

    def ts(eng, out, in0, s1, s2, op0, op1, r, w):
        if op1 is None:
            P.op(eng, lambda h: h.tensor_scalar(out=out, in0=in0, scalar1=s1, scalar2=None, op0=op0), r, w)
        else:
            P.op(eng, lambda h: h.tensor_scalar(out=out, in0=in0, scalar1=s1, scalar2=s2, op0=op0, op1=op1), r, w)

    def stt(out, in0, sc, in1, op0, op1, r, w):
        P.op("dve", lambda h: h.scalar_tensor_tensor(out=out, in0=in0, scalar=sc, in1=in1, op0=op0, op1=op1), r, w)

    def tt(eng, out, in0, in1, op, r, w):
        P.op(eng, lambda h: h.tensor_tensor(out=out, in0=in0, in1=in1, op=op), r, w)

    def cp(eng, out, in_, r, w):
        if eng == "act":
            P.op("act", lambda h: h.activation(out=out, in_=in_, func=AF.Copy), r, w)
        else:
            P.op(eng, lambda h: h.tensor_copy(out=out, in_=in_), r, w)

    def mm(out, lhsT, rhs, start, stop, r, w):
        P.op("pe", lambda h: h.matmul(out, lhsT, rhs, start=start, stop=stop), r, w)

    def tr(out, in_, ident, r, w):
        P.op("pe", lambda h: h.transpose(out, in_, ident), r, w)

    def memset(eng, ap, val, w):
        P.op(eng, lambda h: h.memset(ap, val), (), w)

    tC = T()
    ident = alloc([128, 128], F32)
    identb = alloc([128, 128], BF16)
    ones = alloc([128, 128], F32)
    modT = alloc([128, 48, 2], F32)
    A1 = alloc([128, NCH], F32); B1 = alloc([128, NCH], F32)
    A1c = alloc([128, NCH], F32); B1c = alloc([128, NCH], F32)
    A2 = alloc([128, NCH], F32); B2 = alloc([128, NCH], F32)
    pass
    rgcw = alloc([128, NCH, 5], F32); rgcb = alloc([128, NCH], F32)
    rgbias = alloc([128, 4, NCH], F32); nrgbias = alloc([128, 4, NCH], F32)
    rglam = alloc([128, 2, NCH], F32)
    sa1 = alloc([128, 2, NCH], F32); sa2 = alloc([128, 2, NCH], F32)
    hycw = alloc([96, 11, 3, 3], F32); hycb = alloc([96, 11, 3], F32); hysk = alloc([96, 11], F32)
    H0 = alloc([128, 2, NCH], F32)
    bgeB = alloc([128, 36], F32)
    PERSIST = apos[0]

    P.dma("sp", ident, C["ident"], (), (tC,))
    P.dma("sp", identb, C["identb"], (), (tC,))
    P.dma("sp", ones, C["ones"], (), (tC,))
    for dst, src in ((rgcw, rgcw_d), (rgcb, rgcb_d), (rgbias, rgbias_d), (rglam, rglam_d),
                     (hycw, hycw_d), (hycb, hycb_d), (hysk, hysk_d)):
        P.dma("sp", dst, src, (), (tC,))
    P.dma("sp", bgeB, bge_d.partition_broadcast(128), (), (tC,))

    def phaseA():
        cT = alloc([128, NCH, 2], F32)
        scT = alloc([128, NCH, 2], F32)
        adab = alloc([128, 48], F32)
        g1n = alloc([128, NCH], F32); g2n = alloc([128, NCH], F32)
        tmp = alloc([128, NCH], F32)
        dg = alloc([128, 128], F32)
        wbuf = [alloc([128, 1536], F32) for _ in range(3)]
        tw = [T() for _ in range(3)]
        tS = T()
        P.dma("sp", cT, cT_d, (), (tS,))
        P.dma("sp", adab, adab_d, (), (tS,))
        P.dma("sp", g1n, g1n_d, (), (tS,))
        P.dma("sp", g2n, g2n_d, (), (tS,))
        act(scT, cT, AF.Silu, (tS,), (tS,))
        pb, tb = bank()
        i = 0
        for k in range(NCH):
            for q in range(4):
                wb, twb = wbuf[i % 3], tw[i % 3]
                i += 1
                P.dma("sp", wb, adaw_d[k * 128:(k + 1) * 128, q * 1536:(q + 1) * 1536], (), (twb,))
                for jj in range(12):
                    j = q * 12 + jj
                    mm(pb[:, 2 * j:2 * j + 2], wb[:, jj * 128:(jj + 1) * 128], scT[:, k, :],
                       (k == 0 and j == 0), (k == NCH - 1 and j == 47), (twb, tS), (tb,))
        for col in range(2):
            tt("dve", modT[:, :, col], pb[:, col:96:2], adab, ALU.add, (tb, tS), (tC,))
        for (Ad, Bd, gn, sci, shi, col) in ((A1, B1, g1n, 1, 0, 0), (A1c, B1c, g1n, 1, 0, 1), (A2, B2, g2n, 4, 3, 0)):
            ts("dve", tmp, modT[:, sci * 8:(sci + 1) * 8, col], 1.0, None, ALU.add, None, (tC,), (tS,))
            tt("dve", Ad, tmp, gn, ALU.mult, (tS,), (tC,))
            cp("dve", Bd, modT[:, shi * 8:(shi + 1) * 8, col], (tC,), (tC,))
        act(sa1, rglam, AF.Exp, (tC,), (tC,), scale=-1.0)
        act(sa1, sa1, AF.Ln, (tC,), (tC,), bias=1.0)
        ts("dve", sa2, sa1, -16.0, None, ALU.mult, None, (tC,), (tC,))
        ts("dve", sa1, sa1, -8.0, None, ALU.mult, None, (tC,), (tC,))
        ts("dve", nrgbias, rgbias, -1.0, None, ALU.mult, None, (tC,), (tC,))

    phaseA()
    reset_arena(PERSIST)

    def phaseB(src_d, ntok, W, ccs_all, ccs_fn, Asc, Bsc, dst_d, tdst, sig_from=40):
        nsub = W // 128
        ncol = len(ccs_all)
        pos = {cc: i for i, cc in enumerate(ccs_all)}
        winb = alloc([128, NCH, ncol * 128], BF16)
        tWs = [T() for _ in range((ncol + 7) // 8)]
        c0 = ccs_all[0] * 128
        for blk in range(0, ncol * 128, 1024):
            wd = min(1024, ncol * 128 - blk)
            for k in range(NCH):
                P.dma("pool", winb[:, k, blk:blk + wd], win_d[k * 128:(k + 1) * 128, c0 + blk:c0 + blk + wd], (), (tWs[blk // 1024],))
        xts = [alloc([128, nsub, D], F32) for _ in range(2)]
        txs = [T(), T()]
        junk = alloc([128, D], BF16); tj = T()
        ss = [alloc([128, nsub], F32) for _ in range(2)]
        hxs = [alloc([128, NCH, W], BF16) for _ in range(2)]
        ths = [T(), T()]
        stg = [alloc([128, 4, W], BF16) for _ in range(3)]
        tst = [T() for _ in range(3)]
        si = 0
        for ti in range(ntok // W):
            ccs = ccs_fn(ti)
            xt, tx, s_, hx, th = xts[ti % 2], txs[ti % 2], ss[ti % 2], hxs[ti % 2], ths[ti % 2]
            P.dma("sp", xt, src_d[ti * W:(ti + 1) * W, :].rearrange("(a p) d -> p a d", p=128), (), (tx,))
            for a in range(nsub):
                act(junk, xt[:, a, :], AF.Square, (tx,), (tj, tx), accum=s_[:, a:a + 1])
            act(s_, s_, AF.Sqrt, (tx,), (tx,), scale=1.0 / D, bias=EPS)
            P.op("dve", lambda h, s_=s_: h.reciprocal(out=s_, in_=s_), (tx,), (tx,))
            for a in range(nsub):
                ts("dve" if a % 2 == 0 else "pool", xt[:, a, :], xt[:, a, :], s_[:, a:a + 1], None, ALU.mult, None, (tx,), (tx,))
            for dc in range(NCH):
                pb, tb = bank()
                for a in range(nsub):
                    tr(pb[:, a * 128:(a + 1) * 128], xt[:, a, dc * 128:(dc + 1) * 128], ident, (tx, tC), (tb,))
                act(hx[:, dc, :], pb[:, 0:W], AF.Identity, (tb, tC), (th,), bias=Bsc[:, dc:dc + 1], scale=Asc[:, dc:dc + 1])
            for ci, cc in enumerate(ccs):
                pb, tb = bank()
                wi = pos[cc]
                for k in range(NCH):
                    mm(pb[:, 0:W], winb[:, k, wi * 128:(wi + 1) * 128], hx[:, k, :], k == 0, k == NCH - 1, (tWs[wi // 8], th), (tb,))
                sg, tsg = stg[si % 3], tst[si % 3]
                if cc >= sig_from:
                    act(sg[:, ci % 4, :], pb[:, 0:W], AF.Sigmoid, (tb,), (tsg,))
                elif ci % 2 == 0:
                    cp("act", sg[:, ci % 4, :], pb[:, 0:W], (tb,), (tsg,))
                else:
                    cp("dve", sg[:, ci % 4, :], pb[:, 0:W], (tb,), (tsg,))
                if ci % 4 == 3:
                    r0 = ccs[ci - 3] * 128
                    P.dma("sp", dst_d[r0:r0 + 512, ti * W:(ti + 1) * W].rearrange("(a p) t -> p a t", p=128), sg, (tsg,), (tdst,))
                    si += 1

    NOWN5 = TOWN // 512
    CC_ALL = list(range(56))
    CC_REST = list(range(0, 8)) + list(range(24, 40))
    CC_HALO = CC_REST + list(range(16, 24))

    def ccs_main(ti):
        if ti < NOWN5:
            return CC_ALL
        if ti == NOWN5:
            return CC_HALO
        return CC_REST

    phaseB(x_d, L, 512, CC_ALL, ccs_main, A1, B1, PT_d, tPT)
    reset_arena(PERSIST)
    if stop_after == "B":
        return finish(nc, P, out_d, dbg)

    PTC_d = dscr("PTC", [D, CTX], BF16)
    tPTC = T()
    phaseB(ctx_d, CTX, 256, list(range(8)), lambda ti: list(range(8)), A1c, B1c, PTC_d, tPTC)
    reset_arena(PERSIST)

    def phaseC(src_d, tsrc, Lt, Lown, TW, is_ctx):
        ntile = Lt // TW
        nown = Lown // TW
        nq = TW // 512 if TW >= 512 else 1
        QW = min(512, TW)
        wbd = alloc([128, 4, NCH, 128], BF16); tWb = T()
        P.dma("pool", wbd.rearrange("p a b c -> p (a b c)"), rgbd_d.rearrange("p a b c -> p (a b c)"), (), (tWb,))
        PX = alloc([128, Lt + 4], BF16); tPX = T()
        xc = alloc([128, Lt], BF16); txc = T()
        if not is_ctx:
            PRG = alloc([128, Lown], BF16); tPRG = T()
            HB = alloc([128, Lown], BF16); tHB = T()
            g1 = alloc([128, TW], F32); g2 = alloc([128, TW], F32); tg = T()
            yo = [alloc([128, TW], BF16) for _ in range(2)]; tyo = [T(), T()]
        NG = 2
        rts = [alloc([128, TW], F32) for _ in range(NG)]
        ats = [alloc([128, TW], F32) for _ in range(NG)]
        its = [alloc([128, TW], F32) for _ in range(NG)]
        tgts = [T() for _ in range(NG)]
        hts = [alloc([128, TW], F32) for _ in range(2)]; tht = [T(), T()]
        memset("dve", PX[:, 0:2], 0.0, (tPX,))
        memset("dve", PX[:, Lt + 2:Lt + 4], 0.0, (tPX,))
        hcount = 0
        yi = 0
        gi_ = 0
        for cc in range(NCH):
            P.dma("sp", PX[:, 2:Lt + 2], src_d[cc * 128:(cc + 1) * 128, 0:Lt], (tsrc,), (tPX,))
            if not is_ctx:
                P.dma("sp", PRG, src_d[D + cc * 128:D + (cc + 1) * 128, 0:Lown], (tsrc,), (tPRG,))
            act(xc, PX[:, 0:Lt], AF.Identity, (tPX, tC), (txc,), bias=rgcb[:, cc:cc + 1], scale=rgcw[:, cc, 0:1])
            for j in range(1, 5):
                stt(xc, PX[:, j:j + Lt], rgcw[:, cc, j:j + 1], xc, ALU.mult, ALU.add, (tPX, tC), (txc,))
            for d in (1, 0):
                order = range(ntile - 1, -1, -1) if d == 1 else range(nown)
                first = True
                for ti in order:
                    sl = slice(ti * TW, (ti + 1) * TW)
                    rt, at, it, tgt = rts[gi_ % NG], ats[gi_ % NG], its[gi_ % NG], tgts[gi_ % NG]
                    gi_ += 1
                    prs, pis = [], []
                    for q in range(nq):
                        pr, tr_ = bank(); pi, ti_ = bank()
                        mm(pr[:, 0:QW], wbd[:, 2 * d, cc, :], xc[:, ti * TW + q * QW: ti * TW + (q + 1) * QW], True, True, (tWb, txc), (tr_,))
                        mm(pi[:, 0:QW], wbd[:, 2 * d + 1, cc, :], xc[:, ti * TW + q * QW: ti * TW + (q + 1) * QW], True, True, (tWb, txc), (ti_,))
                        prs.append((pr, tr_)); pis.append((pi, ti_))
                    for q in range(nq):
                        act(rt[:, q * QW:(q + 1) * QW], prs[q][0][:, 0:QW], AF.Sigmoid, (prs[q][1], tC), (tgt,), bias=rgbias[:, 2 * d, cc:cc + 1])
                    for q in range(nq):
                        act(it[:, q * QW:(q + 1) * QW], pis[q][0][:, 0:QW], AF.Sigmoid, (pis[q][1], tC), (tgt,), bias=rgbias[:, 2 * d + 1, cc:cc + 1])
                    act(at, rt, AF.Exp, (tgt, tC), (tgt,), scale=sa1[:, d, cc:cc + 1])
                    act(rt, rt, AF.Exp, (tgt, tC), (tgt,), scale=sa2[:, d, cc:cc + 1])
                    act(rt, rt, AF.Sqrt, (tgt,), (tgt,), scale=-1.0, bias=1.0)
                    tt("pool", it, it, xc[:, sl], ALU.mult, (tgt, txc), (tgt,))
                    tt("dve", rt, rt, it, ALU.mult, (tgt,), (tgt,))
                    ht, th_ = hts[hcount % 2], tht[hcount % 2]
                    hp, thp = hts[(hcount + 1) % 2], tht[(hcount + 1) % 2]
                    hcount += 1
                    if first:
                        init = 0.0 if is_ctx else H0[:, d, cc:cc + 1]
                        rd = (tgt,) if is_ctx else (tgt, tC)
                    else:
                        init = hp[:, 0:1] if d == 1 else hp[:, TW - 1:TW]
                        rd = (tgt, thp)
                    first = False
                    if d == 1:
                        P.op("dve", lambda h, ht=ht, init=init, at=at, rt=rt: h.tensor_tensor_scan(out=ht[:, ::-1], data0=at[:, ::-1], data1=rt[:, ::-1], initial=init, op0=ALU.mult, op1=ALU.add), rd, (th_,))
                    else:
                        P.op("dve", lambda h, ht=ht, init=init, at=at, rt=rt: h.tensor_tensor_scan(out=ht, data0=at, data1=rt, initial=init, op0=ALU.mult, op1=ALU.add), rd, (th_,))
                    if is_ctx:
                        last = (ti == 0) if d == 1 else (ti == ntile - 1)
                        if last:
                            col = ht[:, 0:1] if d == 1 else ht[:, TW - 1:TW]
                            cp("dve", H0[:, d, cc:cc + 1], col, (th_,), (tC,))
                        continue
                    if d == 1:
                        if ti < nown:
                            cp("act", HB[:, sl], ht, (th_,), (tHB,))
                    else:
                        xg = PRG[:, sl]
                        tt("pool", g1, xg, xg, ALU.mult, (tPRG,), (tg,))
                        ts("pool", g1, g1, 0.044715, 1.0, ALU.mult, ALU.add, (tg,), (tg,))
                        tt("pool", g1, g1, xg, ALU.mult, (tg, tPRG), (tg,))
                        act(g1, g1, AF.Sigmoid, (tg,), (tg,), scale=1.5957691216057308)
                        tt("pool", g1, g1, xg, ALU.mult, (tg, tPRG), (tg,))
                        tt("dve", g2, ht, HB[:, sl], ALU.add, (th_, tHB), (tg,))
                        y, ty = yo[yi % 2], tyo[yi % 2]
                        yi += 1
                        tt("dve", y, g2, g1, ALU.mult, (tg,), (ty,))
                        P.dma("sp", YRG_d[cc * 128:(cc + 1) * 128, sl], y, (ty,), (tYRG,))

    phaseC(PTC_d, tPTC, CTX, CTX, 256, True)
    reset_arena(PERSIST)
    phaseC(PT_d, tPT, L, TOWN, 2048, False)
    reset_arena(PERSIST)
    if stop_after == "C":
        return finish(nc, P, out_d, dbg)

    def load_fft_tables():
        tF = T()
        L1 = alloc([128, 16384], BF16); L2 = alloc([128, 16384], BF16)
        for q in range(4):
            P.dma("sp", L1[:, q * 4096:(q + 1) * 4096], C["L1"][:, q * 4096:(q + 1) * 4096], (), (tF,))
            P.dma("sp", L2[:, q * 4096:(q + 1) * 4096], C["L2"][:, q * 4096:(q + 1) * 4096], (), (tF,))
        F1 = alloc([128, 256], BF16)
        P.dma("sp", F1, C["F1"], (), (tF,))
        return tF, L1, L2, F1

    def fft_fwd(src, Krows, A, tA, F1, tF, tsrc, ev):
        for c in range(0, 32, 2):
            pb, tb = bank()
            for h_ in range(2):
                mm(pb[:, h_ * 256:(h_ + 1) * 256], src[0:Krows, :, c + h_], F1[0:Krows, :], True, True, (tsrc, tF), (tb,))
            cp("act" if (ev[0] % 2 == 0) else "dve", A[:, c:c + 2, :, :].rearrange("p c a k -> p (c a k)"), pb, (tb,), (tA,))
            ev[0] += 1

    def fft_s2(A, tA, L1, L2, tF, g):
        pb, tb = bank()
        for j in range(16):
            k1 = g * 16 + j
            mm(pb[:, j * 32:(j + 1) * 32], L1[:, k1 * 128:(k1 + 1) * 128], A[:, :, 0, k1], True, False, (tF, tA), (tb,))
            mm(pb[:, j * 32:(j + 1) * 32], L2[:, k1 * 128:(k1 + 1) * 128], A[:, :, 1, k1], False, True, (tF, tA), (tb,))
        return pb, tb

    def phaseD1():
        tF, L1, L2, F1 = load_fft_tables()
        PA = alloc([128, 128], BF16); PC = alloc([128, 128], BF16)
        P.dma("sp", PA, C["PA"], (), (tF,)); P.dma("sp", PC, C["PC"], (), (tF,))
        z2T = alloc([64, NFFT], BF16); tz = T()
        w3b = alloc([64, 2048], BF16); w3z = alloc([64, 1024], BF16)
        DLT = alloc([128, 1024], F32); ntn2 = alloc([128, 128], F32)
        P.dma("sp", DLT, C["DLT"], (), (tF,)); P.dma("sp", ntn2, C["ntn2"], (), (tF,))
        P.dma("pool", w3b, hyw3_d, (), (tF,))
        P.dma("pool", w3z, hyw3z_d, (), (tF,))
        ts("dve", w3b[:, 1024:2048], w3b[:, 1024:2048], -1.0, None, ALU.mult, None, (tF,), (tF,))
        mark = apos[0]
        w1 = alloc([50, 64], F32); w2 = alloc([64, 64], F32)
        b1 = alloc([64, 1], F32); b2 = alloc([64, 1], F32); fr = alloc([64, 1], F32)
        s1c = alloc([64, 1], F32); o1c = alloc([64, 1], F32); o2c = alloc([64, 1], F32)
        tM = T()
        for dst, src in ((w1, hyw1_d), (w2, hyw2_d), (b1, hyb1_d), (b2, hyb2_d), (fr, hyfr_d)):
            P.dma("sp", dst, src, (), (tM,))
        TWO_PI = 2.0 * np.pi
        ts("dve", s1c, fr, 1.0 / TWO_PI, None, ALU.mult, None, (tM,), (tM,))
        tt("dve", o1c, s1c, b1, ALU.mult, (tM,), (tM,))
        ts("dve", o1c, o1c, 8.0, None, ALU.add, None, (tM,), (tM,))
        tt("dve", o2c, s1c, b2, ALU.mult, (tM,), (tM,))
        ts("dve", o2c, o2c, 8.0, None, ALU.add, None, (tM,), (tM,))
        fts = [alloc([50, 512], F32) for _ in range(2)]; tft = [T(), T()]
        q_ = alloc([64, 512], F32); qi = alloc([64, 512], I32); qf = alloc([64, 512], F32); z1 = alloc([64, 512], F32)
        tq = T()

        def sin_layer(pb, tb, oc, out, tout):
            ts("dve", q_, pb[0:64, :], s1c, oc, ALU.mult, ALU.add, (tb, tM), (tq,))
            cp("dve", qi, q_, (tq,), (tq,))
            cp("dve", qf, qi, (tq,), (tq,))
            tt("dve", q_, q_, qf, ALU.subtract, (tq,), (tq,))
            act(out, q_, AF.Sin, (tq,), (tout,), scale=TWO_PI)

        for i in range(NFFT // 512):
            ft, tf_ = fts[i % 2], tft[i % 2]
            P.dma("sp", ft, C["featsT"][:, i * 512:(i + 1) * 512], (), (tf_,))
            pb, tb = bank()
            mm(pb[0:64, :], w1, ft, True, True, (tM, tf_), (tb,))
            sin_layer(pb, tb, o1c, z1, tq)
            pb2, tb2 = bank()
            mm(pb2[0:64, :], w2, z1, True, True, (tM, tq), (tb2,))
            sin_layer(pb2, tb2, o2c, z2T[:, i * 512:(i + 1) * 512], tz)
        P.barrier()
        apos[0] = mark
        kT = alloc([128, 128, 32], BF16); tk = T()
        A = alloc([128, 32, 2, 128], BF16); tA = T()
        Kpk = alloc([128, 4096], BF16); tK = T()
        KA = alloc([128, 4096], BF16); KC = alloc([128, 4096], BF16); tKA = T()
        decs = [alloc([128, 4096], BF16) for _ in range(2)]; tdec = [T(), T()]
        ev = [0]
        for sc in range(32):
            c0 = sc * 32
            dec, tdec_ = decs[sc % 2], tdec[sc % 2]
            P.dma("sp", dec, C["DEC"][sc], (), (tdec_,))
            for g in range(8):
                pb, tb = bank()
                for j in range(16):
                    n2 = g * 16 + j
                    mm(pb[0:64, j * 32:(j + 1) * 32], z2T[:, n2:8192:128], w3b[:, c0:c0 + 32], True, True, (tz, tF), (tb,))
                    mm(pb[64:128, j * 32:(j + 1) * 32], z2T[:, 8192 + n2:NFFT:128], w3b[:, 1024 + c0:1024 + c0 + 32], True, True, (tz, tF), (tb,))
                tt("dve", kT[:, g * 16:(g + 1) * 16, :].rearrange("p a b -> p (a b)"), pb, dec[:, g * 512:(g + 1) * 512], ALU.mult, (tb, tdec_), (tk,))
            pbz, tbz = bank()
            mm(pbz[0:1, 0:32], z2T[:, 0:1], w3z[:, c0:c0 + 32], True, True, (tz, tF), (tbz,))
            cp("dve", kT[0:1, 0, :], pbz[0:1, 0:32], (tbz,), (tk,))
            fft_fwd(kT, 128, A, tA, F1, tF, tk, ev)
            for g in range(8):
                pb, tb = fft_s2(A, tA, L1, L2, tF, g)
                cp("act", Kpk[:, g * 512:(g + 1) * 512], pb, (tb,), (tK,))
            for g in range(8):
                pa, ta = bank(); pc, tc_ = bank()
                mm(pa, PA, Kpk[:, g * 512:(g + 1) * 512], True, True, (tF, tK), (ta,))
                mm(pc, PC, Kpk[:, g * 512:(g + 1) * 512], True, True, (tF, tK), (tc_,))
                cp("act", KA[:, g * 512:(g + 1) * 512], pa, (ta,), (tKA,))
                cp("dve", KC[:, g * 512:(g + 1) * 512], pc, (tc_,), (tKA,))
            P.dma("sp", KAC_d[sc, :, 0, :], KA, (tKA,), (tKAC,))
            P.dma("sp", KAC_d[sc, :, 1, :], KC, (tKA,), (tKAC,))

    phaseD1()
    reset_arena(PERSIST)
    if stop_after == "D1":
        return finish(nc, P, out_d, dbg)

    def phaseD2():
        NN1 = TOWN // 128
        tF, L1, L2, F1 = load_fft_tables()
        R1 = alloc([128, 256], BF16); R2 = alloc([128, 256], BF16)
        P.dma("sp", R1, C["R1"], (), (tF,)); P.dma("sp", R2, C["R2"], (), (tF,))
        HR = alloc([128, 128, NN1], BF16); HI = alloc([128, 128, NN1], BF16)
        P.dma("sp", HR.rearrange("p a b -> p (a b)"), C["HR"], (), (tF,))
        P.dma("sp", HI.rearrange("p a b -> p (a b)"), C["HI"], (), (tF,))
        U = alloc([128, L], BF16); tU = T()
        X0 = alloc([128, TOWN], BF16); tX0 = T()
        Ys = alloc([128, 128, NN1], BF16); tYs = T()
        mark = apos[0]
        ev = [0]
        NPB = 512 // NN1
        chunks = [(96 * i, 96) for i in range(10)] + [(960, 64)]
        for ci_, (row0, nr) in enumerate(chunks):
            apos[0] = mark
            PH = alloc([128, L + 2], BF16); tPH = T()
            TM = alloc([128, L], BF16); tTM = T()
            memset("dve", PH[0:nr, 0:1], 0.0, (tPH,))
            memset("dve", PH[0:nr, L + 1:L + 2], 0.0, (tPH,))
            for gi, (dst, tdst_, Lg) in enumerate(((X0, tX0, TOWN), (TM, tTM, L), (U, tU, L))):
                r_ = (2 + gi) * D + row0
                Lld = min(L, Lg + 1)
                P.dma("sp", PH[0:nr, 1:Lld + 1], PT_d[r_:r_ + nr, 0:Lld], (tPT,), (tPH,))
                act(dst[0:nr, 0:Lg], PH[0:nr, 0:Lg], AF.Identity, (tPH, tC), (tdst_,), bias=hycb[0:nr, ci_, gi:gi + 1], scale=hycw[0:nr, ci_, gi, 0:1])
                for j in (1, 2):
                    stt(dst[0:nr, 0:Lg], PH[0:nr, j:j + Lg], hycw[0:nr, ci_, gi, j:j + 1], dst[0:nr, 0:Lg], ALU.mult, ALU.add, (tPH, tC), (tdst_,))
            tt("pool", U[0:nr, :], U[0:nr, :], TM[0:nr, :], ALU.mult, (tTM,), (tU,))
            P.barrier()
            apos[0] = mark
            UT = alloc([64, 128, 32], BF16); tUT = T()
            A = alloc([128, 32, 2, 128], BF16); tA = T()
            P1 = alloc([128, 128, 32], BF16); P2 = alloc([128, 128, 32], BF16); tP = T()
            KA = alloc([128, 128, 32], BF16); KC = alloc([128, 128, 32], BF16); tKA = T()
            Bv = alloc([128, 32, 2, 128], BF16); tB = T()

            def st_ka(s):
                sc = (row0 + 32 * s) // 32
                P.dma("sp", KA.rearrange("p a b -> p (a b)"), KAC_d[sc, :, 0, :], (tKAC,), (tKA,))
                P.dma("sp", KC.rearrange("p a b -> p (a b)"), KAC_d[sc, :, 1, :], (tKAC,), (tKA,))

            def st_trs1(s):
                for g in range(4):
                    pb, tb = bank()
                    pbb = pb.bitcast(BF16)
                    for j in range(32):
                        n2 = g * 32 + j
                        tr(pbb[0:64, j * 32:(j + 1) * 32], U[32 * s:32 * s + 32, n2:L:128], identb[32 * s:32 * s + 32, 32 * s:32 * s + 32], (tU, tC), (tb,))
                    cp("act" if g % 2 == 0 else "dve", UT[:, g * 32:(g + 1) * 32, :], pbb[0:64, :].rearrange("p (a b) -> p a b", a=32), (tb,), (tUT,))
                fft_fwd(UT, 64, A, tA, F1, tF, tUT, ev)

            def st_s2(s):
                for g in range(8):
                    pb, tb = fft_s2(A, tA, L1, L2, tF, g)
                    pv = pb.rearrange("p (a b) -> p a b", a=16)
                    tt("dve", P1[:, g * 16:(g + 1) * 16, :], pv, KA[:, g * 16:(g + 1) * 16, :], ALU.mult, (tb, tKA), (tP,))
                    tt("dve", P2[:, g * 16:(g + 1) * 16, :], pv, KC[:, g * 16:(g + 1) * 16, :], ALU.mult, (tb, tKA), (tP,))

            def st_s1p(s):
                for c in range(0, 32, 2):
                    pb, tb = bank()
                    for h_ in range(2):
                        mm(pb[:, h_ * 256:(h_ + 1) * 256], P1[:, :, c + h_], R1, True, False, (tP, tF), (tb,))
                        mm(pb[:, h_ * 256:(h_ + 1) * 256], P2[:, :, c + h_], R2, False, True, (tP, tF), (tb,))
                    cp("act" if (ev[0] % 2 == 0) else "dve", Bv[:, c:c + 2, :, :].rearrange("p c a k -> p (c a k)"), pb, (tb,), (tB,))
                    ev[0] += 1

            def st_s2p(s):
                rows = slice(32 * s, 32 * s + 32)
                for g in range(128 // NPB):
                    pb, tb = bank()
                    for j in range(NPB):
                        n2 = g * NPB + j
                        mm(pb[rows, j * NN1:(j + 1) * NN1], Bv[:, :, 0, n2], HR[:, n2, :], True, False, (tB, tF), (tb,))
                        mm(pb[rows, j * NN1:(j + 1) * NN1], Bv[:, :, 1, n2], HI[:, n2, :], False, True, (tB, tF), (tb,))
                    cp("act", Ys[rows, g * NPB:(g + 1) * NPB, :].rearrange("p a b -> p (a b)"), pb[rows, :], (tb,), (tYs,))

            ns_ = nr // 32
            st_ka(0); st_trs1(0); st_s2(0)
            for s in range(ns_):
                if s + 1 < ns_:
                    st_ka(s + 1)
                    st_trs1(s + 1)
                st_s1p(s)
                st_s2p(s)
                if s + 1 < ns_:
                    st_s2(s + 1)
            uo = U[0:nr, 0:TOWN]
            stt(uo.rearrange("p (n1 n2) -> p n1 n2", n2=128), uo.rearrange("p (n1 n2) -> p n1 n2", n2=128), hysk[0:nr, ci_:ci_ + 1],
                Ys[0:nr, :, :].rearrange("p n2 n1 -> p n1 n2"), ALU.mult, ALU.add, (tYs, tC, tU), (tU,))
            tt("dve", X0[0:nr, :], X0[0:nr, :], uo, ALU.mult, (tU, tX0), (tX0,))
            P.dma("sp", YHY_d[row0:row0 + nr, :], X0[0:nr, :], (tX0,), (tYHY,))
            P.barrier()

    phaseD2()
    reset_arena(PERSIST)
    if stop_after == "D2":
        return finish(nc, P, out_d, dbg)

    G1B = alloc([128, D], F32); G2B = alloc([128, D], F32); FGB = alloc([128, D], F32)
    P.dma("sp", FGB, fg_d.partition_broadcast(128), (), (tC,))
    dg = alloc([128, 128], F32); tdg = T()
    for (dst, gi) in ((G1B, 2), (G2B, 5)):
        for dc in range(NCH):
            ts("dve", dg, ident, modT[:, gi * 8 + dc, 0:1], None, ALU.mult, None, (tC,), (tdg,))
            pb2, tb2 = bank()
            mm(pb2[:, 0:128], ones, dg, True, True, (tC, tdg), (tb2,))
            cp("act", dst[:, dc * 128:(dc + 1) * 128], pb2[:, 0:128], (tb2,), (tC,))
    PERSIST2 = apos[0]

    def load_w(dram, rows_k, cols):
        w = alloc([128, rows_k, cols], BF16); tw = T()
        for k in range(rows_k):
            for c0 in range(0, cols, 1024):
                wd = min(1024, cols - c0)
                P.dma("pool", w[:, k, c0:c0 + wd], dram[k * 128:(k + 1) * 128, c0:c0 + wd], (), (tw,))
        return w, tw

    def phaseE():
        wrg, twrg = load_w(rgp_d, NCH, D)
        why, twhy = load_w(hyp_d, NCH, D)
        wo, two = load_w(wout_d, NCH, D)
        W = 512
        ins = [[alloc([128, NCH, W], BF16) for _ in range(4)] for _ in range(2)]
        tin = [T(), T()]
        mg = alloc([128, NCH, W], BF16); tmg = T()
        m1 = alloc([128, W], F32); m2 = alloc([128, W], F32); tm = T()
        xts = [alloc([128, D], F32) for _ in range(2)]; txs = [T(), T()]
        t1 = alloc([128, D], F32); tt1 = T()
        xi = 0
        for ti in range(TOWN // W):
            bufs, tb_in = ins[ti % 2], tin[ti % 2]
            tsl = slice(ti * W, (ti + 1) * W)
            for bi, (src, tsrc) in enumerate(((YRG_d, tYRG), (YHY_d, tYHY))):
                P.dma("sp", bufs[bi], src[:, tsl].rearrange("(k p) t -> p k t", p=128), (tsrc,), (tb_in,))
            for bi, g in ((2, 5), (3, 6)):
                P.dma("sp", bufs[bi], PT_d[g * D:(g + 1) * D, tsl].rearrange("(k p) t -> p k t", p=128), (tPT,), (tb_in,))
            for dc in range(NCH):
                pa, ta = bank(); ph, th_ = bank()
                for k in range(NCH):
                    mm(pa, wrg[:, k, dc * 128:(dc + 1) * 128], bufs[0][:, k, :], k == 0, k == NCH - 1, (twrg, tb_in), (ta,))
                for k in range(NCH):
                    mm(ph, why[:, k, dc * 128:(dc + 1) * 128], bufs[1][:, k, :], k == 0, k == NCH - 1, (twhy, tb_in), (th_,))
                tt("dve", m1, pa, bufs[2][:, dc, :], ALU.mult, (ta, tb_in), (tm,))
                tt("dve", m2, ph, bufs[3][:, dc, :], ALU.mult, (th_, tb_in), (tm,))
                tt("pool", mg[:, dc, :], m1, m2, ALU.add, (tm,), (tmg,))
            for a in range(W // 128):
                xt, tx = xts[xi % 2], txs[xi % 2]
                xi += 1
                r0 = ti * W + a * 128
                P.dma("sp", xt, x_d[r0:r0 + 128, :], (), (tx,))
                for half in range(2):
                    pb, tb = bank()
                    for k in range(NCH):
                        mm(pb, mg[:, k, a * 128:(a + 1) * 128], wo[:, k, half * 512:(half + 1) * 512], k == 0, k == NCH - 1, (tmg, two), (tb,))
                    tt("dve", t1[:, half * 512:(half + 1) * 512], pb, G1B[:, half * 512:(half + 1) * 512], ALU.mult, (tb, tC), (tt1,))
                tt("pool", xt, xt, t1, ALU.add, (tt1, tx), (tx,))
                P.dma("sp", X1_d[r0:r0 + 128, :], xt, (tx,), (tX1,))

    phaseE()
    reset_arena(PERSIST2)
    if stop_after == "E":
        return finish(nc, P, out_d, dbg)

    def phaseF():
        NT = TOWN // 128
        BS, NB = MOE_BS, MOE_NB
        PTOT = NB * BS
        XS_d = dscr("XS", [PTOT, D], BF16); YS_d = dscr("YS", [PTOT, D], BF16)
        tXS, tYS = T(), T()
        W1f = mw1_d.rearrange("e k f -> (e k) f"); W3f = mw3_d.rearrange("e k f -> (e k) f"); W2f = mw2_d.rearrange("e f d -> (e f) d")
        wge, twge = load_w(wge_d, NCH, 36)
        tK = T()
        tri = alloc([128, 128], F32); pkw = alloc([128, 8], F32); pkw2 = alloc([128, 4], F32); jbs = alloc([128, NB], F32)
        for dst, nm in ((tri, "tri"), (pkw, "pkw"), (pkw2, "pkw2"), (jbs, "jbs")):
            P.dma("sp", dst, C[nm], (), (tK,))
        A2B = alloc([128, D], F32); B2B = alloc([128, D], F32)
        dg2 = alloc([128, 128], F32); tdg2 = T()
        for (dst, col) in ((A2B, A2), (B2B, B2)):
            for dc in range(NCH):
                ts("dve", dg2, ident, col[:, dc:dc + 1], None, ALU.mult, None, (tC,), (tdg2,))
                pb2, tb2 = bank()
                mm(pb2[:, 0:128], ones, dg2, True, True, (tC, tdg2), (tb2,))
                cp("act", dst[:, dc * 128:(dc + 1) * 128], pb2[:, 0:128], (tb2,), (tK,))
        OH = alloc([128, NT, 64], F32); tOH = T()
        RK = alloc([128, NT, 2], F32); WTS = alloc([128, NT, 2], F32); tRK = T()
        DSTf = alloc([128, NT, 2], F32); DSTi = alloc([128, NT, 2], I32); tDST = T()
        S = alloc([128, 64], F32); tS = T()
        CNT = alloc([128, 64], F32); BASE = alloc([128, 64], F32); PEND = alloc([128, 32], F32); PADD = alloc([128, 32], F32)
        Z32 = alloc([128, 32], F32); tmp64 = alloc([128, 64], F32); ttmp = T()
        EB = alloc([128, NB], F32); EBs = alloc([128, NB], F32)
        IDX1f = alloc([128, NB, 8], F32); IDX1 = alloc([128, NB, 8], I32); IDX2f = alloc([128, NB, 4], F32); IDX2 = alloc([128, NB, 4], I32)
        tIDX = T()
        xts = [alloc([128, D], F32) for _ in range(2)]; txs = [T(), T()]
        junk = alloc([128, D], BF16); tj = T()
        sm = alloc([128, 96], F32); tsm = T()
        ss = alloc([128, 2], F32)
        hxT = alloc([128, NCH, 128], BF16); thx = T()
        tm32 = alloc([128, D], F32); ttm = T()
        mark = apos[0]
        HXTM = alloc([128, NT, D], BF16); tHX = T()
        memset("dve", S, 0.0, (tS,))
        memset("dve", Z32, 0.0, (ttmp,))
        for a in range(NT):
            xt, tx = xts[a % 2], txs[a % 2]
            r0 = a * 128
            P.dma("sp", xt, X1_d[r0:r0 + 128, :], (tX1,), (tx,))
            act(junk, xt, AF.Square, (tx,), (tj, tx), accum=ss[:, 0:1])
            act(ss[:, 0:1], ss[:, 0:1], AF.Sqrt, (tx,), (tx,), scale=1.0 / D, bias=EPS)
            P.op("dve", lambda h: h.reciprocal(out=ss[:, 0:1], in_=ss[:, 0:1]), (tx,), (tx,))
            ts("dve", xt, xt, ss[:, 0:1], None, ALU.mult, None, (tx,), (tx,))
            tt("pool", tm32, xt, A2B, ALU.mult, (tx, tK), (ttm,))
            tt("pool", HXTM[:, a, :], tm32, B2B, ALU.add, (ttm, tK), (tHX,))
            for dc in range(0, NCH, 4):
                pb, tb = bank()
                for j in range(4):
                    tr(pb[:, j * 128:(j + 1) * 128], xt[:, (dc + j) * 128:(dc + j + 1) * 128], ident, (tx, tC), (tb,))
                for j in range(4):
                    act(hxT[:, dc + j, :], pb[:, j * 128:(j + 1) * 128], AF.Identity, (tb, tC), (thx,),
                        bias=B2[:, dc + j:dc + j + 1], scale=A2[:, dc + j:dc + j + 1])
            pb, tb = bank()
            for k in range(NCH):
                mm(pb[:, 0:36], hxT[:, k, :], wge[:, k, :], k == 0, k == NCH - 1, (thx, twge), (tb,))
            lg = sm[:, 0:36]
            tt("dve", lg, pb[:, 0:36], bgeB, ALU.add, (tb, tC), (tsm,))
            gmax = sm[:, 36:37]; sg = sm[:, 37:38]; oh = sm[:, 38:42]; ein = sm[:, 42:50]; m8 = sm[:, 50:58]
            e4 = sm[:, 58:62]; ngm = sm[:, 62:63]; dd = sm[:, 63:64]; mk1 = sm[:, 64:72]; mk2 = sm[:, 72:80]
            P.op("dve", lambda h: h.tensor_reduce(out=gmax, in_=lg[:, 0:4], axis=AX.X, op=ALU.max), (tsm,), (tsm,))
            ts("dve", ngm, gmax, -1.0, None, ALU.mult, None, (tsm,), (tsm,))
            act(e4, lg[:, 0:4], AF.Exp, (tsm,), (tsm,), bias=ngm, accum=sg)
            P.op("dve", lambda h: h.reciprocal(out=sg, in_=sg), (tsm,), (tsm,))
            ts("dve", oh, lg[:, 0:4], gmax, None, ALU.is_equal, None, (tsm,), (tsm,))
            ts("dve", ein, lg[:, 4:12], oh[:, 0:1], None, ALU.mult, None, (tsm,), (tsm,))
            for g in range(1, 4):
                stt(ein, lg[:, 4 + 8 * g:12 + 8 * g], oh[:, g:g + 1], ein, ALU.mult, ALU.add, (tsm,), (tsm,))
            P.op("dve", lambda h: h.max(out=m8, in_=ein), (tsm,), (tsm,))
            tt("dve", dd, m8[:, 1:2], m8[:, 0:1], ALU.subtract, (tsm,), (tsm,))
            act(dd, dd, AF.Exp, (tsm,), (tsm,))
            ts("dve", e4[:, 0:1], dd, 1.0, None, ALU.add, None, (tsm,), (tsm,))
            P.op("dve", lambda h: h.reciprocal(out=e4[:, 0:1], in_=e4[:, 0:1]), (tsm,), (tsm,))
            tt("dve", e4[:, 1:2], dd, e4[:, 0:1], ALU.mult, (tsm,), (tsm,))
            tt("dve", WTS[:, a, 0:1], e4[:, 0:1], sg, ALU.mult, (tsm,), (tRK,))
            tt("dve", WTS[:, a, 1:2], e4[:, 1:2], sg, ALU.mult, (tsm,), (tRK,))
            ts("dve", mk1, ein, m8[:, 0:1], None, ALU.is_equal, None, (tsm,), (tsm,))
            ts("dve", mk2, ein, m8[:, 1:2], None, ALU.is_equal, None, (tsm,), (tsm,))
            for g in range(4):
                ts("dve", OH[:, a, g * 8:(g + 1) * 8], mk1, oh[:, g:g + 1], None, ALU.mult, None, (tsm,), (tOH,))
                ts("dve", OH[:, a, 32 + g * 8:32 + (g + 1) * 8], mk2, oh[:, g:g + 1], None, ALU.mult, None, (tsm,), (tOH,))
            pR, tR = bank()
            mm(pR[:, 0:64], tri, OH[:, a, :], True, False, (tK, tOH), (tR,))
            mm(pR[:, 0:64], ones, S, False, True, (tC, tS), (tR,))
            tt("dve", tmp64, pR[:, 0:64], OH[:, a, :], ALU.mult, (tR, tOH), (ttmp,))
            P.op("dve", lambda h, a=a: h.tensor_reduce(out=RK[:, a, :], in_=tmp64.rearrange("p (a b) -> p a b", a=2), axis=AX.X, op=ALU.add), (ttmp,), (tRK,))
            tt("pool", S, S, OH[:, a, :], ALU.add, (tOH, tS), (tS,))
        pT, tT_ = bank()
        mm(pT[:, 0:64], ones, S, True, True, (tC, tS), (tT_,))
        cp("dve", CNT, pT[:, 0:64], (tT_,), (ttmp,))
        tt("dve", PADD, CNT[:, 0:32], CNT[:, 32:64], ALU.add, (ttmp,), (ttmp,))
        ts("dve", PADD, PADD, 1.0 / BS, (BS - 1.0) / BS - 0.5 + 0.5 / BS, ALU.mult, ALU.add, (ttmp,), (ttmp,))
        cp("dve", DSTi[:, 0:16, :].rearrange("p a b -> p (a b)"), PADD, (ttmp,), (tDST,))
        cp("dve", PADD, DSTi[:, 0:16, :].rearrange("p a b -> p (a b)"), (tDST,), (ttmp,))
        ts("dve", PADD, PADD, float(BS), None, ALU.mult, None, (ttmp,), (ttmp,))
        P.op("dve", lambda h: h.tensor_tensor_scan(out=PEND, data0=PADD, data1=Z32, initial=0.0, op0=ALU.add, op1=ALU.add), (ttmp,), (ttmp,))
        tt("dve", BASE[:, 0:32], PEND, PADD, ALU.subtract, (ttmp,), (ttmp,))
        tt("dve", BASE[:, 32:64], BASE[:, 0:32], CNT[:, 0:32], ALU.add, (ttmp,), (ttmp,))
        for a in range(NT):
            tt("dve", tmp64, OH[:, a, :], BASE, ALU.mult, (tOH, ttmp), (ttmp,))
            P.op("dve", lambda h, a=a: h.tensor_reduce(out=DSTf[:, a, :], in_=tmp64.rearrange("p (a b) -> p a b", a=2), axis=AX.X, op=ALU.add), (ttmp,), (tDST,))
        tt("dve", DSTf.rearrange("p a b -> p (a b)"), DSTf.rearrange("p a b -> p (a b)"), RK.rearrange("p a b -> p (a b)"), ALU.add, (tDST, tRK), (tDST,))
        cp("dve", DSTi.rearrange("p a b -> p (a b)"), DSTf.rearrange("p a b -> p (a b)"), (tDST,), (tDST,))
        memset("dve", EB, 0.0, (tIDX,))
        for e in range(NE):
            stt(EB, jbs, PEND[:, e:e + 1], EB, ALU.is_ge, ALU.add, (tK, ttmp, tIDX), (tIDX,))
        ts("dve", EB, EB, float(NE - 1), None, ALU.min, None, (tIDX,), (tIDX,))
        ts("dve", EBs, EB, 1024.0, None, ALU.mult, None, (tIDX,), (tIDX,))
        for j in range(NB):
            ts("dve", IDX1f[:, j, :], pkw, EBs[:, j:j + 1], None, ALU.add, None, (tK, tIDX), (tIDX,))
        ts("dve", EBs, EB, 512.0, None, ALU.mult, None, (tIDX,), (tIDX,))
        for j in range(NB):
            ts("dve", IDX2f[:, j, :], pkw2, EBs[:, j:j + 1], None, ALU.add, None, (tK, tIDX), (tIDX,))
        cp("dve", IDX1.rearrange("p a b -> p (a b)"), IDX1f.rearrange("p a b -> p (a b)"), (tIDX,), (tIDX,))
        cp("dve", IDX2.rearrange("p a b -> p (a b)"), IDX2f.rearrange("p a b -> p (a b)"), (tIDX,), (tIDX,))
        for a in range(NT):
            for k in range(2):
                P.idma("pool", XS_d, HXTM[:, a, :], DSTi[:, a, k:k + 1], None, (tHX, tDST), (tXS,))
        P.barrier()
        apos[0] = mark
        w1s = [alloc([128, NCH, DE], BF16) for _ in range(2)]
        w3s = [alloc([128, NCH, DE], BF16) for _ in range(2)]
        w2s = [alloc([128, 4, D], BF16) for _ in range(2)]
        tws = [T(), T()]
        wstg = [alloc([128, DE], F32) for _ in range(12)]; twstg = [T() for _ in range(12)]
        wstg2 = [alloc([128, D], F32) for _ in range(4)]; twstg2 = [T() for _ in range(4)]
        wi2 = [0]
        wi = [0]
        mark3 = apos[0]
        NR = 3
        xss = [alloc([128, D], BF16) for _ in range(NR)]; txss = [T() for _ in range(NR)]
        xTs = [alloc([128, NCH, 128], BF16) for _ in range(NR)]; txT = [T() for _ in range(NR)]
        hids = [alloc([128, DE], BF16) for _ in range(NR)]; thid = [T() for _ in range(NR)]
        hTs = [alloc([128, 4, 128], BF16) for _ in range(NR)]; thT = [T() for _ in range(NR)]
        ybs = [alloc([128, D], BF16) for _ in range(NR)]; tyb = [T() for _ in range(NR)]
        s1s = [alloc([128, DE], F32) for _ in range(2)]; ts1s = [T(), T()]

        def wpieces(jb, grp):
            w1, w3, w2, tw = w1s[jb % 2], w3s[jb % 2], w2s[jb % 2], tws[jb % 2]
            pcs = []
            for k in range(NCH):
                pcs.append((w1[:, k, :], W1f, IDX1[:, jb, k:k + 1], False))
                pcs.append((w3[:, k, :], W3f, IDX1[:, jb, k:k + 1], False))
            for k in range(4):
                pcs.append((w2[:, k, :], W2f, IDX2[:, jb, k:k + 1], True))
            for (dst, src, idx, big) in pcs[grp * 5:(grp + 1) * 5]:
                if big:
                    sg_, tsg_ = wstg2[wi2[0] % len(wstg2)], twstg2[wi2[0] % len(wstg2)]
                    wi2[0] += 1
                else:
                    sg_, tsg_ = wstg[wi[0] % len(wstg)], twstg[wi[0] % len(wstg)]
                wi[0] += 1
                P.idma("pool", sg_, src, None, idx, (tIDX,), (tsg_,))
                cp("act" if wi[0] % 2 == 0 else "dve", dst, sg_, (tsg_,), (tw,))

        def stageA(s):
            jb, sb = s // 4, s % 4
            w1, w3, tw = w1s[jb % 2], w3s[jb % 2], tws[jb % 2]
            xs, txs_ = xss[s % NR], txss[s % NR]
            xT, txT_ = xTs[s % NR], txT[s % NR]
            hid, thid_ = hids[s % NR], thid[s % NR]
            s1, ts1 = s1s[s % 2], ts1s[s % 2]
            r0 = jb * BS + sb * 128
            P.dma("sp", xs, XS_d[r0:r0 + 128, :], (tXS,), (txs_,))
            pb, tb = bank()
            pbb = pb.bitcast(BF16)
            for k in range(NCH):
                tr(pbb[:, k * 128:(k + 1) * 128], xs[:, k * 128:(k + 1) * 128], identb, (txs_, tC), (tb,))
            cp("dve", xT.rearrange("p a b -> p (a b)"), pbb, (tb,), (txT_,))
            p1, tp1 = bank(); p3, tp3 = bank()
            for k in range(NCH):
                mm(p1, xT[:, k, :], w1[:, k, :], k == 0, k == NCH - 1, (txT_, tw), (tp1,))
            for k in range(NCH):
                mm(p3, xT[:, k, :], w3[:, k, :], k == 0, k == NCH - 1, (txT_, tw), (tp3,))
            act(s1, p1, AF.Silu, (tp1,), (ts1,))
            tt("dve", hid, s1, p3, ALU.mult, (ts1, tp3), (thid_,))

        def stageB(s):
            jb, sb = s // 4, s % 4
            w2, tw = w2s[jb % 2], tws[jb % 2]
            hid, thid_ = hids[s % NR], thid[s % NR]
            hT, thT_ = hTs[s % NR], thT[s % NR]
            yb, tyb_ = ybs[s % NR], tyb[s % NR]
            r0 = jb * BS + sb * 128
            pb2, tb2 = bank()
            pbb2 = pb2.bitcast(BF16)
            for f in range(4):
                tr(pbb2[:, f * 128:(f + 1) * 128], hid[:, f * 128:(f + 1) * 128], identb, (thid_, tC), (tb2,))
            cp("act", hT.rearrange("p a b -> p (a b)"), pbb2[:, 0:512], (tb2,), (thT_,))
            for half in range(2):
                py, tpy = bank()
                for f in range(4):
                    mm(py, hT[:, f, :], w2[:, f, half * 512:(half + 1) * 512], f == 0, f == 3, (thT_, tw), (tpy,))
                cp("act" if half == 0 else "dve", yb[:, half * 512:(half + 1) * 512], py, (tpy,), (tyb_,))
            P.dma("sp", YS_d[r0:r0 + 128, :], yb, (tyb_,), (tYS,))

        NS = NB * (BS // 128)
        for g in range(4):
            wpieces(0, g)
        for t in range(NS + 1):
            if t < NS:
                stageA(t)
            if t >= 1:
                stageB(t - 1)
            if t < NS:
                jn = t // 4 + 1
                if jn < NB:
                    wpieces(jn, t % 4)
        P.barrier()
        apos[0] = mark3
        y1s = [alloc([128, D], BF16) for _ in range(2)]; y2s = [alloc([128, D], BF16) for _ in range(2)]; tys = [T(), T()]
        mo = alloc([128, D], F32); tmo = T()
        for a in range(NT):
            y1, y2, ty = y1s[a % 2], y2s[a % 2], tys[a % 2]
            xt, tx = xts[a % 2], txs[a % 2]
            r0 = a * 128
            P.idma("pool", y1, YS_d, None, DSTi[:, a, 0:1], (tYS, tDST), (ty,))
            P.idma("pool", y2, YS_d, None, DSTi[:, a, 1:2], (tYS, tDST), (ty,))
            P.dma("sp", xt, X1_d[r0:r0 + 128, :], (tX1,), (tx,))
            ts("dve", mo, y1, WTS[:, a, 0:1], None, ALU.mult, None, (ty, tRK), (tmo,))
            stt(mo, y2, WTS[:, a, 1:2], mo, ALU.mult, ALU.add, (ty, tRK), (tmo,))
            tt("pool", mo, mo, G2B, ALU.mult, (tC,), (tmo,))
            tt("pool", xt, xt, mo, ALU.add, (tmo,), (tx,))
            act(junk, xt, AF.Square, (tx,), (tj, tx), accum=ss[:, 1:2])
            act(ss[:, 1:2], ss[:, 1:2], AF.Sqrt, (tx,), (tx,), scale=1.0 / D, bias=EPS)
            P.op("dve", lambda h: h.reciprocal(out=ss[:, 1:2], in_=ss[:, 1:2]), (tx,), (tx,))
            stt(xt, xt, ss[:, 1:2], FGB, ALU.mult, ALU.mult, (tx, tC), (tx,))
            P.dma("sp", out_d[r0:r0 + 128, :], xt, (tx,), (tX1,))

    phaseF()
    return finish(nc, P, out_d, dbg)


def finish(nc, P, out_d, dbg):
    P.barrier()
    P.emit()
    return nc


def make_inputs(inp, core):
    b, rev = core // 2, (core % 2 == 1)
    f = lambda a: np.ascontiguousarray(np.asarray(a, np.float32))
    m = {}
    xs = np.asarray(inp["x"][b], np.float32)
    cs = np.asarray(inp["ctx"][b], np.float32)
    m["x"] = f(xs[::-1] if rev else xs)
    m["ctx"] = f(cs[::-1] if rev else cs)
    cT = np.stack([fm(inp["c"][b]), fm(inp["c_ctx"])], axis=-1)
    m["cT"] = f(cT)
    m["ada_w"] = f(inp["ada_w"][0])
    m["ada_bT"] = fm(inp["ada_b"][0])
    m["g1nT"] = fm(inp["norm1_g"][0])
    m["g2nT"] = fm(inp["norm2_g"][0])
    m["final_g"] = f(inp["final_g"]).reshape(1, D)
    m["w_in"] = f(inp["w_in"][0])
    rw = np.asarray(inp["rg_conv_w"][0], np.float32)
    zero = np.zeros((D,), np.float32)
    taps5 = [zero, rw[3], rw[2], rw[1], rw[0]] if rev else [rw[0], rw[1], rw[2], rw[3], zero]
    m["rg_cwT"] = f(np.stack([fm(tp) for tp in taps5], axis=-1))
    m["rg_cbT"] = fm(inp["rg_conv_b"][0])
    bd = np.zeros((128, 4, NCH, 128), np.float32)
    gnames = ("rg_wa_b", "rg_wx_b", "rg_wa_f", "rg_wx_f") if rev else ("rg_wa_f", "rg_wx_f", "rg_wa_b", "rg_wx_b")
    bnames = ("rg_ba_b", "rg_bx_b", "rg_ba_f", "rg_bx_f") if rev else ("rg_ba_f", "rg_bx_f", "rg_ba_b", "rg_bx_b")
    lnames = ("rg_lam_b", "rg_lam_f") if rev else ("rg_lam_f", "rg_lam_b")
    for gi, nm in enumerate(gnames):
        w = np.asarray(inp[nm][0], np.float32)
        for cc in range(NCH):
            bd[0:64, gi, cc, 0:64] = w[2 * cc]
            bd[64:128, gi, cc, 64:128] = w[2 * cc + 1]
    m["rg_bd"] = bd
    m["rg_biasT"] = f(np.stack([fm(inp[nm][0]) for nm in bnames], axis=1))
    m["rg_lamT"] = f(np.stack([fm(inp[nm][0]) for nm in lnames], axis=1))
    m["rg_proj"] = f(inp["rg_proj"][0])
    def c96(v):
        o = np.zeros((11 * 96,), np.float32); o[:1024] = np.asarray(v, np.float32)
        return np.ascontiguousarray(o.reshape(11, 96).T)
    hw = np.asarray(inp["hy_conv_w"][0], np.float32); hb = np.asarray(inp["hy_conv_b"][0], np.float32)
    jt = (2, 1, 0) if rev else (0, 1, 2)
    m["hy_cw96"] = f(np.stack([np.stack([c96(hw[j, g * 1024:(g + 1) * 1024]) for j in jt], axis=-1) for g in range(3)], axis=2))
    m["hy_cb96"] = f(np.stack([c96(hb[g * 1024:(g + 1) * 1024]) for g in range(3)], axis=-1))
    m["hy_w1"] = f(inp["hy_pos_w1"][0])
    m["hy_b1T"] = f(inp["hy_pos_b1"][0]).reshape(64, 1)
    m["hy_w2"] = f(inp["hy_pos_w2"][0])
    m["hy_b2T"] = f(inp["hy_pos_b2"][0]).reshape(64, 1)
    m["hy_frT"] = f(inp["hy_freq"][0]).reshape(64, 1)
    w3 = np.asarray(inp["hy_pos_w3"][0], np.float32)
    m["hy_w3"] = f(np.concatenate([w3[:, 1024:], w3[:, :1024]], axis=1) if rev else w3)
    m["hy_w3z"] = f(w3[:, :1024])
    m["hy_sk96"] = c96(inp["hy_skip"][0])
    m["hy_proj"] = f(inp["hy_proj"][0])
    m["w_out"] = f(inp["w_out"][0])
    m["moe_wge"] = f(np.concatenate([inp["moe_wg"][0], inp["moe_we"][0]], axis=1))
    m["moe_bge"] = f(np.concatenate([inp["moe_bg"][0], inp["moe_be"][0]])).reshape(1, 36)
    m["moe_w1"] = f(inp["moe_w1"][0])
    m["moe_w3"] = f(inp["moe_w3"][0])
    m["moe_w2"] = f(inp["moe_w2"][0])
    for nm, arr in host_consts().items():
        m["k_" + nm] = arr
    return m


def kernel(**inputs):
    nc = build()
    in_maps = [make_inputs(inputs, c) for c in range(NCORES)]
    res = run_bass_kernel_spmd(nc, in_maps, core_ids=list(range(NCORES)))
    B = NCORES // 2
    out = np.empty((B, L, D), np.float32)
    for c in range(NCORES):
        r = np.asarray(res.results[c]["out"], np.float32)
        if c % 2 == 0:
            out[c // 2, 0:TOWN] = r
        else:
            out[c // 2, TOWN:L] = r[::-1]
    return out
```

```python
import numpy as np
import ml_dtypes
import concourse.bass as bass
import concourse.mybir as mybir
from concourse.bass_utils import run_bass_kernel_spmd

F32 = mybir.dt.float32
BF16 = mybir.dt.bfloat16
I32 = mybir.dt.int32
ALU = mybir.AluOpType
AF = mybir.ActivationFunctionType
AX = mybir.AxisListType

L = 8192
D = 1024
NCH = 8
NTT = L // 128
CTX = 256
NE = 32
DE = 512
NFFT = 16384
EPS = 1e-6
NCORES = 8
MOE_BS = 512
MOE_NB = 2 * 4096 // MOE_BS + 32
TOWN = 4096


class T:
    __slots__ = ("w", "r")

    def __init__(self):
        self.w = {}
        self.r = {}


class Eng:
    def __init__(self, name, key, sem):
        self.name = name
        self.key = key
        self.sem = sem
        self.count = 0
        self.waited = {}
        self.prog = []
        self.dsems = []
        self.dnext = 0


class Planner:
    def __init__(self, nc, ndma=8):
        self.nc = nc
        self.sems = []
        self.totals = []
        self.E = {}
        for name in ("pe", "act", "dve", "pool", "sp"):
            k = self._newsem(name)
            self.E[name] = Eng(name, k, self.sems[k])
        for q in ("sp", "pool", "act"):
            for i in range({"sp": 16, "pool": 24, "act": 2}[q]):
                self.E[q].dsems.append(self._newsem(f"d_{q}{i}"))

    def _newsem(self, name):
        self.sems.append(self.nc.alloc_semaphore(name=name))
        self.totals.append(0)
        return len(self.sems) - 1

    def _need(self, reads, writes):
        need = {}
        for t in reads:
            for k, v in t.w.items():
                if need.get(k, 0) < v:
                    need[k] = v
        for t in writes:
            for k, v in t.w.items():
                if need.get(k, 0) < v:
                    need[k] = v
            for k, v in t.r.items():
                if need.get(k, 0) < v:
                    need[k] = v
        return need

    def _emit_waits(self, E, need, skip_self=False):
        for k, v in need.items():
            if skip_self and k == E.key:
                continue
            if E.waited.get(k, 0) >= v:
                continue
            E.waited[k] = v
            E.prog.append(("w", k, v))

    def op(self, ename, fn, reads=(), writes=()):
        E = self.E[ename]
        need = self._need(reads, writes)
        self._emit_waits(E, need, skip_self=(ename == "pe"))
        E.count += 1
        E.prog.append(("o", fn, E.key))
        for t in reads:
            t.r[E.key] = E.count
        for t in writes:
            t.w[E.key] = E.count

    def dma(self, q, out, in_, reads=(), writes=()):
        E = self.E[q]
        need = self._need(reads, writes)
        k = E.dsems[E.dnext]
        E.dnext = (E.dnext + 1) % len(E.dsems)
        if self.totals[k] > 0:
            need[k] = max(need.get(k, 0), self.totals[k])
        self._emit_waits(E, need)
        self.totals[k] += 16
        E.prog.append(("d", out, in_, k))
        for t in reads:
            t.r[k] = self.totals[k]
        for t in writes:
            t.w[k] = self.totals[k]

    def idma(self, q, out, in_, out_off, in_off, reads=(), writes=(), bound=None):
        E = self.E[q]
        need = self._need(reads, writes)
        k = E.dsems[E.dnext]
        E.dnext = (E.dnext + 1) % len(E.dsems)
        if self.totals[k] > 0:
            need[k] = max(need.get(k, 0), self.totals[k])
        self._emit_waits(E, need)
        self.totals[k] += 16
        E.prog.append(("i", out, in_, out_off, in_off, k, bound))
        for t in reads:
            t.r[k] = self.totals[k]
        for t in writes:
            t.w[k] = self.totals[k]

    def barrier(self):
        need = {}
        for E in self.E.values():
            if E.count:
                need[E.key] = E.count
            for k in E.dsems:
                if self.totals[k]:
                    need[k] = self.totals[k]
        for E in self.E.values():
            n2 = {k: v for k, v in need.items() if k != E.key}
            self._emit_waits(E, n2)

    def emit(self):
        sems = self.sems

        def replay(E, h):
            regs = {}
            for it in E.prog:
                if it[0] == "w":
                    h.wait_ge(sems[it[1]], it[2])
                elif it[0] == "o":
                    it[1](h).then_inc(sems[it[2]], 1)
                elif it[0] == "i":
                    oo = None if it[3] is None else bass.IndirectOffsetOnAxis(ap=it[3], axis=0)
                    io = None if it[4] is None else bass.IndirectOffsetOnAxis(ap=it[4], axis=0)
                    if it[6] is None:
                        h.indirect_dma_start(out=it[1], out_offset=oo, in_=it[2], in_offset=io).then_inc(sems[it[5]], 16)
                    else:
                        if it[6] not in regs:
                            regs[it[6]] = h.to_reg(it[6])
                        h.indirect_dma_start(out=it[1], out_offset=oo, in_=it[2], in_offset=io, bounds_check=regs[it[6]], oob_is_err=False).then_inc(sems[it[5]], 16)
                else:
                    h.dma_start(out=it[1], in_=it[2]).then_inc(sems[it[3]], 16)

        with self.nc.Block() as block:
            @block.tensor
            def _(h):
                replay(self.E["pe"], h)

            @block.scalar
            def _(h):
                replay(self.E["act"], h)

            @block.vector
            def _(h):
                replay(self.E["dve"], h)

            @block.gpsimd
            def _(h):
                replay(self.E["pool"], h)

            @block.sync
            def _(h):
                replay(self.E["sp"], h)


def _bf(a):
    return np.ascontiguousarray(a.astype(np.float32)).astype(ml_dtypes.bfloat16)


_CONST = None


def host_consts():
    global _CONST
    if _CONST is not None:
        return _CONST
    N = NFFT
    c = {}
    c["ident"] = np.eye(128, dtype=np.float32)
    c["identb"] = _bf(np.eye(128))
    c["ones"] = np.ones((128, 128), np.float32)
    n1 = np.arange(128)[:, None].astype(np.float64)
    k1 = np.arange(128)[None, :].astype(np.float64)
    th = -2 * np.pi * n1 * (2 * k1 + 1) / 256.0
    c["F1"] = _bf(np.concatenate([np.cos(th), np.sin(th)], axis=1))
    n2 = np.arange(128)[:, None, None].astype(np.float64)
    kk1 = np.arange(128)[None, :, None].astype(np.float64)
    kk2 = np.arange(64)[None, None, :].astype(np.float64)
    ph = -2 * np.pi * n2 * (kk1 + 0.5 + 128 * kk2) / N
    Gre, Gim = np.cos(ph), np.sin(ph)
    c["L1"] = _bf(np.concatenate([Gre, Gim], axis=2).reshape(128, 128 * 128))
    c["L2"] = _bf(np.concatenate([-Gim, Gre], axis=2).reshape(128, 128 * 128))
    k2 = np.arange(64)[:, None].astype(np.float64)
    nn2 = np.arange(128)[None, :].astype(np.float64)
    cp = 2 * np.pi * k2 * nn2 / 128.0
    Cre, Cim = np.cos(cp), np.sin(cp)
    c["R1"] = _bf(np.concatenate([np.concatenate([Cre, Cim], 1), np.concatenate([-Cim, Cre], 1)], 0))
    c["R2"] = _bf(np.concatenate([np.concatenate([-Cim, Cre], 1), np.concatenate([Cre, Cim], 1)], 0))
    hk1 = np.arange(128)[:, None, None].astype(np.float64)
    hn2 = np.arange(128)[None, :, None].astype(np.float64)
    hn1 = np.arange(TOWN // 128)[None, None, :].astype(np.float64)
    hp = 2 * np.pi * (hk1 + 0.5) * (128 * hn1 + hn2) / N
    c["HR"] = _bf((2.0 / N) * np.cos(hp).reshape(128, 128 * (TOWN // 128)))
    c["HI"] = _bf(-(2.0 / N) * np.sin(hp).reshape(128, 128 * (TOWN // 128)))
    PA = np.zeros((128, 128)); PC = np.zeros((128, 128))
    for m in range(64):
        PA[m, m] = 1; PA[m, m + 64] = 1
        PC[64 + m, m] = 1; PC[64 + m, 64 + m] = -1
    c["PA"] = _bf(PA); c["PC"] = _bf(PC)
    n = np.arange(N)
    s = np.where(n <= 8192, n, N - n).astype(np.int64)
    s = np.where(n == 8192, 0, s)
    sf = s.astype(np.float32)
    t_norm = (sf / np.float32(L - 1)).astype(np.float32)
    bands = np.linspace(1e-4, 15, 16, dtype=np.float32)
    ang = (np.float32(2.0 * np.pi / L) * sf[:, None] * bands[None, :]).astype(np.float32)
    rows = L // 64
    row_lag = (s // 64).astype(np.float32) / np.float32(rows)
    colb = np.arange(1, 9, dtype=np.float32)
    cang = (np.float32(2.0 * np.pi / 64) * (s % 64).astype(np.float32)[:, None] * colb[None, :]).astype(np.float32)
    feats = np.concatenate([t_norm[:, None], np.cos(ang), np.sin(ang), row_lag[:, None],
                            np.cos(cang), np.sin(cang)], axis=-1).astype(np.float32)
    c["featsT"] = np.ascontiguousarray(feats.T)
    tn = t_norm.copy()
    tn[8192] = 1e4
    c["ntn2"] = np.ascontiguousarray((-tn).reshape(128, 128))
    mxd = np.log(1e-2) / 0.3
    mnd = np.log(1e-2) / 1.5
    deltas = np.abs(np.linspace(mnd, mxd, 1024, dtype=np.float32))
    c["DLT"] = np.ascontiguousarray(np.broadcast_to(deltas[None, :], (128, 1024))).astype(np.float32)
    dec = np.exp(-tn.astype(np.float32)[:, None] * deltas[None, :]).astype(np.float32)
    dec = dec.reshape(128, 128, 32, 32).transpose(2, 0, 1, 3).reshape(32, 128, 4096)
    c["DEC"] = _bf(dec)
    c["tri"] = np.triu(np.ones((128, 128), np.float32), k=1)
    pp = np.arange(128, dtype=np.float32)[:, None]
    c["pkw"] = np.ascontiguousarray(pp + 128.0 * np.arange(8, dtype=np.float32)[None, :])
    c["pkw2"] = np.ascontiguousarray(pp + 128.0 * np.arange(4, dtype=np.float32)[None, :])
    c["jbs"] = np.ascontiguousarray(np.broadcast_to((MOE_BS * np.arange(MOE_NB, dtype=np.float32))[None, :], (128, MOE_NB)))
    _CONST = c
    return c


def fm(v, nch=None):
    v = np.asarray(v, np.float32)
    return np.ascontiguousarray(v.reshape(-1, 128).T)


def build(debug=(), stop_after=None):
    nc = bass.Bass("TRN2", target_bir_lowering=False)
    P = Planner(nc)
    dbg = set(debug)

    def din(name, shape, dt=F32):
        return nc.dram_tensor(name, list(shape), dt, kind="ExternalInput").ap()

    def dscr(name, shape, dt):
        kind = "ExternalOutput" if name in dbg else "Internal"
        return nc.dram_tensor(name, list(shape), dt, kind=kind).ap()

    x_d = din("x", [L, D])
    ctx_d = din("ctx", [CTX, D])
    cT_d = din("cT", [128, NCH, 2])
    adaw_d = din("ada_w", [D, 6 * D])
    adab_d = din("ada_bT", [128, 48])
    g1n_d = din("g1nT", [128, NCH])
    g2n_d = din("g2nT", [128, NCH])
    fg_d = din("final_g", [1, D])
    win_d = din("w_in", [D, 7 * D])
    rgcw_d = din("rg_cwT", [128, NCH, 5])
    rgcb_d = din("rg_cbT", [128, NCH])
    rgbd_d = din("rg_bd", [128, 4, NCH, 128])
    rgbias_d = din("rg_biasT", [128, 4, NCH])
    rglam_d = din("rg_lamT", [128, 2, NCH])
    rgp_d = din("rg_proj", [D, D])
    hycw_d = din("hy_cw96", [96, 11, 3, 3])
    hycb_d = din("hy_cb96", [96, 11, 3])
    hyw1_d = din("hy_w1", [50, 64])
    hyb1_d = din("hy_b1T", [64, 1])
    hyw2_d = din("hy_w2", [64, 64])
    hyb2_d = din("hy_b2T", [64, 1])
    hyfr_d = din("hy_frT", [64, 1])
    hyw3_d = din("hy_w3", [64, 2048])
    hyw3z_d = din("hy_w3z", [64, 1024])
    hysk_d = din("hy_sk96", [96, 11])
    hyp_d = din("hy_proj", [D, D])
    wout_d = din("w_out", [D, D])
    wge_d = din("moe_wge", [D, 36])
    bge_d = din("moe_bge", [1, 36])
    mw1_d = din("moe_w1", [NE, D, DE])
    mw3_d = din("moe_w3", [NE, D, DE])
    mw2_d = din("moe_w2", [NE, DE, D])
    C = {}
    hc = host_consts()
    for nm, arr in hc.items():
        C[nm] = din("k_" + nm, arr.shape, BF16 if arr.dtype == ml_dtypes.bfloat16 else F32)
    out_d = nc.dram_tensor("out", [TOWN, D], F32, kind="ExternalOutput").ap()

    PT_d = dscr("PT", [7 * D, L], BF16)
    YRG_d = dscr("YRG", [D, TOWN], BF16)
    YHY_d = dscr("YHY", [D, TOWN], BF16)
    X1_d = dscr("X1", [TOWN, D], F32)
    KAC_d = dscr("KAC", [32, 128, 2, 4096], BF16)
    tPT, tYRG, tYHY, tX1, tKAC = T(), T(), T(), T(), T()

    banks = []
    for i in range(8):
        banks.append((nc.alloc_psum_tensor(f"ps{i}", [128, 512], F32)[:, :], T()))
    bstate = [0]

    def bank():
        b = banks[bstate[0] % 8]
        bstate[0] += 1
        return b

    ARENA = 196608 - 2048
    arena = nc.alloc_sbuf_tensor("arena", [128, ARENA // 2], BF16)
    apos = [0]

    def alloc(shape, dt):
        esz = 4 if dt in (F32, I32) else 2
        n = int(np.prod(shape[1:]))
        nbytes = (n * esz + 63) // 64 * 64
        off = apos[0]
        apos[0] += nbytes
        assert apos[0] <= ARENA, f"SBUF overflow {apos[0]}"
        ap = arena[0:shape[0], off // 2: off // 2 + n * esz // 2]
        if esz == 4:
            ap = ap.bitcast(dt)
        if len(shape) > 2:
            names = " ".join(f"a{i}" for i in range(len(shape) - 1))
            kw = {f"a{i}": int(shape[i + 1]) for i in range(len(shape) - 1)}
            ap = ap.rearrange(f"p ({names}) -> p {names}", **kw)
        return ap

    def reset_arena(to=0):
        P.barrier()
        apos[0] = to

    def act(out, in_, func, r, w, bias=None, scale=None, accum=None, eng="act"):
        kw = {}
        if bias is not None:
            kw["bias"] = bias
        if scale is not None:
            kw["scale"] = scale
        if accum is not None:
            kw["accum_out"] = accum
        P.op("act", lambda h: h.activation(out=out, in_=in_, func=func, **kw), r, w)

    def ts(eng, out, in0, s1, s2, op0, op1, r, w):
        if op1 is None:
            P.op(eng, lambda h: h.tensor_scalar(out=out, in0=in0, scalar1=s1, scalar2=None, op0=op0), r, w)
        else:
            P.op(eng, lambda h: h.tensor_scalar(out=out, in0=in0, scalar1=s1, scalar2=s2, op0=op0, op1=op1), r, w)

    def stt(out, in0, sc, in1, op0, op1, r, w):
        P.op("dve", lambda h: h.scalar_tensor_tensor(out=out, in0=in0, scalar=sc, in1=in1, op0=op0, op1=op1), r, w)

    def tt(eng, out, in0, in1, op, r, w):
        P.op(eng, lambda h: h.tensor_tensor(out=out, in0=in0, in1=in1, op=op), r, w)

    def cp(eng, out, in_, r, w):
        if eng == "act":
            P.op("act", lambda h: h.activation(out=out, in_=in_, func=AF.Copy), r, w)
        else:
            P.op(eng, lambda h: h.tensor_copy(out=out, in_=in_), r, w)

    def mm(out, lhsT, rhs, start, stop, r, w):
        P.op("pe", lambda h: h.matmul(out, lhsT, rhs, start=start, stop=stop), r, w)

    def tr(out, in_, ident, r, w):
        P.op("pe", lambda h: h.transpose(out, in_, ident), r, w)

    def memset(eng, ap, val, w):
        P.op(eng, lambda h: h.memset(ap, val), (), w)

    tC = T()
    ident = alloc([128, 128], F32)
    identb = alloc([128, 128], BF16)
    ones = alloc([128, 128], F32)
    modT = alloc([128, 48, 2], F32)
    A1 = alloc([128, NCH], F32); B1 = alloc([128, NCH], F32)
    A1c = alloc([128, NCH], F32); B1c = alloc([128, NCH], F32)
    A2 = alloc([128, NCH], F32); B2 = alloc([128, NCH], F32)
    pass
    rgcw = alloc([128, NCH, 5], F32); rgcb = alloc([128, NCH], F32)
    rgbias = alloc([128, 4, NCH], F32); nrgbias = alloc([128, 4, NCH], F32)
    rglam = alloc([128, 2, NCH], F32)
    sa1 = alloc([128, 2, NCH], F32); sa2 = alloc([128, 2, NCH], F32)
    hycw = alloc([96, 11, 3, 3], F32); hycb = alloc([96, 11, 3], F32); hysk = alloc([96, 11], F32)
    H0 = alloc([128, 2, NCH], F32)
    bgeB = alloc([128, 36], F32)
    PERSIST = apos[0]

    P.dma("sp", ident, C["ident"], (), (tC,))
    P.dma("sp", identb, C["identb"], (), (tC,))
    P.dma("sp", ones, C["ones"], (), (tC,))
    for dst, src in ((rgcw, rgcw_d), (rgcb, rgcb_d), (rgbias, rgbias_d), (rglam, rglam_d),
                     (hycw, hycw_d), (hycb, hycb_d), (hysk, hysk_d)):
        P.dma("sp", dst, src, (), (tC,))
    P.dma("sp", bgeB, bge_d.partition_broadcast(128), (), (tC,))

    def phaseA():
        cT = alloc([128, NCH, 2], F32)
        scT = alloc([128, NCH, 2], F32)
        adab = alloc([128, 48], F32)
        g1n = alloc([128, NCH], F32); g2n = alloc([128, NCH], F32)
        tmp = alloc([128, NCH], F32)
        dg = alloc([128, 128], F32)
        wbuf = [alloc([128, 1536], F32) for _ in range(3)]
        tw = [T() for _ in range(3)]
        tS = T()
        P.dma("sp", cT, cT_d, (), (tS,))
        P.dma("sp", adab, adab_d, (), (tS,))
        P.dma("sp", g1n, g1n_d, (), (tS,))
        P.dma("sp", g2n, g2n_d, (), (tS,))
        act(scT, cT, AF.Silu, (tS,), (tS,))
        pb, tb = bank()
        i = 0
        for k in range(NCH):
            for q in range(4):
                wb, twb = wbuf[i % 3], tw[i % 3]
                i += 1
                P.dma("sp", wb, adaw_d[k * 128:(k + 1) * 128, q * 1536:(q + 1) * 1536], (), (twb,))
                for jj in range(12):
                    j = q * 12 + jj
                    mm(pb[:, 2 * j:2 * j + 2], wb[:, jj * 128:(jj + 1) * 128], scT[:, k, :],
                       (k == 0 and j == 0), (k == NCH - 1 and j == 47), (twb, tS), (tb,))
        for col in range(2):
            tt("dve", modT[:, :, col], pb[:, col:96:2], adab, ALU.add, (tb, tS), (tC,))
        for (Ad, Bd, gn, sci, shi, col) in ((A1, B1, g1n, 1, 0, 0), (A1c, B1c, g1n, 1, 0, 1), (A2, B2, g2n, 4, 3, 0)):
            ts("dve", tmp, modT[:, sci * 8:(sci + 1) * 8, col], 1.0, None, ALU.add, None, (tC,), (tS,))
            tt("dve", Ad, tmp, gn, ALU.mult, (tS,), (tC,))
            cp("dve", Bd, modT[:, shi * 8:(shi + 1) * 8, col], (tC,), (tC,))
        act(sa1, rglam, AF.Exp, (tC,), (tC,), scale=-1.0)
        act(sa1, sa1, AF.Ln, (tC,), (tC,), bias=1.0)
        ts("dve", sa2, sa1, -16.0, None, ALU.mult, None, (tC,), (tC,))
        ts("dve", sa1, sa1, -8.0, None, ALU.mult, None, (tC,), (tC,))
        ts("dve", nrgbias, rgbias, -1.0, None, ALU.mult, None, (tC,), (tC,))

    phaseA()
    reset_arena(PERSIST)

    def phaseB(src_d, ntok, W, ccs_all, ccs_fn, Asc, Bsc, dst_d, tdst, sig_from=40):
        nsub = W // 128
        ncol = len(ccs_all)
        pos = {cc: i for i, cc in enumerate(ccs_all)}
        winb = alloc([128, NCH, ncol * 128], BF16)
        tWs = [T() for _ in range((ncol + 7) // 8)]
        c0 = ccs_all[0] * 128
        for blk in range(0, ncol * 128, 1024):
            wd = min(1024, ncol * 128 - blk)
            for k in range(NCH):
                P.dma("pool", winb[:, k, blk:blk + wd], win_d[k * 128:(k + 1) * 128, c0 + blk:c0 + blk + wd], (), (tWs[blk // 1024],))
        xts = [alloc([128, nsub, D], F32) for _ in range(2)]
        txs = [T(), T()]
        junk = alloc([128, D], BF16); tj = T()
        ss = [alloc([128, nsub], F32) for _ in range(2)]
        hxs = [alloc([128, NCH, W], BF16) for _ in range(2)]
        ths = [T(), T()]
        stg = [alloc([128, 4, W], BF16) for _ in range(3)]
        tst = [(T(), T()) for _ in range(3)]
        si = 0
        for ti in range(ntok // W):
            ccs = ccs_fn(ti)
            xt, tx, s_, hx, th = xts[ti % 2], txs[ti % 2], ss[ti % 2], hxs[ti % 2], ths[ti % 2]
            P.dma("sp", xt, src_d[ti * W:(ti + 1) * W, :].rearrange("(a p) d -> p a d", p=128), (), (tx,))
            for a in range(nsub):
                act(junk, xt[:, a, :], AF.Square, (tx,), (tj, tx), accum=s_[:, a:a + 1])
            act(s_, s_, AF.Sqrt, (tx,), (tx,), scale=1.0 / D, bias=EPS)
            P.op("dve", lambda h, s_=s_: h.reciprocal(out=s_, in_=s_), (tx,), (tx,))
            for a in range(nsub):
                ts("dve" if a % 2 == 0 else "pool", xt[:, a, :], xt[:, a, :], s_[:, a:a + 1], None, ALU.mult, None, (tx,), (tx,))
            for dc in range(NCH):
                pb, tb = bank()
                for a in range(nsub):
                    tr(pb[:, a * 128:(a + 1) * 128], xt[:, a, dc * 128:(dc + 1) * 128], ident, (tx, tC), (tb,))
                act(hx[:, dc, :], pb[:, 0:W], AF.Identity, (tb, tC), (th,), bias=Bsc[:, dc:dc + 1], scale=Asc[:, dc:dc + 1])
            for ci, cc in enumerate(ccs):
                pb, tb = bank()
                wi = pos[cc]
                for k in range(NCH):
                    mm(pb[:, 0:W], winb[:, k, wi * 128:(wi + 1) * 128], hx[:, k, :], k == 0, k == NCH - 1, (tWs[wi // 8], th), (tb,))
                sg, tsg = stg[si % 3], tst[si % 3]
                if cc >= sig_from:
                    act(sg[:, ci % 4, :], pb[:, 0:W], AF.Sigmoid, (tb,), (tsg[0],))
                elif ci % 2 == 0:
                    cp("act", sg[:, ci % 4, :], pb[:, 0:W], (tb,), (tsg[0],))
                else:
                    cp("dve", sg[:, ci % 4, :], pb[:, 0:W], (tb,), (tsg[1],))
                if ci % 4 == 3:
                    r0 = ccs[ci - 3] * 128
                    P.dma("sp", dst_d[r0:r0 + 512, ti * W:(ti + 1) * W].rearrange("(a p) t -> p a t", p=128), sg, tsg, (tdst,))
                    si += 1

    NOWN5 = TOWN // 512
    CC_ALL = list(range(56))
    CC_REST = list(range(0, 8)) + list(range(24, 40))
    CC_HALO = CC_REST + list(range(16, 24))

    def ccs_main(ti):
        if ti < NOWN5:
            return CC_ALL
        if ti == NOWN5:
            return CC_HALO
        return CC_REST

    phaseB(x_d, L, 512, CC_ALL, ccs_main, A1, B1, PT_d, tPT)
    reset_arena(PERSIST)
    if stop_after == "B":
        return finish(nc, P, out_d, dbg)

    PTC_d = dscr("PTC", [D, CTX], BF16)
    tPTC = T()
    phaseB(ctx_d, CTX, 256, list(range(8)), lambda ti: list(range(8)), A1c, B1c, PTC_d, tPTC)
    reset_arena(PERSIST)

    def phaseC(src_d, tsrc, Lt, Lown, TW, is_ctx):
        ntile = Lt // TW
        nown = Lown // TW
        nq = TW // 512 if TW >= 512 else 1
        QW = min(512, TW)
        wbd = alloc([128, 4, NCH, 128], BF16); tWb = T()
        P.dma("pool", wbd.rearrange("p a b c -> p (a b c)"), rgbd_d.rearrange("p a b c -> p (a b c)"), (), (tWb,))
        PX = alloc([128, Lt + 4], BF16); tPX = T()
        xc = alloc([128, Lt], BF16); txc = T()
        if not is_ctx:
            PRG = alloc([128, Lown], BF16); tPRG = T()
            HB = alloc([128, Lown], BF16); tHB = T()
            g1 = alloc([128, TW], F32); g2 = alloc([128, TW], F32); tg = T()
            yo = [alloc([128, TW], BF16) for _ in range(2)]; tyo = [T(), T()]
        NG = 2
        rts = [alloc([128, TW], F32) for _ in range(NG)]
        ats = [alloc([128, TW], F32) for _ in range(NG)]
        its = [alloc([128, TW], F32) for _ in range(NG)]
        tgts = [T() for _ in range(NG)]
        hts = [alloc([128, TW], F32) for _ in range(2)]; tht = [T(), T()]
        memset("dve", PX[:, 0:2], 0.0, (tPX,))
        memset("dve", PX[:, Lt + 2:Lt + 4], 0.0, (tPX,))
        hcount = 0
        yi = 0
        gi_ = 0
        for cc in range(NCH):
            P.dma("sp", PX[:, 2:Lt + 2], src_d[cc * 128:(cc + 1) * 128, 0:Lt], (tsrc,), (tPX,))
            if not is_ctx:
                P.dma("sp", PRG, src_d[D + cc * 128:D + (cc + 1) * 128, 0:Lown], (tsrc,), (tPRG,))
            act(xc, PX[:, 0:Lt], AF.Identity, (tPX, tC), (txc,), bias=rgcb[:, cc:cc + 1], scale=rgcw[:, cc, 0:1])
            for j in range(1, 5):
                stt(xc, PX[:, j:j + Lt], rgcw[:, cc, j:j + 1], xc, ALU.mult, ALU.add, (tPX, tC), (txc,))
            for d in (1, 0):
                order = range(ntile - 1, -1, -1) if d == 1 else range(nown)
                first = True
                for ti in order:
                    sl = slice(ti * TW, (ti + 1) * TW)
                    rt, at, it, tgt = rts[gi_ % NG], ats[gi_ % NG], its[gi_ % NG], tgts[gi_ % NG]
                    gi_ += 1
                    prs, pis = [], []
                    for q in range(nq):
                        pr, tr_ = bank(); pi, ti_ = bank()
                        mm(pr[:, 0:QW], wbd[:, 2 * d, cc, :], xc[:, ti * TW + q * QW: ti * TW + (q + 1) * QW], True, True, (tWb, txc), (tr_,))
                        mm(pi[:, 0:QW], wbd[:, 2 * d + 1, cc, :], xc[:, ti * TW + q * QW: ti * TW + (q + 1) * QW], True, True, (tWb, txc), (ti_,))
                        prs.append((pr, tr_)); pis.append((pi, ti_))
                    for q in range(nq):
                        act(rt[:, q * QW:(q + 1) * QW], prs[q][0][:, 0:QW], AF.Sigmoid, (prs[q][1], tC), (tgt,), bias=rgbias[:, 2 * d, cc:cc + 1])
                    for q in range(nq):
                        act(it[:, q * QW:(q + 1) * QW], pis[q][0][:, 0:QW], AF.Sigmoid, (pis[q][1], tC), (tgt,), bias=rgbias[:, 2 * d + 1, cc:cc + 1])
                    act(at, rt, AF.Exp, (tgt, tC), (tgt,), scale=sa1[:, d, cc:cc + 1])
                    act(rt, rt, AF.Exp, (tgt, tC), (tgt,), scale=sa2[:, d, cc:cc + 1])
                    act(rt, rt, AF.Sqrt, (tgt,), (tgt,), scale=-1.0, bias=1.0)
                    tt("pool", it, it, xc[:, sl], ALU.mult, (tgt, txc), (tgt,))
                    tt("dve", rt, rt, it, ALU.mult, (tgt,), (tgt,))
                    ht, th_ = hts[hcount % 2], tht[hcount % 2]
                    hp, thp = hts[(hcount + 1) % 2], tht[(hcount + 1) % 2]
                    hcount += 1
                    if first:
                        init = 0.0 if is_ctx else H0[:, d, cc:cc + 1]
                        rd = (tgt,) if is_ctx else (tgt, tC)
                    else:
                        init = hp[:, 0:1] if d == 1 else hp[:, TW - 1:TW]
                        rd = (tgt, thp)
                    first = False
                    if d == 1:
                        P.op("dve", lambda h, ht=ht, init=init, at=at, rt=rt: h.tensor_tensor_scan(out=ht[:, ::-1], data0=at[:, ::-1], data1=rt[:, ::-1], initial=init, op0=ALU.mult, op1=ALU.add), rd, (th_,))
                    else:
                        P.op("dve", lambda h, ht=ht, init=init, at=at, rt=rt: h.tensor_tensor_scan(out=ht, data0=at, data1=rt, initial=init, op0=ALU.mult, op1=ALU.add), rd, (th_,))
                    if is_ctx:
                        last = (ti == 0) if d == 1 else (ti == ntile - 1)
                        if last:
                            col = ht[:, 0:1] if d == 1 else ht[:, TW - 1:TW]
                            cp("dve", H0[:, d, cc:cc + 1], col, (th_,), (tC,))
                        continue
                    if d == 1:
                        if ti < nown:
                            cp("act", HB[:, sl], ht, (th_,), (tHB,))
                    else:
                        xg = PRG[:, sl]
                        tt("pool", g1, xg, xg, ALU.mult, (tPRG,), (tg,))
                        ts("pool", g1, g1, 0.044715, 1.0, ALU.mult, ALU.add, (tg,), (tg,))
                        tt("pool", g1, g1, xg, ALU.mult, (tg, tPRG), (tg,))
                        act(g1, g1, AF.Sigmoid, (tg,), (tg,), scale=1.5957691216057308)
                        tt("pool", g1, g1, xg, ALU.mult, (tg, tPRG), (tg,))
                        tt("dve", g2, ht, HB[:, sl], ALU.add, (th_, tHB), (tg,))
                        y, ty = yo[yi % 2], tyo[yi % 2]
                        yi += 1
                        tt("dve", y, g2, g1, ALU.mult, (tg,), (ty,))
                        P.dma("sp", YRG_d[cc * 128:(cc + 1) * 128, sl], y, (ty,), (tYRG,))

    phaseC(PTC_d, tPTC, CTX, CTX, 256, True)
    reset_arena(PERSIST)
    phaseC(PT_d, tPT, L, TOWN, 2048, False)
    reset_arena(PERSIST)
    if stop_after == "C":
        return finish(nc, P, out_d, dbg)

    def load_fft_tables():
        tF = T()
        L1 = alloc([128, 16384], BF16); L2 = alloc([128, 16384], BF16)
        for q in range(4):
            P.dma("sp", L1[:, q * 4096:(q + 1) * 4096], C["L1"][:, q * 4096:(q + 1) * 4096], (), (tF,))
            P.dma("sp", L2[:, q * 4096:(q + 1) * 4096], C["L2"][:, q * 4096:(q + 1) * 4096], (), (tF,))
        F1 = alloc([128, 256], BF16)
        P.dma("sp", F1, C["F1"], (), (tF,))
        return tF, L1, L2, F1

    def fft_fwd(src, Krows, A, tA, F1, tF, tsrc, ev):
        for c in range(0, 32, 2):
            pb, tb = bank()
            for h_ in range(2):
                mm(pb[:, h_ * 256:(h_ + 1) * 256], src[0:Krows, :, c + h_], F1[0:Krows, :], True, True, tuple(tsrc) + (tF,), (tb,))
            cp("act" if (ev[0] % 2 == 0) else "dve", A[:, c:c + 2, :, :].rearrange("p c a k -> p (c a k)"), pb, (tb,), (tA[ev[0] % 2],))
            ev[0] += 1

    def fft_s2(A, tA, L1, L2, tF, g):
        pb, tb = bank()
        for j in range(16):
            k1 = g * 16 + j
            mm(pb[:, j * 32:(j + 1) * 32], L1[:, k1 * 128:(k1 + 1) * 128], A[:, :, 0, k1], True, False, (tF,) + tuple(tA), (tb,))
            mm(pb[:, j * 32:(j + 1) * 32], L2[:, k1 * 128:(k1 + 1) * 128], A[:, :, 1, k1], False, True, (tF,) + tuple(tA), (tb,))
        return pb, tb

    def phaseD1():
        tF, L1, L2, F1 = load_fft_tables()
        PA = alloc([128, 128], BF16); PC = alloc([128, 128], BF16)
        P.dma("sp", PA, C["PA"], (), (tF,)); P.dma("sp", PC, C["PC"], (), (tF,))
        z2T = alloc([64, NFFT], BF16); tz = T()
        w3b = alloc([64, 2048], BF16); w3z = alloc([64, 1024], BF16)
        DLT = alloc([128, 1024], F32); ntn2 = alloc([128, 128], F32)
        P.dma("sp", DLT, C["DLT"], (), (tF,)); P.dma("sp", ntn2, C["ntn2"], (), (tF,))
        P.dma("pool", w3b, hyw3_d, (), (tF,))
        P.dma("pool", w3z, hyw3z_d, (), (tF,))
        ts("dve", w3b[:, 1024:2048], w3b[:, 1024:2048], -1.0, None, ALU.mult, None, (tF,), (tF,))
        mark = apos[0]
        w1 = alloc([50, 64], F32); w2 = alloc([64, 64], F32)
        b1 = alloc([64, 1], F32); b2 = alloc([64, 1], F32); fr = alloc([64, 1], F32)
        s1c = alloc([64, 1], F32); o1c = alloc([64, 1], F32); o2c = alloc([64, 1], F32)
        tM = T()
        for dst, src in ((w1, hyw1_d), (w2, hyw2_d), (b1, hyb1_d), (b2, hyb2_d), (fr, hyfr_d)):
            P.dma("sp", dst, src, (), (tM,))
        TWO_PI = 2.0 * np.pi
        ts("dve", s1c, fr, 1.0 / TWO_PI, None, ALU.mult, None, (tM,), (tM,))
        tt("dve", o1c, s1c, b1, ALU.mult, (tM,), (tM,))
        ts("dve", o1c, o1c, 8.0, None, ALU.add, None, (tM,), (tM,))
        tt("dve", o2c, s1c, b2, ALU.mult, (tM,), (tM,))
        ts("dve", o2c, o2c, 8.0, None, ALU.add, None, (tM,), (tM,))
        fts = [alloc([50, 512], F32) for _ in range(2)]; tft = [T(), T()]
        q_ = alloc([64, 512], F32); qi = alloc([64, 512], I32); qf = alloc([64, 512], F32); z1 = alloc([64, 512], F32)
        tq = T()

        def sin_layer(pb, tb, oc, out, tout):
            ts("dve", q_, pb[0:64, :], s1c, oc, ALU.mult, ALU.add, (tb, tM), (tq,))
            cp("dve", qi, q_, (tq,), (tq,))
            cp("dve", qf, qi, (tq,), (tq,))
            tt("dve", q_, q_, qf, ALU.subtract, (tq,), (tq,))
            act(out, q_, AF.Sin, (tq,), (tout,), scale=TWO_PI)

        for i in range(NFFT // 512):
            ft, tf_ = fts[i % 2], tft[i % 2]
            P.dma("sp", ft, C["featsT"][:, i * 512:(i + 1) * 512], (), (tf_,))
            pb, tb = bank()
            mm(pb[0:64, :], w1, ft, True, True, (tM, tf_), (tb,))
            sin_layer(pb, tb, o1c, z1, tq)
            pb2, tb2 = bank()
            mm(pb2[0:64, :], w2, z1, True, True, (tM, tq), (tb2,))
            sin_layer(pb2, tb2, o2c, z2T[:, i * 512:(i + 1) * 512], tz)
        P.barrier()
        apos[0] = mark
        kT = alloc([128, 128, 32], BF16); tk = T()
        A = alloc([128, 32, 2, 128], BF16); tA = (T(), T())
        Kpk = alloc([128, 4096], BF16); tK = T()
        KA = alloc([128, 4096], BF16); KC = alloc([128, 4096], BF16); tKA = (T(), T())
        decs = [alloc([128, 4096], BF16) for _ in range(2)]; tdec = [T(), T()]
        ev = [0]
        for sc in range(32):
            c0 = sc * 32
            dec, tdec_ = decs[sc % 2], tdec[sc % 2]
            P.dma("sp", dec, C["DEC"][sc], (), (tdec_,))
            for g in range(8):
                pb, tb = bank()
                for j in range(16):
                    n2 = g * 16 + j
                    mm(pb[0:64, j * 32:(j + 1) * 32], z2T[:, n2:8192:128], w3b[:, c0:c0 + 32], True, True, (tz, tF), (tb,))
                    mm(pb[64:128, j * 32:(j + 1) * 32], z2T[:, 8192 + n2:NFFT:128], w3b[:, 1024 + c0:1024 + c0 + 32], True, True, (tz, tF), (tb,))
                tt("dve", kT[:, g * 16:(g + 1) * 16, :].rearrange("p a b -> p (a b)"), pb, dec[:, g * 512:(g + 1) * 512], ALU.mult, (tb, tdec_), (tk,))
            pbz, tbz = bank()
            mm(pbz[0:1, 0:32], z2T[:, 0:1], w3z[:, c0:c0 + 32], True, True, (tz, tF), (tbz,))
            cp("dve", kT[0:1, 0, :], pbz[0:1, 0:32], (tbz,), (tk,))
            fft_fwd(kT, 128, A, tA, F1, tF, (tk,), ev)
            for g in range(8):
                pb, tb = fft_s2(A, tA, L1, L2, tF, g)
                cp("act", Kpk[:, g * 512:(g + 1) * 512], pb, (tb,), (tK,))
            for g in range(8):
                pa, ta = bank(); pc, tc_ = bank()
                mm(pa, PA, Kpk[:, g * 512:(g + 1) * 512], True, True, (tF, tK), (ta,))
                mm(pc, PC, Kpk[:, g * 512:(g + 1) * 512], True, True, (tF, tK), (tc_,))
                cp("act", KA[:, g * 512:(g + 1) * 512], pa, (ta,), (tKA[0],))
                cp("dve", KC[:, g * 512:(g + 1) * 512], pc, (tc_,), (tKA[1],))
            P.dma("sp", KAC_d[sc, :, 0, :], KA, (tKA[0],), (tKAC,))
            P.dma("sp", KAC_d[sc, :, 1, :], KC, (tKA[1],), (tKAC,))

    phaseD1()
    reset_arena(PERSIST)
    if stop_after == "D1":
        return finish(nc, P, out_d, dbg)

    def phaseD2():
        NN1 = TOWN // 128
        tF, L1, L2, F1 = load_fft_tables()
        R1 = alloc([128, 256], BF16); R2 = alloc([128, 256], BF16)
        P.dma("sp", R1, C["R1"], (), (tF,)); P.dma("sp", R2, C["R2"], (), (tF,))
        HR = alloc([128, 128, NN1], BF16); HI = alloc([128, 128, NN1], BF16)
        P.dma("sp", HR.rearrange("p a b -> p (a b)"), C["HR"], (), (tF,))
        P.dma("sp", HI.rearrange("p a b -> p (a b)"), C["HI"], (), (tF,))
        U = alloc([128, L], BF16); tU = T()
        X0 = alloc([128, TOWN], BF16); tX0 = T()
        Ys = alloc([128, 128, NN1], BF16); tYs = T()
        mark = apos[0]
        ev = [0]
        NPB = 512 // NN1
        chunks = [(96 * i, 96) for i in range(10)] + [(960, 64)]
        for ci_, (row0, nr) in enumerate(chunks):
            apos[0] = mark
            PH = alloc([128, L + 2], BF16); tPH = T()
            TM = alloc([128, L], BF16); tTM = T()
            memset("dve", PH[0:nr, 0:1], 0.0, (tPH,))
            memset("dve", PH[0:nr, L + 1:L + 2], 0.0, (tPH,))
            for gi, (dst, tdst_, Lg) in enumerate(((X0, tX0, TOWN), (TM, tTM, L), (U, tU, L))):
                r_ = (2 + gi) * D + row0
                Lld = min(L, Lg + 1)
                P.dma("sp", PH[0:nr, 1:Lld + 1], PT_d[r_:r_ + nr, 0:Lld], (tPT,), (tPH,))
                act(dst[0:nr, 0:Lg], PH[0:nr, 0:Lg], AF.Identity, (tPH, tC), (tdst_,), bias=hycb[0:nr, ci_, gi:gi + 1], scale=hycw[0:nr, ci_, gi, 0:1])
                for j in (1, 2):
                    stt(dst[0:nr, 0:Lg], PH[0:nr, j:j + Lg], hycw[0:nr, ci_, gi, j:j + 1], dst[0:nr, 0:Lg], ALU.mult, ALU.add, (tPH, tC), (tdst_,))
            tt("pool", U[0:nr, :], U[0:nr, :], TM[0:nr, :], ALU.mult, (tTM,), (tU,))
            P.barrier()
            apos[0] = mark
            UT = alloc([64, 128, 32], BF16); tUT = (T(), T())
            A = alloc([128, 32, 2, 128], BF16); tA = (T(), T())
            P1 = alloc([128, 128, 32], BF16); P2 = alloc([128, 128, 32], BF16); tP = T()
            KA = alloc([128, 128, 32], BF16); KC = alloc([128, 128, 32], BF16); tKA = T()
            Bv = alloc([128, 32, 2, 128], BF16); tB = (T(), T())

            def st_ka(s):
                sc = (row0 + 32 * s) // 32
                P.dma("sp", KA.rearrange("p a b -> p (a b)"), KAC_d[sc, :, 0, :], (tKAC,), (tKA,))
                P.dma("sp", KC.rearrange("p a b -> p (a b)"), KAC_d[sc, :, 1, :], (tKAC,), (tKA,))

            def st_trs1(s):
                for g in range(4):
                    pb, tb = bank()
                    pbb = pb.bitcast(BF16)
                    for j in range(32):
                        n2 = g * 32 + j
                        tr(pbb[0:64, j * 32:(j + 1) * 32], U[32 * s:32 * s + 32, n2:L:128], identb[32 * s:32 * s + 32, 32 * s:32 * s + 32], (tU, tC), (tb,))
                    cp("act" if g % 2 == 0 else "dve", UT[:, g * 32:(g + 1) * 32, :], pbb[0:64, :].rearrange("p (a b) -> p a b", a=32), (tb,), (tUT[g % 2],))
                fft_fwd(UT, 64, A, tA, F1, tF, tUT, ev)

            def st_s2(s):
                for g in range(8):
                    pb, tb = fft_s2(A, tA, L1, L2, tF, g)
                    pv = pb.rearrange("p (a b) -> p a b", a=16)
                    tt("dve", P1[:, g * 16:(g + 1) * 16, :], pv, KA[:, g * 16:(g + 1) * 16, :], ALU.mult, (tb, tKA), (tP,))
                    tt("dve", P2[:, g * 16:(g + 1) * 16, :], pv, KC[:, g * 16:(g + 1) * 16, :], ALU.mult, (tb, tKA), (tP,))

            def st_s1p(s):
                for c in range(0, 32, 2):
                    pb, tb = bank()
                    for h_ in range(2):
                        mm(pb[:, h_ * 256:(h_ + 1) * 256], P1[:, :, c + h_], R1, True, False, (tP, tF), (tb,))
                        mm(pb[:, h_ * 256:(h_ + 1) * 256], P2[:, :, c + h_], R2, False, True, (tP, tF), (tb,))
                    cp("act" if (ev[0] % 2 == 0) else "dve", Bv[:, c:c + 2, :, :].rearrange("p c a k -> p (c a k)"), pb, (tb,), (tB[ev[0] % 2],))
                    ev[0] += 1

            def st_s2p(s):
                rows = slice(32 * s, 32 * s + 32)
                for g in range(128 // NPB):
                    pb, tb = bank()
                    for j in range(NPB):
                        n2 = g * NPB + j
                        mm(pb[rows, j * NN1:(j + 1) * NN1], Bv[:, :, 0, n2], HR[:, n2, :], True, False, tB + (tF,), (tb,))
                        mm(pb[rows, j * NN1:(j + 1) * NN1], Bv[:, :, 1, n2], HI[:, n2, :], False, True, tB + (tF,), (tb,))
                    cp("act", Ys[rows, g * NPB:(g + 1) * NPB, :].rearrange("p a b -> p (a b)"), pb[rows, :], (tb,), (tYs,))

            ns_ = nr // 32
            st_ka(0); st_trs1(0); st_s2(0)
            for s in range(ns_):
                if s + 1 < ns_:
                    st_ka(s + 1)
                    st_trs1(s + 1)
                st_s1p(s)
                st_s2p(s)
                if s + 1 < ns_:
                    st_s2(s + 1)
            uo = U[0:nr, 0:TOWN]
            stt(uo.rearrange("p (n1 n2) -> p n1 n2", n2=128), uo.rearrange("p (n1 n2) -> p n1 n2", n2=128), hysk[0:nr, ci_:ci_ + 1],
                Ys[0:nr, :, :].rearrange("p n2 n1 -> p n1 n2"), ALU.mult, ALU.add, (tYs, tC, tU), (tU,))
            tt("dve", X0[0:nr, :], X0[0:nr, :], uo, ALU.mult, (tU, tX0), (tX0,))
            P.dma("sp", YHY_d[row0:row0 + nr, :], X0[0:nr, :], (tX0,), (tYHY,))
            P.barrier()

    phaseD2()
    reset_arena(PERSIST)
    if stop_after == "D2":
        return finish(nc, P, out_d, dbg)

    G1B = alloc([128, D], F32); G2B = alloc([128, D], F32); FGB = alloc([128, D], F32)
    P.dma("sp", FGB, fg_d.partition_broadcast(128), (), (tC,))
    dg = alloc([128, 128], F32); tdg = T()
    for (dst, gi) in ((G1B, 2), (G2B, 5)):
        for dc in range(NCH):
            ts("dve", dg, ident, modT[:, gi * 8 + dc, 0:1], None, ALU.mult, None, (tC,), (tdg,))
            pb2, tb2 = bank()
            mm(pb2[:, 0:128], ones, dg, True, True, (tC, tdg), (tb2,))
            cp("act", dst[:, dc * 128:(dc + 1) * 128], pb2[:, 0:128], (tb2,), (tC,))
    PERSIST2 = apos[0]

    def load_w(dram, rows_k, cols):
        w = alloc([128, rows_k, cols], BF16); tw = T()
        for k in range(rows_k):
            for c0 in range(0, cols, 1024):
                wd = min(1024, cols - c0)
                P.dma("pool", w[:, k, c0:c0 + wd], dram[k * 128:(k + 1) * 128, c0:c0 + wd], (), (tw,))
        return w, tw

    def phaseE():
        wrg, twrg = load_w(rgp_d, NCH, D)
        why, twhy = load_w(hyp_d, NCH, D)
        wo, two = load_w(wout_d, NCH, D)
        W = 512
        ins = [[alloc([128, NCH, W], BF16) for _ in range(4)] for _ in range(2)]
        tin = [T(), T()]
        mg = alloc([128, NCH, W], BF16); tmg = T()
        m1 = alloc([128, W], F32); m2 = alloc([128, W], F32); tm = T()
        xts = [alloc([128, D], F32) for _ in range(2)]; txs = [T(), T()]
        t1 = alloc([128, D], F32); tt1 = T()
        xi = 0
        for ti in range(TOWN // W):
            bufs, tb_in = ins[ti % 2], tin[ti % 2]
            tsl = slice(ti * W, (ti + 1) * W)
            for bi, (src, tsrc) in enumerate(((YRG_d, tYRG), (YHY_d, tYHY))):
                P.dma("sp", bufs[bi], src[:, tsl].rearrange("(k p) t -> p k t", p=128), (tsrc,), (tb_in,))
            for bi, g in ((2, 5), (3, 6)):
                P.dma("sp", bufs[bi], PT_d[g * D:(g + 1) * D, tsl].rearrange("(k p) t -> p k t", p=128), (tPT,), (tb_in,))
            for dc in range(NCH):
                pa, ta = bank(); ph, th_ = bank()
                for k in range(NCH):
                    mm(pa, wrg[:, k, dc * 128:(dc + 1) * 128], bufs[0][:, k, :], k == 0, k == NCH - 1, (twrg, tb_in), (ta,))
                for k in range(NCH):
                    mm(ph, why[:, k, dc * 128:(dc + 1) * 128], bufs[1][:, k, :], k == 0, k == NCH - 1, (twhy, tb_in), (th_,))
                tt("dve", m1, pa, bufs[2][:, dc, :], ALU.mult, (ta, tb_in), (tm,))
                tt("dve", m2, ph, bufs[3][:, dc, :], ALU.mult, (th_, tb_in), (tm,))
                tt("pool", mg[:, dc, :], m1, m2, ALU.add, (tm,), (tmg,))
            for a in range(W // 128):
                xt, tx = xts[xi % 2], txs[xi % 2]
                xi += 1
                r0 = ti * W + a * 128
                P.dma("sp", xt, x_d[r0:r0 + 128, :], (), (tx,))
                for half in range(2):
                    pb, tb = bank()
                    for k in range(NCH):
                        mm(pb, mg[:, k, a * 128:(a + 1) * 128], wo[:, k, half * 512:(half + 1) * 512], k == 0, k == NCH - 1, (tmg, two), (tb,))
                    tt("dve", t1[:, half * 512:(half + 1) * 512], pb, G1B[:, half * 512:(half + 1) * 512], ALU.mult, (tb, tC), (tt1,))
                tt("pool", xt, xt, t1, ALU.add, (tt1, tx), (tx,))
                P.dma("sp", X1_d[r0:r0 + 128, :], xt, (tx,), (tX1,))

    phaseE()
    reset_arena(PERSIST2)
    if stop_after == "E":
        return finish(nc, P, out_d, dbg)

    def phaseF():
        NT = TOWN // 128
        BS, NB = MOE_BS, MOE_NB
        PTOT = NB * BS
        XS_d = dscr("XS", [PTOT, D], BF16); YS_d = dscr("YS", [PTOT, D], BF16)
        tXS, tYS = T(), T()
        W1f = mw1_d.rearrange("e k f -> (e k) f"); W3f = mw3_d.rearrange("e k f -> (e k) f"); W2f = mw2_d.rearrange("e f d -> (e f) d")
        wge, twge = load_w(wge_d, NCH, 36)
        tK = T()
        tri = alloc([128, 128], F32); pkw = alloc([128, 8], F32); pkw2 = alloc([128, 4], F32); jbs = alloc([128, NB], F32)
        for dst, nm in ((tri, "tri"), (pkw, "pkw"), (pkw2, "pkw2"), (jbs, "jbs")):
            P.dma("sp", dst, C[nm], (), (tK,))
        A2B = alloc([128, D], F32); B2B = alloc([128, D], F32)
        dg2 = alloc([128, 128], F32); tdg2 = T()
        for (dst, col) in ((A2B, A2), (B2B, B2)):
            for dc in range(NCH):
                ts("dve", dg2, ident, col[:, dc:dc + 1], None, ALU.mult, None, (tC,), (tdg2,))
                pb2, tb2 = bank()
                mm(pb2[:, 0:128], ones, dg2, True, True, (tC, tdg2), (tb2,))
                cp("act", dst[:, dc * 128:(dc + 1) * 128], pb2[:, 0:128], (tb2,), (tK,))
        OH = alloc([128, NT, 64], F32); tOH = T()
        RK = alloc([128, NT, 2], F32); WTS = alloc([128, NT, 2], F32); tRK = T()
        DSTf = alloc([128, NT, 2], F32); DSTi = alloc([128, NT, 2], I32); tDST = T()
        S = alloc([128, 64], F32); tS = T()
        CNT = alloc([128, 64], F32); BASE = alloc([128, 64], F32); PEND = alloc([128, 32], F32); PADD = alloc([128, 32], F32)
        Z32 = alloc([128, 32], F32); tmp64 = alloc([128, 64], F32); ttmp = T()
        EB = alloc([128, NB], F32); EBs = alloc([128, NB], F32)
        IDX1f = alloc([128, NB, 8], F32); IDX1 = alloc([128, NB, 8], I32); IDX2f = alloc([128, NB, 4], F32); IDX2 = alloc([128, NB, 4], I32)
        tIDX = T()
        xts = [alloc([128, D], F32) for _ in range(2)]; txs = [T(), T()]
        junk = alloc([128, D], BF16); tj = T()
        sm = alloc([128, 96], F32); tsm = T()
        ss = alloc([128, 2], F32)
        hxT = alloc([128, NCH, 128], BF16); thx = T()
        tm32 = alloc([128, D], F32); ttm = T()
        mark = apos[0]
        HXTM = alloc([128, NT, D], BF16); tHX = T()
        memset("dve", S, 0.0, (tS,))
        memset("dve", Z32, 0.0, (ttmp,))
        for a in range(NT):
            xt, tx = xts[a % 2], txs[a % 2]
            r0 = a * 128
            P.dma("sp", xt, X1_d[r0:r0 + 128, :], (tX1,), (tx,))
            act(junk, xt, AF.Square, (tx,), (tj, tx), accum=ss[:, 0:1])
            act(ss[:, 0:1], ss[:, 0:1], AF.Sqrt, (tx,), (tx,), scale=1.0 / D, bias=EPS)
            P.op("dve", lambda h: h.reciprocal(out=ss[:, 0:1], in_=ss[:, 0:1]), (tx,), (tx,))
            ts("dve", xt, xt, ss[:, 0:1], None, ALU.mult, None, (tx,), (tx,))
            tt("pool", tm32, xt, A2B, ALU.mult, (tx, tK), (ttm,))
            tt("pool", HXTM[:, a, :], tm32, B2B, ALU.add, (ttm, tK), (tHX,))
            for dc in range(0, NCH, 4):
                pb, tb = bank()
                for j in range(4):
                    tr(pb[:, j * 128:(j + 1) * 128], xt[:, (dc + j) * 128:(dc + j + 1) * 128], ident, (tx, tC), (tb,))
                for j in range(4):
                    act(hxT[:, dc + j, :], pb[:, j * 128:(j + 1) * 128], AF.Identity, (tb, tC), (thx,),
                        bias=B2[:, dc + j:dc + j + 1], scale=A2[:, dc + j:dc + j + 1])
            pb, tb = bank()
            for k in range(NCH):
                mm(pb[:, 0:36], hxT[:, k, :], wge[:, k, :], k == 0, k == NCH - 1, (thx, twge), (tb,))
            lg = sm[:, 0:36]
            tt("dve", lg, pb[:, 0:36], bgeB, ALU.add, (tb, tC), (tsm,))
            gmax = sm[:, 36:37]; sg = sm[:, 37:38]; oh = sm[:, 38:42]; ein = sm[:, 42:50]; m8 = sm[:, 50:58]
            e4 = sm[:, 58:62]; ngm = sm[:, 62:63]; dd = sm[:, 63:64]; mk1 = sm[:, 64:72]; mk2 = sm[:, 72:80]
            P.op("dve", lambda h: h.tensor_reduce(out=gmax, in_=lg[:, 0:4], axis=AX.X, op=ALU.max), (tsm,), (tsm,))
            ts("dve", ngm, gmax, -1.0, None, ALU.mult, None, (tsm,), (tsm,))
            act(e4, lg[:, 0:4], AF.Exp, (tsm,), (tsm,), bias=ngm, accum=sg)
            P.op("dve", lambda h: h.reciprocal(out=sg, in_=sg), (tsm,), (tsm,))
            ts("dve", oh, lg[:, 0:4], gmax, None, ALU.is_equal, None, (tsm,), (tsm,))
            ts("dve", ein, lg[:, 4:12], oh[:, 0:1], None, ALU.mult, None, (tsm,), (tsm,))
            for g in range(1, 4):
                stt(ein, lg[:, 4 + 8 * g:12 + 8 * g], oh[:, g:g + 1], ein, ALU.mult, ALU.add, (tsm,), (tsm,))
            P.op("dve", lambda h: h.max(out=m8, in_=ein), (tsm,), (tsm,))
            tt("dve", dd, m8[:, 1:2], m8[:, 0:1], ALU.subtract, (tsm,), (tsm,))
            act(dd, dd, AF.Exp, (tsm,), (tsm,))
            ts("dve", e4[:, 0:1], dd, 1.0, None, ALU.add, None, (tsm,), (tsm,))
            P.op("dve", lambda h: h.reciprocal(out=e4[:, 0:1], in_=e4[:, 0:1]), (tsm,), (tsm,))
            tt("dve", e4[:, 1:2], dd, e4[:, 0:1], ALU.mult, (tsm,), (tsm,))
            tt("dve", WTS[:, a, 0:1], e4[:, 0:1], sg, ALU.mult, (tsm,), (tRK,))
            tt("dve", WTS[:, a, 1:2], e4[:, 1:2], sg, ALU.mult, (tsm,), (tRK,))
            ts("dve", mk1, ein, m8[:, 0:1], None, ALU.is_equal, None, (tsm,), (tsm,))
            ts("dve", mk2, ein, m8[:, 1:2], None, ALU.is_equal, None, (tsm,), (tsm,))
            for g in range(4):
                ts("dve", OH[:, a, g * 8:(g + 1) * 8], mk1, oh[:, g:g + 1], None, ALU.mult, None, (tsm,), (tOH,))
                ts("dve", OH[:, a, 32 + g * 8:32 + (g + 1) * 8], mk2, oh[:, g:g + 1], None, ALU.mult, None, (tsm,), (tOH,))
            pR, tR = bank()
            mm(pR[:, 0:64], tri, OH[:, a, :], True, False, (tK, tOH), (tR,))
            mm(pR[:, 0:64], ones, S, False, True, (tC, tS), (tR,))
            tt("dve", tmp64, pR[:, 0:64], OH[:, a, :], ALU.mult, (tR, tOH), (ttmp,))
            P.op("dve", lambda h, a=a: h.tensor_reduce(out=RK[:, a, :], in_=tmp64.rearrange("p (a b) -> p a b", a=2), axis=AX.X, op=ALU.add), (ttmp,), (tRK,))
            tt("pool", S, S, OH[:, a, :], ALU.add, (tOH, tS), (tS,))
        pT, tT_ = bank()
        mm(pT[:, 0:64], ones, S, True, True, (tC, tS), (tT_,))
        cp("dve", CNT, pT[:, 0:64], (tT_,), (ttmp,))
        tt("dve", PADD, CNT[:, 0:32], CNT[:, 32:64], ALU.add, (ttmp,), (ttmp,))
        ts("dve", PADD, PADD, 1.0 / BS, (BS - 1.0) / BS - 0.5 + 0.5 / BS, ALU.mult, ALU.add, (ttmp,), (ttmp,))
        cp("dve", DSTi[:, 0:16, :].rearrange("p a b -> p (a b)"), PADD, (ttmp,), (tDST,))
        cp("dve", PADD, DSTi[:, 0:16, :].rearrange("p a b -> p (a b)"), (tDST,), (ttmp,))
        ts("dve", PADD, PADD, float(BS), None, ALU.mult, None, (ttmp,), (ttmp,))
        P.op("dve", lambda h: h.tensor_tensor_scan(out=PEND, data0=PADD, data1=Z32, initial=0.0, op0=ALU.add, op1=ALU.add), (ttmp,), (ttmp,))
        tt("dve", BASE[:, 0:32], PEND, PADD, ALU.subtract, (ttmp,), (ttmp,))
        tt("dve", BASE[:, 32:64], BASE[:, 0:32], CNT[:, 0:32], ALU.add, (ttmp,), (ttmp,))
        for a in range(NT):
            tt("dve", tmp64, OH[:, a, :], BASE, ALU.mult, (tOH, ttmp), (ttmp,))
            P.op("dve", lambda h, a=a: h.tensor_reduce(out=DSTf[:, a, :], in_=tmp64.rearrange("p (a b) -> p a b", a=2), axis=AX.X, op=ALU.add), (ttmp,), (tDST,))
        tt("dve", DSTf.rearrange("p a b -> p (a b)"), DSTf.rearrange("p a b -> p (a b)"), RK.rearrange("p a b -> p (a b)"), ALU.add, (tDST, tRK), (tDST,))
        cp("dve", DSTi.rearrange("p a b -> p (a b)"), DSTf.rearrange("p a b -> p (a b)"), (tDST,), (tDST,))
        memset("dve", EB, 0.0, (tIDX,))
        for e in range(NE):
            stt(EB, jbs, PEND[:, e:e + 1], EB, ALU.is_ge, ALU.add, (tK, ttmp, tIDX), (tIDX,))
        ts("dve", EB, EB, float(NE - 1), None, ALU.min, None, (tIDX,), (tIDX,))
        ts("dve", EBs, EB, 1024.0, None, ALU.mult, None, (tIDX,), (tIDX,))
        for j in range(NB):
            ts("dve", IDX1f[:, j, :], pkw, EBs[:, j:j + 1], None, ALU.add, None, (tK, tIDX), (tIDX,))
        ts("dve", EBs, EB, 512.0, None, ALU.mult, None, (tIDX,), (tIDX,))
        for j in range(NB):
            ts("dve", IDX2f[:, j, :], pkw2, EBs[:, j:j + 1], None, ALU.add, None, (tK, tIDX), (tIDX,))
        cp("dve", IDX1.rearrange("p a b -> p (a b)"), IDX1f.rearrange("p a b -> p (a b)"), (tIDX,), (tIDX,))
        cp("dve", IDX2.rearrange("p a b -> p (a b)"), IDX2f.rearrange("p a b -> p (a b)"), (tIDX,), (tIDX,))
        for a in range(NT):
            for k in range(2):
                P.idma("pool", XS_d, HXTM[:, a, :], DSTi[:, a, k:k + 1], None, (tHX, tDST), (tXS,))
        P.barrier()
        apos[0] = mark
        w1s = [alloc([128, NCH, DE], BF16) for _ in range(2)]
        w3s = [alloc([128, NCH, DE], BF16) for _ in range(2)]
        w2s = [alloc([128, 4, D], BF16) for _ in range(2)]
        tws = [T(), T()]
        wstg = [alloc([128, DE], F32) for _ in range(12)]; twstg = [T() for _ in range(12)]
        wstg2 = [alloc([128, D], F32) for _ in range(4)]; twstg2 = [T() for _ in range(4)]
        wi2 = [0]
        wi = [0]
        mark3 = apos[0]
        NR = 3
        xss = [alloc([128, D], BF16) for _ in range(NR)]; txss = [T() for _ in range(NR)]
        xTs = [alloc([128, NCH, 128], BF16) for _ in range(NR)]; txT = [T() for _ in range(NR)]
        hids = [alloc([128, DE], BF16) for _ in range(NR)]; thid = [T() for _ in range(NR)]
        hTs = [alloc([128, 4, 128], BF16) for _ in range(NR)]; thT = [T() for _ in range(NR)]
        ybs = [alloc([128, D], BF16) for _ in range(NR)]; tyb = [T() for _ in range(NR)]
        s1s = [alloc([128, DE], F32) for _ in range(2)]; ts1s = [T(), T()]

        def wpieces(jb, grp):
            w1, w3, w2, tw = w1s[jb % 2], w3s[jb % 2], w2s[jb % 2], tws[jb % 2]
            pcs = []
            for k in range(NCH):
                pcs.append((w1[:, k, :], W1f, IDX1[:, jb, k:k + 1], False))
                pcs.append((w3[:, k, :], W3f, IDX1[:, jb, k:k + 1], False))
            for k in range(4):
                pcs.append((w2[:, k, :], W2f, IDX2[:, jb, k:k + 1], True))
            for (dst, src, idx, big) in pcs[grp * 5:(grp + 1) * 5]:
                if big:
                    sg_, tsg_ = wstg2[wi2[0] % len(wstg2)], twstg2[wi2[0] % len(wstg2)]
                    wi2[0] += 1
                else:
                    sg_, tsg_ = wstg[wi[0] % len(wstg)], twstg[wi[0] % len(wstg)]
                wi[0] += 1
                P.idma("pool", sg_, src, None, idx, (tIDX,), (tsg_,))
                cp("act" if wi[0] % 2 == 0 else "dve", dst, sg_, (tsg_,), (tw,))

        def stageA(s):
            jb, sb = s // 4, s % 4
            w1, w3, tw = w1s[jb % 2], w3s[jb % 2], tws[jb % 2]
            xs, txs_ = xss[s % NR], txss[s % NR]
            xT, txT_ = xTs[s % NR], txT[s % NR]
            hid, thid_ = hids[s % NR], thid[s % NR]
            s1, ts1 = s1s[s % 2], ts1s[s % 2]
            r0 = jb * BS + sb * 128
            P.dma("sp", xs, XS_d[r0:r0 + 128, :], (tXS,), (txs_,))
            pb, tb = bank()
            pbb = pb.bitcast(BF16)
            for k in range(NCH):
                tr(pbb[:, k * 128:(k + 1) * 128], xs[:, k * 128:(k + 1) * 128], identb, (txs_, tC), (tb,))
            cp("dve", xT.rearrange("p a b -> p (a b)"), pbb, (tb,), (txT_,))
            p1, tp1 = bank(); p3, tp3 = bank()
            for k in range(NCH):
                mm(p1, xT[:, k, :], w1[:, k, :], k == 0, k == NCH - 1, (txT_, tw), (tp1,))
            for k in range(NCH):
                mm(p3, xT[:, k, :], w3[:, k, :], k == 0, k == NCH - 1, (txT_, tw), (tp3,))
            act(s1, p1, AF.Silu, (tp1,), (ts1,))
            tt("dve", hid, s1, p3, ALU.mult, (ts1, tp3), (thid_,))

        def stageB(s):
            jb, sb = s // 4, s % 4
            w2, tw = w2s[jb % 2], tws[jb % 2]
            hid, thid_ = hids[s % NR], thid[s % NR]
            hT, thT_ = hTs[s % NR], thT[s % NR]
            yb, tyb_ = ybs[s % NR], tyb[s % NR]
            r0 = jb * BS + sb * 128
            pb2, tb2 = bank()
            pbb2 = pb2.bitcast(BF16)
            for f in range(4):
                tr(pbb2[:, f * 128:(f + 1) * 128], hid[:, f * 128:(f + 1) * 128], identb, (thid_, tC), (tb2,))
            cp("act", hT.rearrange("p a b -> p (a b)"), pbb2[:, 0:512], (tb2,), (thT_,))
            for half in range(2):
                py, tpy = bank()
                for f in range(4):
                    mm(py, hT[:, f, :], w2[:, f, half * 512:(half + 1) * 512], f == 0, f == 3, (thT_, tw), (tpy,))
                cp("act" if half == 0 else "dve", yb[:, half * 512:(half + 1) * 512], py, (tpy,), (tyb_,))
            P.dma("sp", YS_d[r0:r0 + 128, :], yb, (tyb_,), (tYS,))

        NS = NB * (BS // 128)
        for g in range(4):
            wpieces(0, g)
        for t in range(NS + 1):
            if t < NS:
                stageA(t)
            if t >= 1:
                stageB(t - 1)
            if t < NS:
                jn = t // 4 + 1
                if jn < NB:
                    wpieces(jn, t % 4)
        P.barrier()
        apos[0] = mark3
        y1s = [alloc([128, D], BF16) for _ in range(2)]; y2s = [alloc([128, D], BF16) for _ in range(2)]; tys = [T(), T()]
        mo = alloc([128, D], F32); tmo = T()
        for a in range(NT):
            y1, y2, ty = y1s[a % 2], y2s[a % 2], tys[a % 2]
            xt, tx = xts[a % 2], txs[a % 2]
            r0 = a * 128
            P.idma("pool", y1, YS_d, None, DSTi[:, a, 0:1], (tYS, tDST), (ty,))
            P.idma("pool", y2, YS_d, None, DSTi[:, a, 1:2], (tYS, tDST), (ty,))
            P.dma("sp", xt, X1_d[r0:r0 + 128, :], (tX1,), (tx,))
            ts("dve", mo, y1, WTS[:, a, 0:1], None, ALU.mult, None, (ty, tRK), (tmo,))
            stt(mo, y2, WTS[:, a, 1:2], mo, ALU.mult, ALU.add, (ty, tRK), (tmo,))
            tt("pool", mo, mo, G2B, ALU.mult, (tC,), (tmo,))
            tt("pool", xt, xt, mo, ALU.add, (tmo,), (tx,))
            act(junk, xt, AF.Square, (tx,), (tj, tx), accum=ss[:, 1:2])
            act(ss[:, 1:2], ss[:, 1:2], AF.Sqrt, (tx,), (tx,), scale=1.0 / D, bias=EPS)
            P.op("dve", lambda h: h.reciprocal(out=ss[:, 1:2], in_=ss[:, 1:2]), (tx,), (tx,))
            stt(xt, xt, ss[:, 1:2], FGB, ALU.mult, ALU.mult, (tx, tC), (tx,))
            P.dma("sp", out_d[r0:r0 + 128, :], xt, (tx,), (tX1,))

    phaseF()
    return finish(nc, P, out_d, dbg)


def finish(nc, P, out_d, dbg):
    P.barrier()
    P.emit()
    return nc


def make_inputs(inp, core):
    b, rev = core // 2, (core % 2 == 1)
    f = lambda a: np.ascontiguousarray(np.asarray(a, np.float32))
    m = {}
    xs = np.asarray(inp["x"][b], np.float32)
    cs = np.asarray(inp["ctx"][b], np.float32)
    m["x"] = f(xs[::-1] if rev else xs)
    m["ctx"] = f(cs[::-1] if rev else cs)
    cT = np.stack([fm(inp["c"][b]), fm(inp["c_ctx"])], axis=-1)
    m["cT"] = f(cT)
    m["ada_w"] = f(inp["ada_w"][0])
    m["ada_bT"] = fm(inp["ada_b"][0])
    m["g1nT"] = fm(inp["norm1_g"][0])
    m["g2nT"] = fm(inp["norm2_g"][0])
    m["final_g"] = f(inp["final_g"]).reshape(1, D)
    m["w_in"] = f(inp["w_in"][0])
    rw = np.asarray(inp["rg_conv_w"][0], np.float32)
    zero = np.zeros((D,), np.float32)
    taps5 = [zero, rw[3], rw[2], rw[1], rw[0]] if rev else [rw[0], rw[1], rw[2], rw[3], zero]
    m["rg_cwT"] = f(np.stack([fm(tp) for tp in taps5], axis=-1))
    m["rg_cbT"] = fm(inp["rg_conv_b"][0])
    bd = np.zeros((128, 4, NCH, 128), np.float32)
    gnames = ("rg_wa_b", "rg_wx_b", "rg_wa_f", "rg_wx_f") if rev else ("rg_wa_f", "rg_wx_f", "rg_wa_b", "rg_wx_b")
    bnames = ("rg_ba_b", "rg_bx_b", "rg_ba_f", "rg_bx_f") if rev else ("rg_ba_f", "rg_bx_f", "rg_ba_b", "rg_bx_b")
    lnames = ("rg_lam_b", "rg_lam_f") if rev else ("rg_lam_f", "rg_lam_b")
    for gi, nm in enumerate(gnames):
        w = np.asarray(inp[nm][0], np.float32)
        for cc in range(NCH):
            bd[0:64, gi, cc, 0:64] = w[2 * cc]
            bd[64:128, gi, cc, 64:128] = w[2 * cc + 1]
    m["rg_bd"] = bd
    m["rg_biasT"] = f(np.stack([fm(inp[nm][0]) for nm in bnames], axis=1))
    m["rg_lamT"] = f(np.stack([fm(inp[nm][0]) for nm in lnames], axis=1))
    m["rg_proj"] = f(inp["rg_proj"][0])
    def c96(v):
        o = np.zeros((11 * 96,), np.float32); o[:1024] = np.asarray(v, np.float32)
        return np.ascontiguousarray(o.reshape(11, 96).T)
    hw = np.asarray(inp["hy_conv_w"][0], np.float32); hb = np.asarray(inp["hy_conv_b"][0], np.float32)
    jt = (2, 1, 0) if rev else (0, 1, 2)
    m["hy_cw96"] = f(np.stack([np.stack([c96(hw[j, g * 1024:(g + 1) * 1024]) for j in jt], axis=-1) for g in range(3)], axis=2))
    m["hy_cb96"] = f(np.stack([c96(hb[g * 1024:(g + 1) * 1024]) for g in range(3)], axis=-1))
    m["hy_w1"] = f(inp["hy_pos_w1"][0])
    m["hy_b1T"] = f(inp["hy_pos_b1"][0]).reshape(64, 1)
    m["hy_w2"] = f(inp["hy_pos_w2"][0])
    m["hy_b2T"] = f(inp["hy_pos_b2"][0]).reshape(64, 1)
    m["hy_frT"] = f(inp["hy_freq"][0]).reshape(64, 1)
    w3 = np.asarray(inp["hy_pos_w3"][0], np.float32)
    m["hy_w3"] = f(np.concatenate([w3[:, 1024:], w3[:, :1024]], axis=1) if rev else w3)
    m["hy_w3z"] = f(w3[:, :1024])
    m["hy_sk96"] = c96(inp["hy_skip"][0])
    m["hy_proj"] = f(inp["hy_proj"][0])
    m["w_out"] = f(inp["w_out"][0])
    m["moe_wge"] = f(np.concatenate([inp["moe_wg"][0], inp["moe_we"][0]], axis=1))
    m["moe_bge"] = f(np.concatenate([inp["moe_bg"][0], inp["moe_be"][0]])).reshape(1, 36)
    m["moe_w1"] = f(inp["moe_w1"][0])
    m["moe_w3"] = f(inp["moe_w3"][0])
    m["moe_w2"] = f(inp["moe_w2"][0])
    for nm, arr in host_consts().items():
        m["k_" + nm] = arr
    return m


def kernel(**inputs):
    nc = build()
    in_maps = [make_inputs(inputs, c) for c in range(NCORES)]
    res = run_bass_kernel_spmd(nc, in_maps, core_ids=list(range(NCORES)))
    B = NCORES // 2
    out = np.empty((B, L, D), np.float32)
    for c in range(NCORES):
        r = np.asarray(res.results[c]["out"], np.float32)
        if c % 2 == 0:
            out[c // 2, 0:TOWN] = r
        else:
            out[c // 2, TOWN:L] = r[::-1]
    return out
```

```python
import numpy as np
import ml_dtypes
import concourse.bass as bass
import concourse.mybir as mybir
from concourse.bass_utils import run_bass_kernel_spmd

F32 = mybir.dt.float32
BF16 = mybir.dt.bfloat16
I32 = mybir.dt.int32
ALU = mybir.AluOpType
AF = mybir.ActivationFunctionType
AX = mybir.AxisListType

L = 8192
D = 1024
NCH = 8
NTT = L // 128
CTX = 256
NE = 32
DE = 512
NFFT = 16384
EPS = 1e-6
NCORES = 8
MOE_BS = 512
MOE_NB = 2 * 4096 // MOE_BS + 32
TOWN = 4096


class T:
    __slots__ = ("w", "r")

    def __init__(self):
        self.w = {}
        self.r = {}


class Eng:
    def __init__(self, name, key, sem):
        self.name = name
        self.key = key
        self.sem = sem
        self.count = 0
        self.waited = {}
        self.prog = []
        self.dsems = []
        self.dnext = 0


class Planner:
    def __init__(self, nc, ndma=8):
        self.nc = nc
        self.sems = []
        self.totals = []
        self.E = {}
        for name in ("pe", "act", "dve", "pool", "sp"):
            k = self._newsem(name)
            self.E[name] = Eng(name, k, self.sems[k])
        for q in ("sp", "pool", "act"):
            for i in range({"sp": 16, "pool": 24, "act": 2}[q]):
                self.E[q].dsems.append(self._newsem(f"d_{q}{i}"))

    def _newsem(self, name):
        self.sems.append(self.nc.alloc_semaphore(name=name))
        self.totals.append(0)
        return len(self.sems) - 1

    def _need(self, reads, writes):
        need = {}
        for t in reads:
            for k, v in t.w.items():
                if need.get(k, 0) < v:
                    need[k] = v
        for t in writes:
            for k, v in t.w.items():
                if need.get(k, 0) < v:
                    need[k] = v
            for k, v in t.r.items():
                if need.get(k, 0) < v:
                    need[k] = v
        return need

    def _emit_waits(self, E, need, skip_self=False):
        for k, v in need.items():
            if skip_self and k == E.key:
                continue
            if E.waited.get(k, 0) >= v:
                continue
            E.waited[k] = v
            E.prog.append(("w", k, v))

    def op(self, ename, fn, reads=(), writes=()):
        E = self.E[ename]
        need = self._need(reads, writes)
        self._emit_waits(E, need, skip_self=(ename == "pe"))
        E.count += 1
        E.prog.append(("o", fn, E.key))
        for t in reads:
            t.r[E.key] = E.count
        for t in writes:
            t.w[E.key] = E.count

    def dma(self, q, out, in_, reads=(), writes=()):
        E = self.E[q]
        need = self._need(reads, writes)
        k = E.dsems[E.dnext]
        E.dnext = (E.dnext + 1) % len(E.dsems)
        if self.totals[k] > 0:
            need[k] = max(need.get(k, 0), self.totals[k])
        self._emit_waits(E, need)
        self.totals[k] += 16
        E.prog.append(("d", out, in_, k))
        for t in reads:
            t.r[k] = self.totals[k]
        for t in writes:
            t.w[k] = self.totals[k]

    def idma(self, q, out, in_, out_off, in_off, reads=(), writes=(), bound=None):
        E = self.E[q]
        need = self._need(reads, writes)
        k = E.dsems[E.dnext]
        E.dnext = (E.dnext + 1) % len(E.dsems)
        if self.totals[k] > 0:
            need[k] = max(need.get(k, 0), self.totals[k])
        self._emit_waits(E, need)
        self.totals[k] += 16
        E.prog.append(("i", out, in_, out_off, in_off, k, bound))
        for t in reads:
            t.r[k] = self.totals[k]
        for t in writes:
            t.w[k] = self.totals[k]

    def barrier(self):
        need = {}
        for E in self.E.values():
            if E.count:
                need[E.key] = E.count
            for k in E.dsems:
                if self.totals[k]:
                    need[k] = self.totals[k]
        for E in self.E.values():
            n2 = {k: v for k, v in need.items() if k != E.key}
            self._emit_waits(E, n2)

    def emit(self):
        sems = self.sems

        def replay(E, h):
            regs = {}
            for it in E.prog:
                if it[0] == "w":
                    h.wait_ge(sems[it[1]], it[2])
                elif it[0] == "o":
                    it[1](h).then_inc(sems[it[2]], 1)
                elif it[0] == "i":
                    oo = None if it[3] is None else bass.IndirectOffsetOnAxis(ap=it[3], axis=0)
                    io = None if it[4] is None else bass.IndirectOffsetOnAxis(ap=it[4], axis=0)
                    if it[6] is None:
                        h.indirect_dma_start(out=it[1], out_offset=oo, in_=it[2], in_offset=io).then_inc(sems[it[5]], 16)
                    else:
                        if it[6] not in regs:
                            regs[it[6]] = h.to_reg(it[6])
                        h.indirect_dma_start(out=it[1], out_offset=oo, in_=it[2], in_offset=io, bounds_check=regs[it[6]], oob_is_err=False).then_inc(sems[it[5]], 16)
                else:
                    h.dma_start(out=it[1], in_=it[2]).then_inc(sems[it[3]], 16)

        with self.nc.Block() as block:
            @block.tensor
            def _(h):
                replay(self.E["pe"], h)

            @block.scalar
            def _(h):
                replay(self.E["act"], h)

            @block.vector
            def _(h):
                replay(self.E["dve"], h)

            @block.gpsimd
            def _(h):
                replay(self.E["pool"], h)

            @block.sync
            def _(h):
                replay(self.E["sp"], h)


def _bf(a):
    return np.ascontiguousarray(a.astype(np.float32)).astype(ml_dtypes.bfloat16)


_CONST = None


def host_consts():
    global _CONST
    if _CONST is not None:
        return _CONST
    N = NFFT
    c = {}
    c["ident"] = np.eye(128, dtype=np.float32)
    c["identb"] = _bf(np.eye(128))
    c["ones"] = np.ones((128, 128), np.float32)
    n1 = np.arange(128)[:, None].astype(np.float64)
    k1 = np.arange(128)[None, :].astype(np.float64)
    th = -2 * np.pi * n1 * (2 * k1 + 1) / 256.0
    c["F1"] = _bf(np.concatenate([np.cos(th), np.sin(th)], axis=1))
    n2 = np.arange(128)[:, None, None].astype(np.float64)
    kk1 = np.arange(128)[None, :, None].astype(np.float64)
    kk2 = np.arange(64)[None, None, :].astype(np.float64)
    ph = -2 * np.pi * n2 * (kk1 + 0.5 + 128 * kk2) / N
    Gre, Gim = np.cos(ph), np.sin(ph)
    c["L1"] = _bf(np.concatenate([Gre, Gim], axis=2).reshape(128, 128 * 128))
    c["L2"] = _bf(np.concatenate([-Gim, Gre], axis=2).reshape(128, 128 * 128))
    k2 = np.arange(64)[:, None].astype(np.float64)
    nn2 = np.arange(128)[None, :].astype(np.float64)
    cp = 2 * np.pi * k2 * nn2 / 128.0
    Cre, Cim = np.cos(cp), np.sin(cp)
    c["R1"] = _bf(np.concatenate([np.concatenate([Cre, Cim], 1), np.concatenate([-Cim, Cre], 1)], 0))
    c["R2"] = _bf(np.concatenate([np.concatenate([-Cim, Cre], 1), np.concatenate([Cre, Cim], 1)], 0))
    hk1 = np.arange(128)[:, None, None].astype(np.float64)
    hn2 = np.arange(128)[None, :, None].astype(np.float64)
    hn1 = np.arange(TOWN // 128)[None, None, :].astype(np.float64)
    hp = 2 * np.pi * (hk1 + 0.5) * (128 * hn1 + hn2) / N
    c["HR"] = _bf((2.0 / N) * np.cos(hp).reshape(128, 128 * (TOWN // 128)))
    c["HI"] = _bf(-(2.0 / N) * np.sin(hp).reshape(128, 128 * (TOWN // 128)))
    PA = np.zeros((128, 128)); PC = np.zeros((128, 128))
    for m in range(64):
        PA[m, m] = 1; PA[m, m + 64] = 1
        PC[64 + m, m] = 1; PC[64 + m, 64 + m] = -1
    c["PA"] = _bf(PA); c["PC"] = _bf(PC)
    n = np.arange(N)
    s = np.where(n <= 8192, n, N - n).astype(np.int64)
    s = np.where(n == 8192, 0, s)
    sf = s.astype(np.float32)
    t_norm = (sf / np.float32(L - 1)).astype(np.float32)
    bands = np.linspace(1e-4, 15, 16, dtype=np.float32)
    ang = (np.float32(2.0 * np.pi / L) * sf[:, None] * bands[None, :]).astype(np.float32)
    rows = L // 64
    row_lag = (s // 64).astype(np.float32) / np.float32(rows)
    colb = np.arange(1, 9, dtype=np.float32)
    cang = (np.float32(2.0 * np.pi / 64) * (s % 64).astype(np.float32)[:, None] * colb[None, :]).astype(np.float32)
    feats = np.concatenate([t_norm[:, None], np.cos(ang), np.sin(ang), row_lag[:, None],
                            np.cos(cang), np.sin(cang)], axis=-1).astype(np.float32)
    c["featsT"] = np.ascontiguousarray(feats.T)
    tn = t_norm.copy()
    tn[8192] = 1e4
    c["ntn2"] = np.ascontiguousarray((-tn).reshape(128, 128))
    mxd = np.log(1e-2) / 0.3
    mnd = np.log(1e-2) / 1.5
    deltas = np.abs(np.linspace(mnd, mxd, 1024, dtype=np.float32))
    c["DLT"] = np.ascontiguousarray(np.broadcast_to(deltas[None, :], (128, 1024))).astype(np.float32)
    dec = np.exp(-tn.astype(np.float32)[:, None] * deltas[None, :]).astype(np.float32)
    dec = dec.reshape(128, 128, 32, 32).transpose(2, 0, 1, 3).reshape(32, 128, 4096)
    c["DEC"] = _bf(dec)
    c["tri"] = np.triu(np.ones((128, 128), np.float32), k=1)
    pp = np.arange(128, dtype=np.float32)[:, None]
    c["pkw"] = np.ascontiguousarray(pp + 128.0 * np.arange(8, dtype=np.float32)[None, :])
    c["pkw2"] = np.ascontiguousarray(pp + 128.0 * np.arange(4, dtype=np.float32)[None, :])
    c["jbs"] = np.ascontiguousarray(np.broadcast_to((MOE_BS * np.arange(MOE_NB, dtype=np.float32))[None, :], (128, MOE_NB)))
    _CONST = c
    return c


def fm(v, nch=None):
    v = np.asarray(v, np.float32)
    return np.ascontiguousarray(v.reshape(-1, 128).T)


def build(debug=(), stop_after=None):
    nc = bass.Bass("TRN2", target_bir_lowering=False)
    P = Planner(nc)
    dbg = set(debug)

    def din(name, shape, dt=F32):
        return nc.dram_tensor(name, list(shape), dt, kind="ExternalInput").ap()

    def dscr(name, shape, dt):
        kind = "ExternalOutput" if name in dbg else "Internal"
        return nc.dram_tensor(name, list(shape), dt, kind=kind).ap()

    x_d = din("x", [L, D])
    ctx_d = din("ctx", [CTX, D])
    cT_d = din("cT", [128, NCH, 2])
    adaw_d = din("ada_w", [D, 6 * D])
    adab_d = din("ada_bT", [128, 48])
    g1n_d = din("g1nT", [128, NCH])
    g2n_d = din("g2nT", [128, NCH])
    fg_d = din("final_g", [1, D])
    win_d = din("w_in", [D, 7 * D])
    rgcw_d = din("rg_cwT", [128, NCH, 5])
    rgcb_d = din("rg_cbT", [128, NCH])
    rgbd_d = din("rg_bd", [128, 4, NCH, 128])
    rgbias_d = din("rg_biasT", [128, 4, NCH])
    rglam_d = din("rg_lamT", [128, 2, NCH])
    rgp_d = din("rg_proj", [D, D])
    hycw_d = din("hy_cw96", [96, 11, 3, 3])
    hycb_d = din("hy_cb96", [96, 11, 3])
    hyw1_d = din("hy_w1", [50, 64])
    hyb1_d = din("hy_b1T", [64, 1])
    hyw2_d = din("hy_w2", [64, 64])
    hyb2_d = din("hy_b2T", [64, 1])
    hyfr_d = din("hy_frT", [64, 1])
    hyw3_d = din("hy_w3", [64, 2048])
    hyw3z_d = din("hy_w3z", [64, 1024])
    hysk_d = din("hy_sk96", [96, 11])
    hyp_d = din("hy_proj", [D, D])
    wout_d = din("w_out", [D, D])
    wge_d = din("moe_wge", [D, 36])
    bge_d = din("moe_bge", [1, 36])
    mw1_d = din("moe_w1", [NE, D, DE])
    mw3_d = din("moe_w3", [NE, D, DE])
    mw2_d = din("moe_w2", [NE, DE, D])
    C = {}
    hc = host_consts()
    for nm, arr in hc.items():
        C[nm] = din("k_" + nm, arr.shape, BF16 if arr.dtype == ml_dtypes.bfloat16 else F32)
    out_d = nc.dram_tensor("out", [TOWN, D], F32, kind="ExternalOutput").ap()

    PT_d = dscr("PT", [7 * D, L], BF16)
    YRG_d = dscr("YRG", [D, TOWN], BF16)
    YHY_d = dscr("YHY", [D, TOWN], BF16)
    X1_d = dscr("X1", [TOWN, D], F32)
    KAC_d = dscr("KAC", [32, 128, 2, 4096], BF16)
    tPT, tYRG, tYHY, tX1, tKAC = T(), T(), T(), T(), T()

    banks = []
    for i in range(8):
        banks.append((nc.alloc_psum_tensor(f"ps{i}", [128, 512], F32)[:, :], T()))
    bstate = [0]

    def bank():
        b = banks[bstate[0] % 8]
        bstate[0] += 1
        return b

    ARENA = 196608 - 2048
    arena = nc.alloc_sbuf_tensor("arena", [128, ARENA // 2], BF16)
    apos = [0]

    def alloc(shape, dt):
        esz = 4 if dt in (F32, I32) else 2
        n = int(np.prod(shape[1:]))
        nbytes = (n * esz + 63) // 64 * 64
        off = apos[0]
        apos[0] += nbytes
        assert apos[0] <= ARENA, f"SBUF overflow {apos[0]}"
        ap = arena[0:shape[0], off // 2: off // 2 + n * esz // 2]
        if esz == 4:
            ap = ap.bitcast(dt)
        if len(shape) > 2:
            names = " ".join(f"a{i}" for i in range(len(shape) - 1))
            kw = {f"a{i}": int(shape[i + 1]) for i in range(len(shape) - 1)}
            ap = ap.rearrange(f"p ({names}) -> p {names}", **kw)
        return ap

    def reset_arena(to=0):
        P.barrier()
        apos[0] = to

    def act(out, in_, func, r, w, bias=None, scale=None, accum=None, eng="act"):
        kw = {}
        if bias is not None:
            kw["bias"] = bias
        if scale is not None:
            kw["scale"] = scale
        if accum is not None:
            kw["accum_out"] = accum
        P.op("act", lambda h: h.activation(out=out, in_=in_, func=func, **kw), r, w)

    def ts(eng, out, in0, s1, s2, op0, op1, r, w):
        if op1 is None:
            P.op(eng, lambda h: h.tensor_scalar(out=out, in0=in0, scalar1=s1, scalar2=None, op0=op0), r, w)
        else:
            P.op(eng, lambda h: h.tensor_scalar(out=out, in0=in0, scalar1=s1, scalar2=s2, op0=op0, op1=op1), r, w)

    def stt(out, in0, sc, in1, op0, op1, r, w):
        P.op("dve", lambda h: h.scalar_tensor_tensor(out=out, in0=in0, scalar=sc, in1=in1, op0=op0, op1=op1), r, w)

    def tt(eng, out, in0, in1, op, r, w):
        P.op(eng, lambda h: h.tensor_tensor(out=out, in0=in0, in1=in1, op=op), r, w)

    def cp(eng, out, in_, r, w):
        if eng == "act":
            P.op("act", lambda h: h.activation(out=out, in_=in_, func=AF.Copy), r, w)
        else:
            P.op(eng, lambda h: h.tensor_copy(out=out, in_=in_), r, w)

    def mm(out, lhsT, rhs, start, stop, r, w):
        P.op("pe", lambda h: h.matmul(out, lhsT, rhs, start=start, stop=stop), r, w)

    def tr(out, in_, ident, r, w):
        P.op("pe", lambda h: h.transpose(out, in_, ident), r, w)

    def memset(eng, ap, val, w):
        P.op(eng, lambda h: h.memset(ap, val), (), w)

    tC = T()
    ident = alloc([128, 128], F32)
    identb = alloc([128, 128], BF16)
    ones = alloc([128, 128], F32)
    modT = alloc([128, 48, 2], F32)
    A1 = alloc([128, NCH], F32); B1 = alloc([128, NCH], F32)
    A1c = alloc([128, NCH], F32); B1c = alloc([128, NCH], F32)
    A2 = alloc([128, NCH], F32); B2 = alloc([128, NCH], F32)
    pass
    rgcw = alloc([128, NCH, 5], F32); rgcb = alloc([128, NCH], F32)
    rgbias = alloc([128, 4, NCH], F32); nrgbias = alloc([128, 4, NCH], F32)
    rglam = alloc([128, 2, NCH], F32)
    sa1 = alloc([128, 2, NCH], F32); sa2 = alloc([128, 2, NCH], F32)
    hycw = alloc([96, 11, 3, 3], F32); hycb = alloc([96, 11, 3], F32); hysk = alloc([96, 11], F32)
    H0 = alloc([128, 2, NCH], F32)
    bgeB = alloc([128, 36], F32)
    PERSIST = apos[0]

    P.dma("sp", ident, C["ident"], (), (tC,))
    P.dma("sp", identb, C["identb"], (), (tC,))
    P.dma("sp", ones, C["ones"], (), (tC,))
    for dst, src in ((rgcw, rgcw_d), (rgcb, rgcb_d), (rgbias, rgbias_d), (rglam, rglam_d),
                     (hycw, hycw_d), (hycb, hycb_d), (hysk, hysk_d)):
        P.dma("sp", dst, src, (), (tC,))
    P.dma("sp", bgeB, bge_d.partition_broadcast(128), (), (tC,))

    def phaseA():
        cT = alloc([128, NCH, 2], F32)
        scT = alloc([128, NCH, 2], F32)
        adab = alloc([128, 48], F32)
        g1n = alloc([128, NCH], F32); g2n = alloc([128, NCH], F32)
        tmp = alloc([128, NCH], F32)
        dg = alloc([128, 128], F32)
        wbuf = [alloc([128, 1536], F32) for _ in range(3)]
        tw = [T() for _ in range(3)]
        tS = T()
        P.dma("sp", cT, cT_d, (), (tS,))
        P.dma("sp", adab, adab_d, (), (tS,))
        P.dma("sp", g1n, g1n_d, (), (tS,))
        P.dma("sp", g2n, g2n_d, (), (tS,))
        act(scT, cT, AF.Silu, (tS,), (tS,))
        pb, tb = bank()
        i = 0
        for k in range(NCH):
            for q in range(4):
                wb, twb = wbuf[i % 3], tw[i % 3]
                i += 1
                P.dma("sp", wb, adaw_d[k * 128:(k + 1) * 128, q * 1536:(q + 1) * 1536], (), (twb,))
                for jj in range(12):
                    j = q * 12 + jj
                    mm(pb[:, 2 * j:2 * j + 2], wb[:, jj * 128:(jj + 1) * 128], scT[:, k, :],
                       (k == 0 and j == 0), (k == NCH - 1 and j == 47), (twb, tS), (tb,))
        for col in range(2):
            tt("dve", modT[:, :, col], pb[:, col:96:2], adab, ALU.add, (tb, tS), (tC,))
        for (Ad, Bd, gn, sci, shi, col) in ((A1, B1, g1n, 1, 0, 0), (A1c, B1c, g1n, 1, 0, 1), (A2, B2, g2n, 4, 3, 0)):
            ts("dve", tmp, modT[:, sci * 8:(sci + 1) * 8, col], 1.0, None, ALU.add, None, (tC,), (tS,))
            tt("dve", Ad, tmp, gn, ALU.mult, (tS,), (tC,))
            cp("dve", Bd, modT[:, shi * 8:(shi + 1) * 8, col], (tC,), (tC,))
        act(sa1, rglam, AF.Exp, (tC,), (tC,), scale=-1.0)
        act(sa1, sa1, AF.Ln, (tC,), (tC,), bias=1.0)
        ts("dve", sa2, sa1, -16.0, None, ALU.mult, None, (tC,), (tC,))
        ts("dve", sa1, sa1, -8.0, None, ALU.mult, None, (tC,), (tC,))
        ts("dve", nrgbias, rgbias, -1.0, None, ALU.mult, None, (tC,), (tC,))

    phaseA()
    reset_arena(PERSIST)

    def phaseB(src_d, ntok, W, ccs_all, ccs_fn, Asc, Bsc, dst_d, tdst, sig_from=40):
        nsub = W // 128
        ncol = len(ccs_all)
        pos = {cc: i for i, cc in enumerate(ccs_all)}
        winb = alloc([128, NCH, ncol * 128], BF16)
        tWs = [T() for _ in range((ncol + 7) // 8)]
        c0 = ccs_all[0] * 128
        for blk in range(0, ncol * 128, 1024):
            wd = min(1024, ncol * 128 - blk)
            for k in range(NCH):
                P.dma("pool", winb[:, k, blk:blk + wd], win_d[k * 128:(k + 1) * 128, c0 + blk:c0 + blk + wd], (), (tWs[blk // 1024],))
        xts = [alloc([128, nsub, D], F32) for _ in range(2)]
        txs = [T(), T()]
        junk = alloc([128, D], BF16); tj = T()
        ss = [alloc([128, nsub], F32) for _ in range(2)]
        hxs = [alloc([128, NCH, W], BF16) for _ in range(2)]
        ths = [T(), T()]
        stg = [alloc([128, 4, W], BF16) for _ in range(3)]
        tst = [(T(), T()) for _ in range(3)]
        si = 0
        for ti in range(ntok // W):
            ccs = ccs_fn(ti)
            xt, tx, s_, hx, th = xts[ti % 2], txs[ti % 2], ss[ti % 2], hxs[ti % 2], ths[ti % 2]
            P.dma("sp", xt, src_d[ti * W:(ti + 1) * W, :].rearrange("(a p) d -> p a d", p=128), (), (tx,))
            for a in range(nsub):
                act(junk, xt[:, a, :], AF.Square, (tx,), (tj, tx), accum=s_[:, a:a + 1])
            act(s_, s_, AF.Sqrt, (tx,), (tx,), scale=1.0 / D, bias=EPS)
            P.op("dve", lambda h, s_=s_: h.reciprocal(out=s_, in_=s_), (tx,), (tx,))
            for a in range(nsub):
                ts("dve" if a % 2 == 0 else "pool", xt[:, a, :], xt[:, a, :], s_[:, a:a + 1], None, ALU.mult, None, (tx,), (tx,))
            for dc in range(NCH):
                pb, tb = bank()
                for a in range(nsub):
                    tr(pb[:, a * 128:(a + 1) * 128], xt[:, a, dc * 128:(dc + 1) * 128], ident, (tx, tC), (tb,))
                act(hx[:, dc, :], pb[:, 0:W], AF.Identity, (tb, tC), (th,), bias=Bsc[:, dc:dc + 1], scale=Asc[:, dc:dc + 1])
            for ci, cc in enumerate(ccs):
                pb, tb = bank()
                wi = pos[cc]
                for k in range(NCH):
                    mm(pb[:, 0:W], winb[:, k, wi * 128:(wi + 1) * 128], hx[:, k, :], k == 0, k == NCH - 1, (tWs[wi // 8], th), (tb,))
                sg, tsg = stg[si % 3], tst[si % 3]
                if cc >= sig_from:
                    act(sg[:, ci % 4, :], pb[:, 0:W], AF.Sigmoid, (tb,), (tsg[0],))
                elif ci % 2 == 0:
                    cp("act", sg[:, ci % 4, :], pb[:, 0:W], (tb,), (tsg[0],))
                else:
                    cp("dve", sg[:, ci % 4, :], pb[:, 0:W], (tb,), (tsg[1],))
                if ci % 4 == 3:
                    r0 = ccs[ci - 3] * 128
                    P.dma("sp", dst_d[r0:r0 + 512, ti * W:(ti + 1) * W].rearrange("(a p) t -> p a t", p=128), sg, tsg, (tdst,))
                    si += 1

    NOWN5 = TOWN // 512
    CC_ALL = list(range(56))
    CC_REST = list(range(0, 8)) + list(range(24, 40))
    CC_HALO = CC_REST + list(range(16, 24))

    def ccs_main(ti):
        if ti < NOWN5:
            return CC_ALL
        if ti == NOWN5:
            return CC_HALO
        return CC_REST

    phaseB(x_d, L, 512, CC_ALL, ccs_main, A1, B1, PT_d, tPT)
    reset_arena(PERSIST)
    if stop_after == "B":
        return finish(nc, P, out_d, dbg)

    PTC_d = dscr("PTC", [D, CTX], BF16)
    tPTC = T()
    phaseB(ctx_d, CTX, 256, list(range(8)), lambda ti: list(range(8)), A1c, B1c, PTC_d, tPTC)
    reset_arena(PERSIST)

    def phaseC(src_d, tsrc, Lt, Lown, TW, is_ctx):
        ntile = Lt // TW
        nown = Lown // TW
        nq = TW // 512 if TW >= 512 else 1
        QW = min(512, TW)
        wbd = alloc([128, 4, NCH, 128], BF16); tWb = T()
        P.dma("pool", wbd.rearrange("p a b c -> p (a b c)"), rgbd_d.rearrange("p a b c -> p (a b c)"), (), (tWb,))
        PX = alloc([128, Lt + 4], BF16); tPX = T()
        xc = alloc([128, Lt], BF16); txc = T()
        if not is_ctx:
            PRG = alloc([128, Lown], BF16); tPRG = T()
            HB = alloc([128, Lown], BF16); tHB = T()
            g1 = alloc([128, TW], F32); g2 = alloc([128, TW], F32); tg = T()
            yo = [alloc([128, TW], BF16) for _ in range(2)]; tyo = [T(), T()]
        NG = 2
        rts = [alloc([128, TW], F32) for _ in range(NG)]
        ats = [alloc([128, TW], F32) for _ in range(NG)]
        its = [alloc([128, TW], F32) for _ in range(NG)]
        tgts = [T() for _ in range(NG)]
        hts = [alloc([128, TW], F32) for _ in range(2)]; tht = [T(), T()]
        memset("dve", PX[:, 0:2], 0.0, (tPX,))
        memset("dve", PX[:, Lt + 2:Lt + 4], 0.0, (tPX,))
        hcount = 0
        yi = 0
        gi_ = 0
        for cc in range(NCH):
            P.dma("sp", PX[:, 2:Lt + 2], src_d[cc * 128:(cc + 1) * 128, 0:Lt], (tsrc,), (tPX,))
            if not is_ctx:
                P.dma("sp", PRG, src_d[D + cc * 128:D + (cc + 1) * 128, 0:Lown], (tsrc,), (tPRG,))
            act(xc, PX[:, 0:Lt], AF.Identity, (tPX, tC), (txc,), bias=rgcb[:, cc:cc + 1], scale=rgcw[:, cc, 0:1])
            for j in range(1, 5):
                stt(xc, PX[:, j:j + Lt], rgcw[:, cc, j:j + 1], xc, ALU.mult, ALU.add, (tPX, tC), (txc,))
            for d in (1, 0):
                order = range(ntile - 1, -1, -1) if d == 1 else range(nown)
                first = True
                for ti in order:
                    sl = slice(ti * TW, (ti + 1) * TW)
                    rt, at, it, tgt = rts[gi_ % NG], ats[gi_ % NG], its[gi_ % NG], tgts[gi_ % NG]
                    gi_ += 1
                    prs, pis = [], []
                    for q in range(nq):
                        pr, tr_ = bank(); pi, ti_ = bank()
                        mm(pr[:, 0:QW], wbd[:, 2 * d, cc, :], xc[:, ti * TW + q * QW: ti * TW + (q + 1) * QW], True, True, (tWb, txc), (tr_,))
                        mm(pi[:, 0:QW], wbd[:, 2 * d + 1, cc, :], xc[:, ti * TW + q * QW: ti * TW + (q + 1) * QW], True, True, (tWb, txc), (ti_,))
                        prs.append((pr, tr_)); pis.append((pi, ti_))
                    for q in range(nq):
                        act(rt[:, q * QW:(q + 1) * QW], prs[q][0][:, 0:QW], AF.Sigmoid, (prs[q][1], tC), (tgt,), bias=rgbias[:, 2 * d, cc:cc + 1])
                    for q in range(nq):
                        act(it[:, q * QW:(q + 1) * QW], pis[q][0][:, 0:QW], AF.Sigmoid, (pis[q][1], tC), (tgt,), bias=rgbias[:, 2 * d + 1, cc:cc + 1])
                    act(at, rt, AF.Exp, (tgt, tC), (tgt,), scale=sa1[:, d, cc:cc + 1])
                    act(rt, rt, AF.Exp, (tgt, tC), (tgt,), scale=sa2[:, d, cc:cc + 1])
                    act(rt, rt, AF.Sqrt, (tgt,), (tgt,), scale=-1.0, bias=1.0)
                    tt("pool", it, it, xc[:, sl], ALU.mult, (tgt, txc), (tgt,))
                    tt("dve", rt, rt, it, ALU.mult, (tgt,), (tgt,))
                    ht, th_ = hts[hcount % 2], tht[hcount % 2]
                    hp, thp = hts[(hcount + 1) % 2], tht[(hcount + 1) % 2]
                    hcount += 1
                    if first:
                        init = 0.0 if is_ctx else H0[:, d, cc:cc + 1]
                        rd = (tgt,) if is_ctx else (tgt, tC)
                    else:
                        init = hp[:, 0:1] if d == 1 else hp[:, TW - 1:TW]
                        rd = (tgt, thp)
                    first = False
                    if d == 1:
                        P.op("dve", lambda h, ht=ht, init=init, at=at, rt=rt: h.tensor_tensor_scan(out=ht[:, ::-1], data0=at[:, ::-1], data1=rt[:, ::-1], initial=init, op0=ALU.mult, op1=ALU.add), rd, (th_,))
                    else:
                        P.op("dve", lambda h, ht=ht, init=init, at=at, rt=rt: h.tensor_tensor_scan(out=ht, data0=at, data1=rt, initial=init, op0=ALU.mult, op1=ALU.add), rd, (th_,))
                    if is_ctx:
                        last = (ti == 0) if d == 1 else (ti == ntile - 1)
                        if last:
                            col = ht[:, 0:1] if d == 1 else ht[:, TW - 1:TW]
                            cp("dve", H0[:, d, cc:cc + 1], col, (th_,), (tC,))
                        continue
                    if d == 1:
                        if ti < nown:
                            cp("act", HB[:, sl], ht, (th_,), (tHB,))
                    else:
                        xg = PRG[:, sl]
                        tt("pool", g1, xg, xg, ALU.mult, (tPRG,), (tg,))
                        ts("pool", g1, g1, 0.044715, 1.0, ALU.mult, ALU.add, (tg,), (tg,))
                        tt("pool", g1, g1, xg, ALU.mult, (tg, tPRG), (tg,))
                        act(g1, g1, AF.Sigmoid, (tg,), (tg,), scale=1.5957691216057308)
                        tt("pool", g1, g1, xg, ALU.mult, (tg, tPRG), (tg,))
                        tt("dve", g2, ht, HB[:, sl], ALU.add, (th_, tHB), (tg,))
                        y, ty = yo[yi % 2], tyo[yi % 2]
                        yi += 1
                        tt("dve", y, g2, g1, ALU.mult, (tg,), (ty,))
                        P.dma("sp", YRG_d[cc * 128:(cc + 1) * 128, sl], y, (ty,), (tYRG,))

    phaseC(PTC_d, tPTC, CTX, CTX, 256, True)
    reset_arena(PERSIST)
    phaseC(PT_d, tPT, L, TOWN, 2048, False)
    reset_arena(PERSIST)
    if stop_after == "C":
        return finish(nc, P, out_d, dbg)

    def load_fft_tables():
        tF = T()
        L1 = alloc([128, 16384], BF16); L2 = alloc([128, 16384], BF16)
        for q in range(4):
            P.dma("sp", L1[:, q * 4096:(q + 1) * 4096], C["L1"][:, q * 4096:(q + 1) * 4096], (), (tF,))
            P.dma("sp", L2[:, q * 4096:(q + 1) * 4096], C["L2"][:, q * 4096:(q + 1) * 4096], (), (tF,))
        F1 = alloc([128, 256], BF16)
        P.dma("sp", F1, C["F1"], (), (tF,))
        return tF, L1, L2, F1

    def fft_fwd(src, Krows, A, tA, F1, tF, tsrc, ev):
        for c in range(0, 32, 2):
            pb, tb = bank()
            for h_ in range(2):
                mm(pb[:, h_ * 256:(h_ + 1) * 256], src[0:Krows, :, c + h_], F1[0:Krows, :], True, True, tuple(tsrc) + (tF,), (tb,))
            cp("act" if (ev[0] % 2 == 0) else "dve", A[:, c:c + 2, :, :].rearrange("p c a k -> p (c a k)"), pb, (tb,), (tA[ev[0] % 2],))
            ev[0] += 1

    def fft_s2(A, tA, L1, L2, tF, g):
        pb, tb = bank()
        for j in range(16):
            k1 = g * 16 + j
            mm(pb[:, j * 32:(j + 1) * 32], L1[:, k1 * 128:(k1 + 1) * 128], A[:, :, 0, k1], True, False, (tF,) + tuple(tA), (tb,))
            mm(pb[:, j * 32:(j + 1) * 32], L2[:, k1 * 128:(k1 + 1) * 128], A[:, :, 1, k1], False, True, (tF,) + tuple(tA), (tb,))
        return pb, tb

    def phaseD1():
        tF, L1, L2, F1 = load_fft_tables()
        PA = alloc([128, 128], BF16); PC = alloc([128, 128], BF16)
        P.dma("sp", PA, C["PA"], (), (tF,)); P.dma("sp", PC, C["PC"], (), (tF,))
        z2T = alloc([64, NFFT], BF16); tz = T()
        w3b = alloc([64, 2048], BF16); w3z = alloc([64, 1024], BF16)
        DLT = alloc([128, 1024], F32); ntn2 = alloc([128, 128], F32)
        P.dma("sp", DLT, C["DLT"], (), (tF,)); P.dma("sp", ntn2, C["ntn2"], (), (tF,))
        P.dma("pool", w3b, hyw3_d, (), (tF,))
        P.dma("pool", w3z, hyw3z_d, (), (tF,))
        ts("dve", w3b[:, 1024:2048], w3b[:, 1024:2048], -1.0, None, ALU.mult, None, (tF,), (tF,))
        mark = apos[0]
        w1 = alloc([50, 64], F32); w2 = alloc([64, 64], F32)
        b1 = alloc([64, 1], F32); b2 = alloc([64, 1], F32); fr = alloc([64, 1], F32)
        s1c = alloc([64, 1], F32); o1c = alloc([64, 1], F32); o2c = alloc([64, 1], F32)
        tM = T()
        for dst, src in ((w1, hyw1_d), (w2, hyw2_d), (b1, hyb1_d), (b2, hyb2_d), (fr, hyfr_d)):
            P.dma("sp", dst, src, (), (tM,))
        TWO_PI = 2.0 * np.pi
        ts("dve", s1c, fr, 1.0 / TWO_PI, None, ALU.mult, None, (tM,), (tM,))
        tt("dve", o1c, s1c, b1, ALU.mult, (tM,), (tM,))
        ts("dve", o1c, o1c, 8.0, None, ALU.add, None, (tM,), (tM,))
        tt("dve", o2c, s1c, b2, ALU.mult, (tM,), (tM,))
        ts("dve", o2c, o2c, 8.0, None, ALU.add, None, (tM,), (tM,))
        fts = [alloc([50, 512], F32) for _ in range(2)]; tft = [T(), T()]
        q_ = alloc([64, 512], F32); qi = alloc([64, 512], I32); qf = alloc([64, 512], F32); z1 = alloc([64, 512], F32)
        tq = T()

        def sin_layer(pb, tb, oc, out, tout):
            ts("dve", q_, pb[0:64, :], s1c, oc, ALU.mult, ALU.add, (tb, tM), (tq,))
            cp("dve", qi, q_, (tq,), (tq,))
            cp("dve", qf, qi, (tq,), (tq,))
            tt("dve", q_, q_, qf, ALU.subtract, (tq,), (tq,))
            act(out, q_, AF.Sin, (tq,), (tout,), scale=TWO_PI)

        for i in range(NFFT // 512):
            ft, tf_ = fts[i % 2], tft[i % 2]
            P.dma("sp", ft, C["featsT"][:, i * 512:(i + 1) * 512], (), (tf_,))
            pb, tb = bank()
            mm(pb[0:64, :], w1, ft, True, True, (tM, tf_), (tb,))
            sin_layer(pb, tb, o1c, z1, tq)
            pb2, tb2 = bank()
            mm(pb2[0:64, :], w2, z1, True, True, (tM, tq), (tb2,))
            sin_layer(pb2, tb2, o2c, z2T[:, i * 512:(i + 1) * 512], tz)
        P.barrier()
        apos[0] = mark
        kT = alloc([128, 128, 32], BF16); tk = T()
        A = alloc([128, 32, 2, 128], BF16); tA = (T(), T())
        Kpk = alloc([128, 4096], BF16); tK = T()
        KA = alloc([128, 4096], BF16); KC = alloc([128, 4096], BF16); tKA = (T(), T())
        decs = [alloc([128, 4096], BF16) for _ in range(2)]; tdec = [T(), T()]
        ev = [0]
        for sc in range(32):
            c0 = sc * 32
            dec, tdec_ = decs[sc % 2], tdec[sc % 2]
            P.dma("sp", dec, C["DEC"][sc], (), (tdec_,))
            for g in range(8):
                pb, tb = bank()
                for j in range(16):
                    n2 = g * 16 + j
                    mm(pb[0:64, j * 32:(j + 1) * 32], z2T[:, n2:8192:128], w3b[:, c0:c0 + 32], True, True, (tz, tF), (tb,))
                    mm(pb[64:128, j * 32:(j + 1) * 32], z2T[:, 8192 + n2:NFFT:128], w3b[:, 1024 + c0:1024 + c0 + 32], True, True, (tz, tF), (tb,))
                tt("dve", kT[:, g * 16:(g + 1) * 16, :].rearrange("p a b -> p (a b)"), pb, dec[:, g * 512:(g + 1) * 512], ALU.mult, (tb, tdec_), (tk,))
            pbz, tbz = bank()
            mm(pbz[0:1, 0:32], z2T[:, 0:1], w3z[:, c0:c0 + 32], True, True, (tz, tF), (tbz,))
            cp("dve", kT[0:1, 0, :], pbz[0:1, 0:32], (tbz,), (tk,))
            fft_fwd(kT, 128, A, tA, F1, tF, (tk,), ev)
            for g in range(8):
                pb, tb = fft_s2(A, tA, L1, L2, tF, g)
                cp("act", Kpk[:, g * 512:(g + 1) * 512], pb, (tb,), (tK,))
            for g in range(8):
                pa, ta = bank(); pc, tc_ = bank()
                mm(pa, PA, Kpk[:, g * 512:(g + 1) * 512], True, True, (tF, tK), (ta,))
                mm(pc, PC, Kpk[:, g * 512:(g + 1) * 512], True, True, (tF, tK), (tc_,))
                cp("act", KA[:, g * 512:(g + 1) * 512], pa, (ta,), (tKA[0],))
                cp("dve", KC[:, g * 512:(g + 1) * 512], pc, (tc_,), (tKA[1],))
            P.dma("sp", KAC_d[sc, :, 0, :], KA, (tKA[0],), (tKAC,))
            P.dma("sp", KAC_d[sc, :, 1, :], KC, (tKA[1],), (tKAC,))

    phaseD1()
    reset_arena(PERSIST)
    if stop_after == "D1":
        return finish(nc, P, out_d, dbg)

    def phaseD2():
        NN1 = TOWN // 128
        tF, L1, L2, F1 = load_fft_tables()
        R1 = alloc([128, 256], BF16); R2 = alloc([128, 256], BF16)
        P.dma("sp", R1, C["R1"], (), (tF,)); P.dma("sp", R2, C["R2"], (), (tF,))
        HR = alloc([128, 128, NN1], BF16); HI = alloc([128, 128, NN1], BF16)
        P.dma("sp", HR.rearrange("p a b -> p (a b)"), C["HR"], (), (tF,))
        P.dma("sp", HI.rearrange("p a b -> p (a b)"), C["HI"], (), (tF,))
        U = alloc([128, L], BF16); tU = T()
        X0 = alloc([128, TOWN], BF16); tX0 = T()
        Ys = alloc([128, 128, NN1], BF16); tYs = T()
        mark = apos[0]
        ev = [0]
        NPB = 512 // NN1
        chunks = [(96 * i, 96) for i in range(10)] + [(960, 64)]
        for ci_, (row0, nr) in enumerate(chunks):
            apos[0] = mark
            PH = alloc([128, L + 2], BF16); tPH = T()
            TM = alloc([128, L], BF16); tTM = T()
            memset("dve", PH[0:nr, 0:1], 0.0, (tPH,))
            memset("dve", PH[0:nr, L + 1:L + 2], 0.0, (tPH,))
            for gi, (dst, tdst_, Lg) in enumerate(((X0, tX0, TOWN), (TM, tTM, L), (U, tU, L))):
                r_ = (2 + gi) * D + row0
                Lld = min(L, Lg + 1)
                P.dma("sp", PH[0:nr, 1:Lld + 1], PT_d[r_:r_ + nr, 0:Lld], (tPT,), (tPH,))
                act(dst[0:nr, 0:Lg], PH[0:nr, 0:Lg], AF.Identity, (tPH, tC), (tdst_,), bias=hycb[0:nr, ci_, gi:gi + 1], scale=hycw[0:nr, ci_, gi, 0:1])
                for j in (1, 2):
                    stt(dst[0:nr, 0:Lg], PH[0:nr, j:j + Lg], hycw[0:nr, ci_, gi, j:j + 1], dst[0:nr, 0:Lg], ALU.mult, ALU.add, (tPH, tC), (tdst_,))
            tt("pool", U[0:nr, :], U[0:nr, :], TM[0:nr, :], ALU.mult, (tTM,), (tU,))
            P.barrier()
            apos[0] = mark
            UT = alloc([64, 128, 32], BF16); tUT = (T(), T())
            A = alloc([128, 32, 2, 128], BF16); tA = (T(), T())
            P1 = alloc([128, 128, 32], BF16); P2 = alloc([128, 128, 32], BF16); tP = T()
            KA = alloc([128, 128, 32], BF16); KC = alloc([128, 128, 32], BF16); tKA = T()
            Bv = alloc([128, 32, 2, 128], BF16); tB = (T(), T())

            def st_ka(s):
                sc = (row0 + 32 * s) // 32
                P.dma("sp", KA.rearrange("p a b -> p (a b)"), KAC_d[sc, :, 0, :], (tKAC,), (tKA,))
                P.dma("sp", KC.rearrange("p a b -> p (a b)"), KAC_d[sc, :, 1, :], (tKAC,), (tKA,))

            def st_trs1(s):
                for g in range(4):
                    pb, tb = bank()
                    pbb = pb.bitcast(BF16)
                    for j in range(32):
                        n2 = g * 32 + j
                        tr(pbb[0:64, j * 32:(j + 1) * 32], U[32 * s:32 * s + 32, n2:L:128], identb[32 * s:32 * s + 32, 32 * s:32 * s + 32], (tU, tC), (tb,))
                    cp("act" if g % 2 == 0 else "dve", UT[:, g * 32:(g + 1) * 32, :], pbb[0:64, :].rearrange("p (a b) -> p a b", a=32), (tb,), (tUT[g % 2],))
                fft_fwd(UT, 64, A, tA, F1, tF, tUT, ev)

            def st_s2(s):
                for g in range(8):
                    pb, tb = fft_s2(A, tA, L1, L2, tF, g)
                    pv = pb.rearrange("p (a b) -> p a b", a=16)
                    tt("dve", P1[:, g * 16:(g + 1) * 16, :], pv, KA[:, g * 16:(g + 1) * 16, :], ALU.mult, (tb, tKA), (tP,))
                    tt("dve", P2[:, g * 16:(g + 1) * 16, :], pv, KC[:, g * 16:(g + 1) * 16, :], ALU.mult, (tb, tKA), (tP,))

            def st_s1p(s):
                for c in range(0, 32, 2):
                    pb, tb = bank()
                    for h_ in range(2):
                        mm(pb[:, h_ * 256:(h_ + 1) * 256], P1[:, :, c + h_], R1, True, False, (tP, tF), (tb,))
                        mm(pb[:, h_ * 256:(h_ + 1) * 256], P2[:, :, c + h_], R2, False, True, (tP, tF), (tb,))
                    cp("act" if (ev[0] % 2 == 0) else "dve", Bv[:, c:c + 2, :, :].rearrange("p c a k -> p (c a k)"), pb, (tb,), (tB[ev[0] % 2],))
                    ev[0] += 1

            def st_s2p(s):
                rows = slice(32 * s, 32 * s + 32)
                for g in range(128 // NPB):
                    pb, tb = bank()
                    for j in range(NPB):
                        n2 = g * NPB + j
                        mm(pb[rows, j * NN1:(j + 1) * NN1], Bv[:, :, 0, n2], HR[:, n2, :], True, False, tB + (tF,), (tb,))
                        mm(pb[rows, j * NN1:(j + 1) * NN1], Bv[:, :, 1, n2], HI[:, n2, :], False, True, tB + (tF,), (tb,))
                    cp("act", Ys[rows, g * NPB:(g + 1) * NPB, :].rearrange("p a b -> p (a b)"), pb[rows, :], (tb,), (tYs,))

            ns_ = nr // 32
            st_ka(0); st_trs1(0); st_s2(0)
            for s in range(ns_):
                if s + 1 < ns_:
                    st_ka(s + 1)
                    st_trs1(s + 1)
                st_s1p(s)
                st_s2p(s)
                if s + 1 < ns_:
                    st_s2(s + 1)
            uo = U[0:nr, 0:TOWN]
            stt(uo.rearrange("p (n1 n2) -> p n1 n2", n2=128), uo.rearrange("p (n1 n2) -> p n1 n2", n2=128), hysk[0:nr, ci_:ci_ + 1],
                Ys[0:nr, :, :].rearrange("p n2 n1 -> p n1 n2"), ALU.mult, ALU.add, (tYs, tC, tU), (tU,))
            tt("dve", X0[0:nr, :], X0[0:nr, :], uo, ALU.mult, (tU, tX0), (tX0,))
            P.dma("sp", YHY_d[row0:row0 + nr, :], X0[0:nr, :], (tX0,), (tYHY,))
            P.barrier()

    phaseD2()
    reset_arena(PERSIST)
    if stop_after == "D2":
        return finish(nc, P, out_d, dbg)

    G1B = alloc([128, D], F32); G2B = alloc([128, D], F32); FGB = alloc([128, D], F32)
    P.dma("sp", FGB, fg_d.partition_broadcast(128), (), (tC,))
    dg = alloc([128, 128], F32); tdg = T()
    for (dst, gi) in ((G1B, 2), (G2B, 5)):
        for dc in range(NCH):
            ts("dve", dg, ident, modT[:, gi * 8 + dc, 0:1], None, ALU.mult, None, (tC,), (tdg,))
            pb2, tb2 = bank()
            mm(pb2[:, 0:128], ones, dg, True, True, (tC, tdg), (tb2,))
            cp("act", dst[:, dc * 128:(dc + 1) * 128], pb2[:, 0:128], (tb2,), (tC,))
    PERSIST2 = apos[0]

    def load_w(dram, rows_k, cols):
        w = alloc([128, rows_k, cols], BF16); tw = T()
        for k in range(rows_k):
            for c0 in range(0, cols, 1024):
                wd = min(1024, cols - c0)
                P.dma("pool", w[:, k, c0:c0 + wd], dram[k * 128:(k + 1) * 128, c0:c0 + wd], (), (tw,))
        return w, tw

    def phaseE():
        wrg, twrg = load_w(rgp_d, NCH, D)
        why, twhy = load_w(hyp_d, NCH, D)
        wo, two = load_w(wout_d, NCH, D)
        W = 512
        ins = [[alloc([128, NCH, W], BF16) for _ in range(4)] for _ in range(2)]
        tin = [T(), T()]
        mg = alloc([128, NCH, W], BF16); tmg = T()
        m1 = alloc([128, W], F32); m2 = alloc([128, W], F32); tm = T()
        xts = [alloc([128, D], F32) for _ in range(2)]; txs = [T(), T()]
        t1 = alloc([128, D], F32); tt1 = T()
        xi = 0
        for ti in range(TOWN // W):
            bufs, tb_in = ins[ti % 2], tin[ti % 2]
            tsl = slice(ti * W, (ti + 1) * W)
            for bi, (src, tsrc) in enumerate(((YRG_d, tYRG), (YHY_d, tYHY))):
                P.dma("sp", bufs[bi], src[:, tsl].rearrange("(k p) t -> p k t", p=128), (tsrc,), (tb_in,))
            for bi, g in ((2, 5), (3, 6)):
                P.dma("sp", bufs[bi], PT_d[g * D:(g + 1) * D, tsl].rearrange("(k p) t -> p k t", p=128), (tPT,), (tb_in,))
            for dc in range(NCH):
                pa, ta = bank(); ph, th_ = bank()
                for k in range(NCH):
                    mm(pa, wrg[:, k, dc * 128:(dc + 1) * 128], bufs[0][:, k, :], k == 0, k == NCH - 1, (twrg, tb_in), (ta,))
                for k in range(NCH):
                    mm(ph, why[:, k, dc * 128:(dc + 1) * 128], bufs[1][:, k, :], k == 0, k == NCH - 1, (twhy, tb_in), (th_,))
                tt("dve", m1, pa, bufs[2][:, dc, :], ALU.mult, (ta, tb_in), (tm,))
                tt("dve", m2, ph, bufs[3][:, dc, :], ALU.mult, (th_, tb_in), (tm,))
                tt("pool", mg[:, dc, :], m1, m2, ALU.add, (tm,), (tmg,))
            for a in range(W // 128):
                xt, tx = xts[xi % 2], txs[xi % 2]
                xi += 1
                r0 = ti * W + a * 128
                P.dma("sp", xt, x_d[r0:r0 + 128, :], (), (tx,))
                for half in range(2):
                    pb, tb = bank()
                    for k in range(NCH):
                        mm(pb, mg[:, k, a * 128:(a + 1) * 128], wo[:, k, half * 512:(half + 1) * 512], k == 0, k == NCH - 1, (tmg, two), (tb,))
                    tt("dve", t1[:, half * 512:(half + 1) * 512], pb, G1B[:, half * 512:(half + 1) * 512], ALU.mult, (tb, tC), (tt1,))
                tt("pool", xt, xt, t1, ALU.add, (tt1, tx), (tx,))
                P.dma("sp", X1_d[r0:r0 + 128, :], xt, (tx,), (tX1,))

    phaseE()
    reset_arena(PERSIST2)
    if stop_after == "E":
        return finish(nc, P, out_d, dbg)

    def phaseF():
        NT = TOWN // 128
        BS, NB = MOE_BS, MOE_NB
        PTOT = NB * BS
        XS_d = dscr("XS", [PTOT, D], BF16); YS_d = dscr("YS", [PTOT, D], BF16)
        tXS, tYS = T(), T()
        W1f = mw1_d.rearrange("e k f -> (e k) f"); W3f = mw3_d.rearrange("e k f -> (e k) f"); W2f = mw2_d.rearrange("e f d -> (e f) d")
        wge, twge = load_w(wge_d, NCH, 36)
        tK = T()
        tri = alloc([128, 128], F32); pkw = alloc([128, 8], F32); pkw2 = alloc([128, 4], F32); jbs = alloc([128, NB], F32)
        for dst, nm in ((tri, "tri"), (pkw, "pkw"), (pkw2, "pkw2"), (jbs, "jbs")):
            P.dma("sp", dst, C[nm], (), (tK,))
        A2B = alloc([128, D], F32); B2B = alloc([128, D], F32)
        dg2 = alloc([128, 128], F32); tdg2 = T()
        for (dst, col) in ((A2B, A2), (B2B, B2)):
            for dc in range(NCH):
                ts("dve", dg2, ident, col[:, dc:dc + 1], None, ALU.mult, None, (tC,), (tdg2,))
                pb2, tb2 = bank()
                mm(pb2[:, 0:128], ones, dg2, True, True, (tC, tdg2), (tb2,))
                cp("act", dst[:, dc * 128:(dc + 1) * 128], pb2[:, 0:128], (tb2,), (tK,))
        OH = alloc([128, NT, 64], F32); tOH = T()
        RK = alloc([128, NT, 2], F32); WTS = alloc([128, NT, 2], F32); tRK = T()
        DSTf = alloc([128, NT, 2], F32); DSTi = alloc([128, NT, 2], I32); tDST = T()
        S = alloc([128, 64], F32); tS = T()
        CNT = alloc([128, 64], F32); BASE = alloc([128, 64], F32); PEND = alloc([128, 32], F32); PADD = alloc([128, 32], F32)
        Z32 = alloc([128, 32], F32); tmp64 = alloc([128, 64], F32); ttmp = T()
        EB = alloc([128, NB], F32); EBs = alloc([128, NB], F32)
        IDX1f = alloc([128, NB, 8], F32); IDX1 = alloc([128, NB, 8], I32); IDX2f = alloc([128, NB, 4], F32); IDX2 = alloc([128, NB, 4], I32)
        tIDX = T()
        xts = [alloc([128, D], F32) for _ in range(2)]; txs = [T(), T()]
        junk = alloc([128, D], BF16); tj = T()
        sm = alloc([128, 96], F32); tsm = T()
        ss = alloc([128, 2], F32)
        hxT = alloc([128, NCH, 128], BF16); thx = T()
        tm32 = alloc([128, D], F32); ttm = T()
        mark = apos[0]
        HXTM = alloc([128, NT, D], BF16); tHX = T()
        memset("dve", S, 0.0, (tS,))
        memset("dve", Z32, 0.0, (ttmp,))
        for a in range(NT):
            xt, tx = xts[a % 2], txs[a % 2]
            r0 = a * 128
            P.dma("sp", xt, X1_d[r0:r0 + 128, :], (tX1,), (tx,))
            act(junk, xt, AF.Square, (tx,), (tj, tx), accum=ss[:, 0:1])
            act(ss[:, 0:1], ss[:, 0:1], AF.Sqrt, (tx,), (tx,), scale=1.0 / D, bias=EPS)
            P.op("dve", lambda h: h.reciprocal(out=ss[:, 0:1], in_=ss[:, 0:1]), (tx,), (tx,))
            ts("dve", xt, xt, ss[:, 0:1], None, ALU.mult, None, (tx,), (tx,))
            tt("pool", tm32, xt, A2B, ALU.mult, (tx, tK), (ttm,))
            tt("pool", HXTM[:, a, :], tm32, B2B, ALU.add, (ttm, tK), (tHX,))
            for dc in range(0, NCH, 4):
                pb, tb = bank()
                for j in range(4):
                    tr(pb[:, j * 128:(j + 1) * 128], xt[:, (dc + j) * 128:(dc + j + 1) * 128], ident, (tx, tC), (tb,))
                for j in range(4):
                    act(hxT[:, dc + j, :], pb[:, j * 128:(j + 1) * 128], AF.Identity, (tb, tC), (thx,),
                        bias=B2[:, dc + j:dc + j + 1], scale=A2[:, dc + j:dc + j + 1])
            pb, tb = bank()
            for k in range(NCH):
                mm(pb[:, 0:36], hxT[:, k, :], wge[:, k, :], k == 0, k == NCH - 1, (thx, twge), (tb,))
            lg = sm[:, 0:36]
            tt("dve", lg, pb[:, 0:36], bgeB, ALU.add, (tb, tC), (tsm,))
            gmax = sm[:, 36:37]; sg = sm[:, 37:38]; oh = sm[:, 38:42]; ein = sm[:, 42:50]; m8 = sm[:, 50:58]
            e4 = sm[:, 58:62]; ngm = sm[:, 62:63]; dd = sm[:, 63:64]; mk1 = sm[:, 64:72]; mk2 = sm[:, 72:80]
            P.op("dve", lambda h: h.tensor_reduce(out=gmax, in_=lg[:, 0:4], axis=AX.X, op=ALU.max), (tsm,), (tsm,))
            ts("dve", ngm, gmax, -1.0, None, ALU.mult, None, (tsm,), (tsm,))
            act(e4, lg[:, 0:4], AF.Exp, (tsm,), (tsm,), bias=ngm, accum=sg)
            P.op("dve", lambda h: h.reciprocal(out=sg, in_=sg), (tsm,), (tsm,))
            ts("dve", oh, lg[:, 0:4], gmax, None, ALU.is_equal, None, (tsm,), (tsm,))
            ts("dve", ein, lg[:, 4:12], oh[:, 0:1], None, ALU.mult, None, (tsm,), (tsm,))
            for g in range(1, 4):
                stt(ein, lg[:, 4 + 8 * g:12 + 8 * g], oh[:, g:g + 1], ein, ALU.mult, ALU.add, (tsm,), (tsm,))
            P.op("dve", lambda h: h.max(out=m8, in_=ein), (tsm,), (tsm,))
            tt("dve", dd, m8[:, 1:2], m8[:, 0:1], ALU.subtract, (tsm,), (tsm,))
            act(dd, dd, AF.Exp, (tsm,), (tsm,))
            ts("dve", e4[:, 0:1], dd, 1.0, None, ALU.add, None, (tsm,), (tsm,))
            P.op("dve", lambda h: h.reciprocal(out=e4[:, 0:1], in_=e4[:, 0:1]), (tsm,), (tsm,))
            tt("dve", e4[:, 1:2], dd, e4[:, 0:1], ALU.mult, (tsm,), (tsm,))
            tt("dve", WTS[:, a, 0:1], e4[:, 0:1], sg, ALU.mult, (tsm,), (tRK,))
            tt("dve", WTS[:, a, 1:2], e4[:, 1:2], sg, ALU.mult, (tsm,), (tRK,))
            ts("dve", mk1, ein, m8[:, 0:1], None, ALU.is_equal, None, (tsm,), (tsm,))
            ts("dve", mk2, ein, m8[:, 1:2], None, ALU.is_equal, None, (tsm,), (tsm,))
            for g in range(4):
                ts("dve", OH[:, a, g * 8:(g + 1) * 8], mk1, oh[:, g:g + 1], None, ALU.mult, None, (tsm,), (tOH,))
                ts("dve", OH[:, a, 32 + g * 8:32 + (g + 1) * 8], mk2, oh[:, g:g + 1], None, ALU.mult, None, (tsm,), (tOH,))
            pR, tR = bank()
            mm(pR[:, 0:64], tri, OH[:, a, :], True, False, (tK, tOH), (tR,))
            mm(pR[:, 0:64], ones, S, False, True, (tC, tS), (tR,))
            tt("dve", tmp64, pR[:, 0:64], OH[:, a, :], ALU.mult, (tR, tOH), (ttmp,))
            P.op("dve", lambda h, a=a: h.tensor_reduce(out=RK[:, a, :], in_=tmp64.rearrange("p (a b) -> p a b", a=2), axis=AX.X, op=ALU.add), (ttmp,), (tRK,))
            tt("pool", S, S, OH[:, a, :], ALU.add, (tOH, tS), (tS,))
        pT, tT_ = bank()
        mm(pT[:, 0:64], ones, S, True, True, (tC, tS), (tT_,))
        cp("dve", CNT, pT[:, 0:64], (tT_,), (ttmp,))
        tt("dve", PADD, CNT[:, 0:32], CNT[:, 32:64], ALU.add, (ttmp,), (ttmp,))
        ts("dve", PADD, PADD, 1.0 / BS, (BS - 1.0) / BS - 0.5 + 0.5 / BS, ALU.mult, ALU.add, (ttmp,), (ttmp,))
        cp("dve", DSTi[:, 0:16, :].rearrange("p a b -> p (a b)"), PADD, (ttmp,), (tDST,))
        cp("dve", PADD, DSTi[:, 0:16, :].rearrange("p a b -> p (a b)"), (tDST,), (ttmp,))
        ts("dve", PADD, PADD, float(BS), None, ALU.mult, None, (ttmp,), (ttmp,))
        P.op("dve", lambda h: h.tensor_tensor_scan(out=PEND, data0=PADD, data1=Z32, initial=0.0, op0=ALU.add, op1=ALU.add), (ttmp,), (ttmp,))
        tt("dve", BASE[:, 0:32], PEND, PADD, ALU.subtract, (ttmp,), (ttmp,))
        tt("dve", BASE[:, 32:64], BASE[:, 0:32], CNT[:, 0:32], ALU.add, (ttmp,), (ttmp,))
        for a in range(NT):
            tt("dve", tmp64, OH[:, a, :], BASE, ALU.mult, (tOH, ttmp), (ttmp,))
            P.op("dve", lambda h, a=a: h.tensor_reduce(out=DSTf[:, a, :], in_=tmp64.rearrange("p (a b) -> p a b", a=2), axis=AX.X, op=ALU.add), (ttmp,), (tDST,))
        tt("dve", DSTf.rearrange("p a b -> p (a b)"), DSTf.rearrange("p a b -> p (a b)"), RK.rearrange("p a b -> p (a b)"), ALU.add, (tDST, tRK), (tDST,))
        cp("dve", DSTi.rearrange("p a b -> p (a b)"), DSTf.rearrange("p a b -> p (a b)"), (tDST,), (tDST,))
        memset("dve", EB, 0.0, (tIDX,))
        for e in range(NE):
            stt(EB, jbs, PEND[:, e:e + 1], EB, ALU.is_ge, ALU.add, (tK, ttmp, tIDX), (tIDX,))
        ts("dve", EB, EB, float(NE - 1), None, ALU.min, None, (tIDX,), (tIDX,))
        ts("dve", EBs, EB, 1024.0, None, ALU.mult, None, (tIDX,), (tIDX,))
        for j in range(NB):
            ts("dve", IDX1f[:, j, :], pkw, EBs[:, j:j + 1], None, ALU.add, None, (tK, tIDX), (tIDX,))
        ts("dve", EBs, EB, 512.0, None, ALU.mult, None, (tIDX,), (tIDX,))
        for j in range(NB):
            ts("dve", IDX2f[:, j, :], pkw2, EBs[:, j:j + 1], None, ALU.add, None, (tK, tIDX), (tIDX,))
        cp("dve", IDX1.rearrange("p a b -> p (a b)"), IDX1f.rearrange("p a b -> p (a b)"), (tIDX,), (tIDX,))
        cp("dve", IDX2.rearrange("p a b -> p (a b)"), IDX2f.rearrange("p a b -> p (a b)"), (tIDX,), (tIDX,))
        for a in range(NT):
            for k in range(2):
                P.idma("pool", XS_d, HXTM[:, a, :], DSTi[:, a, k:k + 1], None, (tHX, tDST), (tXS,))
        P.barrier()
        apos[0] = mark
        w1s = [alloc([128, NCH, DE], BF16) for _ in range(2)]
        w3s = [alloc([128, NCH, DE], BF16) for _ in range(2)]
        w2s = [alloc([128, 4, D], BF16) for _ in range(2)]
        tws = [(T(), T()), (T(), T())]
        wstg = [alloc([128, DE], F32) for _ in range(12)]; twstg = [T() for _ in range(12)]
        wstg2 = [alloc([128, D], F32) for _ in range(4)]; twstg2 = [T() for _ in range(4)]
        wi2 = [0]
        wi = [0]
        mark3 = apos[0]
        NR = 3
        xss = [alloc([128, D], BF16) for _ in range(NR)]; txss = [T() for _ in range(NR)]
        xTs = [alloc([128, NCH, 128], BF16) for _ in range(NR)]; txT = [T() for _ in range(NR)]
        hids = [alloc([128, DE], BF16) for _ in range(NR)]; thid = [T() for _ in range(NR)]
        hTs = [alloc([128, 4, 128], BF16) for _ in range(NR)]; thT = [T() for _ in range(NR)]
        ybs = [alloc([128, D], BF16) for _ in range(NR)]; tyb = [(T(), T()) for _ in range(NR)]
        s1s = [alloc([128, DE], F32) for _ in range(2)]; ts1s = [T(), T()]

        def wpieces(jb, grp):
            w1, w3, w2, tw = w1s[jb % 2], w3s[jb % 2], w2s[jb % 2], tws[jb % 2]
            pcs = []
            for k in range(NCH):
                pcs.append((w1[:, k, :], W1f, IDX1[:, jb, k:k + 1], False))
                pcs.append((w3[:, k, :], W3f, IDX1[:, jb, k:k + 1], False))
            for k in range(4):
                pcs.append((w2[:, k, :], W2f, IDX2[:, jb, k:k + 1], True))
            for (dst, src, idx, big) in pcs[grp * 5:(grp + 1) * 5]:
                if big:
                    sg_, tsg_ = wstg2[wi2[0] % len(wstg2)], twstg2[wi2[0] % len(wstg2)]
                    wi2[0] += 1
                else:
                    sg_, tsg_ = wstg[wi[0] % len(wstg)], twstg[wi[0] % len(wstg)]
                wi[0] += 1
                P.idma("pool", sg_, src, None, idx, (tIDX,), (tsg_,))
                cp("act" if wi[0] % 2 == 0 else "dve", dst, sg_, (tsg_,), (tw[wi[0] % 2],))

        def stageA(s):
            jb, sb = s // 4, s % 4
            w1, w3, tw = w1s[jb % 2], w3s[jb % 2], tws[jb % 2]
            xs, txs_ = xss[s % NR], txss[s % NR]
            xT, txT_ = xTs[s % NR], txT[s % NR]
            hid, thid_ = hids[s % NR], thid[s % NR]
            s1, ts1 = s1s[s % 2], ts1s[s % 2]
            r0 = jb * BS + sb * 128
            P.dma("sp", xs, XS_d[r0:r0 + 128, :], (tXS,), (txs_,))
            pb, tb = bank()
            pbb = pb.bitcast(BF16)
            for k in range(NCH):
                tr(pbb[:, k * 128:(k + 1) * 128], xs[:, k * 128:(k + 1) * 128], identb, (txs_, tC), (tb,))
            cp("dve", xT.rearrange("p a b -> p (a b)"), pbb, (tb,), (txT_,))
            p1, tp1 = bank(); p3, tp3 = bank()
            for k in range(NCH):
                mm(p1, xT[:, k, :], w1[:, k, :], k == 0, k == NCH - 1, (txT_,) + tw, (tp1,))
            for k in range(NCH):
                mm(p3, xT[:, k, :], w3[:, k, :], k == 0, k == NCH - 1, (txT_,) + tw, (tp3,))
            act(s1, p1, AF.Silu, (tp1,), (ts1,))
            tt("dve", hid, s1, p3, ALU.mult, (ts1, tp3), (thid_,))

        def stageB(s):
            jb, sb = s // 4, s % 4
            w2, tw = w2s[jb % 2], tws[jb % 2]
            hid, thid_ = hids[s % NR], thid[s % NR]
            hT, thT_ = hTs[s % NR], thT[s % NR]
            yb, tyb_ = ybs[s % NR], tyb[s % NR]
            r0 = jb * BS + sb * 128
            pb2, tb2 = bank()
            pbb2 = pb2.bitcast(BF16)
            for f in range(4):
                tr(pbb2[:, f * 128:(f + 1) * 128], hid[:, f * 128:(f + 1) * 128], identb, (thid_, tC), (tb2,))
            cp("act", hT.rearrange("p a b -> p (a b)"), pbb2[:, 0:512], (tb2,), (thT_,))
            for half in range(2):
                py, tpy = bank()
                for f in range(4):
                    mm(py, hT[:, f, :], w2[:, f, half * 512:(half + 1) * 512], f == 0, f == 3, (thT_,) + tw, (tpy,))
                cp("act" if half == 0 else "dve", yb[:, half * 512:(half + 1) * 512], py, (tpy,), (tyb_[half],))
            P.dma("sp", YS_d[r0:r0 + 128, :], yb, tyb_, (tYS,))

        NS = NB * (BS // 128)
        for g in range(4):
            wpieces(0, g)
        for t in range(NS + 1):
            if t < NS:
                stageA(t)
            if t >= 1:
                stageB(t - 1)
            if t < NS:
                jn = t // 4 + 1
                if jn < NB:
                    wpieces(jn, t % 4)
        P.barrier()
        apos[0] = mark3
        y1s = [alloc([128, D], BF16) for _ in range(2)]; y2s = [alloc([128, D], BF16) for _ in range(2)]; tys = [T(), T()]
        mo = alloc([128, D], F32); tmo = T()
        for a in range(NT):
            y1, y2, ty = y1s[a % 2], y2s[a % 2], tys[a % 2]
            xt, tx = xts[a % 2], txs[a % 2]
            r0 = a * 128
            P.idma("pool", y1, YS_d, None, DSTi[:, a, 0:1], (tYS, tDST), (ty,))
            P.idma("pool", y2, YS_d, None, DSTi[:, a, 1:2], (tYS, tDST), (ty,))
            P.dma("sp", xt, X1_d[r0:r0 + 128, :], (tX1,), (tx,))
            ts("dve", mo, y1, WTS[:, a, 0:1], None, ALU.mult, None, (ty, tRK), (tmo,))
            stt(mo, y2, WTS[:, a, 1:2], mo, ALU.mult, ALU.add, (ty, tRK), (tmo,))
            tt("pool", mo, mo, G2B, ALU.mult, (tC,), (tmo,))
            tt("pool", xt, xt, mo, ALU.add, (tmo,), (tx,))
            act(junk, xt, AF.Square, (tx,), (tj, tx), accum=ss[:, 1:2])
            act(ss[:, 1:2], ss[:, 1:2], AF.Sqrt, (tx,), (tx,), scale=1.0 / D, bias=EPS)
            P.op("dve", lambda h: h.reciprocal(out=ss[:, 1:2], in_=ss[:, 1:2]), (tx,), (tx,))
            stt(xt, xt, ss[:, 1:2], FGB, ALU.mult, ALU.mult, (tx, tC), (tx,))
            P.dma("sp", out_d[r0:r0 + 128, :], xt, (tx,), (tX1,))

    phaseF()
    return finish(nc, P, out_d, dbg)


def finish(nc, P, out_d, dbg):
    P.barrier()
    P.emit()
    return nc


def make_inputs(inp, core):
    b, rev = core // 2, (core % 2 == 1)
    f = lambda a: np.ascontiguousarray(np.asarray(a, np.float32))
    m = {}
    xs = np.asarray(inp["x"][b], np.float32)
    cs = np.asarray(inp["ctx"][b], np.float32)
    m["x"] = f(xs[::-1] if rev else xs)
    m["ctx"] = f(cs[::-1] if rev else cs)
    cT = np.stack([fm(inp["c"][b]), fm(inp["c_ctx"])], axis=-1)
    m["cT"] = f(cT)
    m["ada_w"] = f(inp["ada_w"][0])
    m["ada_bT"] = fm(inp["ada_b"][0])
    m["g1nT"] = fm(inp["norm1_g"][0])
    m["g2nT"] = fm(inp["norm2_g"][0])
    m["final_g"] = f(inp["final_g"]).reshape(1, D)
    m["w_in"] = f(inp["w_in"][0])
    rw = np.asarray(inp["rg_conv_w"][0], np.float32)
    zero = np.zeros((D,), np.float32)
    taps5 = [zero, rw[3], rw[2], rw[1], rw[0]] if rev else [rw[0], rw[1], rw[2], rw[3], zero]
    m["rg_cwT"] = f(np.stack([fm(tp) for tp in taps5], axis=-1))
    m["rg_cbT"] = fm(inp["rg_conv_b"][0])
    bd = np.zeros((128, 4, NCH, 128), np.float32)
    gnames = ("rg_wa_b", "rg_wx_b", "rg_wa_f", "rg_wx_f") if rev else ("rg_wa_f", "rg_wx_f", "rg_wa_b", "rg_wx_b")
    bnames = ("rg_ba_b", "rg_bx_b", "rg_ba_f", "rg_bx_f") if rev else ("rg_ba_f", "rg_bx_f", "rg_ba_b", "rg_bx_b")
    lnames = ("rg_lam_b", "rg_lam_f") if rev else ("rg_lam_f", "rg_lam_b")
    for gi, nm in enumerate(gnames):
        w = np.asarray(inp[nm][0], np.float32)
        for cc in range(NCH):
            bd[0:64, gi, cc, 0:64] = w[2 * cc]
            bd[64:128, gi, cc, 64:128] = w[2 * cc + 1]
    m["rg_bd"] = bd
    m["rg_biasT"] = f(np.stack([fm(inp[nm][0]) for nm in bnames], axis=1))
    m["rg_lamT"] = f(np.stack([fm(inp[nm][0]) for nm in lnames], axis=1))
    m["rg_proj"] = f(inp["rg_proj"][0])
    def c96(v):
        o = np.zeros((11 * 96,), np.float32); o[:1024] = np.asarray(v, np.float32)
        return np.ascontiguousarray(o.reshape(11, 96).T)
    hw = np.asarray(inp["hy_conv_w"][0], np.float32); hb = np.asarray(inp["hy_conv_b"][0], np.float32)
    jt = (2, 1, 0) if rev else (0, 1, 2)
    m["hy_cw96"] = f(np.stack([np.stack([c96(hw[j, g * 1024:(g + 1) * 1024]) for j in jt], axis=-1) for g in range(3)], axis=2))
    m["hy_cb96"] = f(np.stack([c96(hb[g * 1024:(g + 1) * 1024]) for g in range(3)], axis=-1))
    m["hy_w1"] = f(inp["hy_pos_w1"][0])
    m["hy_b1T"] = f(inp["hy_pos_b1"][0]).reshape(64, 1)
    m["hy_w2"] = f(inp["hy_pos_w2"][0])
    m["hy_b2T"] = f(inp["hy_pos_b2"][0]).reshape(64, 1)
    m["hy_frT"] = f(inp["hy_freq"][0]).reshape(64, 1)
    w3 = np.asarray(inp["hy_pos_w3"][0], np.float32)
    m["hy_w3"] = f(np.concatenate([w3[:, 1024:], w3[:, :1024]], axis=1) if rev else w3)
    m["hy_w3z"] = f(w3[:, :1024])
    m["hy_sk96"] = c96(inp["hy_skip"][0])
    m["hy_proj"] = f(inp["hy_proj"][0])
    m["w_out"] = f(inp["w_out"][0])
    m["moe_wge"] = f(np.concatenate([inp["moe_wg"][0], inp["moe_we"][0]], axis=1))
    m["moe_bge"] = f(np.concatenate([inp["moe_bg"][0], inp["moe_be"][0]])).reshape(1, 36)
    m["moe_w1"] = f(inp["moe_w1"][0])
    m["moe_w3"] = f(inp["moe_w3"][0])
    m["moe_w2"] = f(inp["moe_w2"][0])
    for nm, arr in host_consts().items():
        m["k_" + nm] = arr
    return m


def kernel(**inputs):
    nc = build()
    in_maps = [make_inputs(inputs, c) for c in range(NCORES)]
    res = run_bass_kernel_spmd(nc, in_maps, core_ids=list(range(NCORES)))
    B = NCORES // 2
    out = np.empty((B, L, D), np.float32)
    for c in range(NCORES):
        r = np.asarray(res.results[c]["out"], np.float32)
        if c % 2 == 0:
            out[c // 2, 0:TOWN] = r
        else:
            out[c // 2, TOWN:L] = r[::-1]
    return out
```

```python
import numpy as np
import ml_dtypes
import concourse.bass as bass
import concourse.mybir as mybir
from concourse.bass_utils import run_bass_kernel_spmd

F32 = mybir.dt.float32
BF16 = mybir.dt.bfloat16
I32 = mybir.dt.int32
ALU = mybir.AluOpType
AF = mybir.ActivationFunctionType
AX = mybir.AxisListType

L = 8192
D = 1024
NCH = 8
NTT = L // 128
CTX = 256
NE = 32
DE = 512
NFFT = 16384
EPS = 1e-6
NCORES = 8
MOE_BS = 512
MOE_NB = 2 * 4096 // MOE_BS + 32
TOWN = 4096


class T:
    __slots__ = ("w", "r")

    def __init__(self):
        self.w = {}
        self.r = {}


class Eng:
    def __init__(self, name, key, sem):
        self.name = name
        self.key = key
        self.sem = sem
        self.count = 0
        self.waited = {}
        self.prog = []
        self.dsems = []
        self.dnext = 0


class Planner:
    def __init__(self, nc, ndma=8):
        self.nc = nc
        self.sems = []
        self.totals = []
        self.E = {}
        for name in ("pe", "act", "dve", "pool", "sp"):
            k = self._newsem(name)
            self.E[name] = Eng(name, k, self.sems[k])
        for q in ("sp", "pool", "act"):
            for i in range({"sp": 16, "pool": 24, "act": 2}[q]):
                self.E[q].dsems.append(self._newsem(f"d_{q}{i}"))

    def _newsem(self, name):
        self.sems.append(self.nc.alloc_semaphore(name=name))
        self.totals.append(0)
        return len(self.sems) - 1

    def _need(self, reads, writes):
        need = {}
        for t in reads:
            for k, v in t.w.items():
                if need.get(k, 0) < v:
                    need[k] = v
        for t in writes:
            for k, v in t.w.items():
                if need.get(k, 0) < v:
                    need[k] = v
            for k, v in t.r.items():
                if need.get(k, 0) < v:
                    need[k] = v
        return need

    def _emit_waits(self, E, need, skip_self=False):
        for k, v in need.items():
            if skip_self and k == E.key:
                continue
            if E.waited.get(k, 0) >= v:
                continue
            E.waited[k] = v
            E.prog.append(("w", k, v))

    def op(self, ename, fn, reads=(), writes=()):
        E = self.E[ename]
        need = self._need(reads, writes)
        self._emit_waits(E, need, skip_self=(ename == "pe"))
        E.count += 1
        E.prog.append(("o", fn, E.key))
        for t in reads:
            t.r[E.key] = E.count
        for t in writes:
            t.w[E.key] = E.count

    def dma(self, q, out, in_, reads=(), writes=()):
        E = self.E[q]
        need = self._need(reads, writes)
        k = E.dsems[E.dnext]
        E.dnext = (E.dnext + 1) % len(E.dsems)
        if self.totals[k] > 0:
            need[k] = max(need.get(k, 0), self.totals[k])
        self._emit_waits(E, need)
        self.totals[k] += 16
        E.prog.append(("d", out, in_, k))
        for t in reads:
            t.r[k] = self.totals[k]
        for t in writes:
            t.w[k] = self.totals[k]

    def idma(self, q, out, in_, out_off, in_off, reads=(), writes=(), bound=None):
        E = self.E[q]
        need = self._need(reads, writes)
        k = E.dsems[E.dnext]
        E.dnext = (E.dnext + 1) % len(E.dsems)
        if self.totals[k] > 0:
            need[k] = max(need.get(k, 0), self.totals[k])
        self._emit_waits(E, need)
        self.totals[k] += 16
        E.prog.append(("i", out, in_, out_off, in_off, k, bound))
        for t in reads:
            t.r[k] = self.totals[k]
        for t in writes:
            t.w[k] = self.totals[k]

    def barrier(self):
        need = {}
        for E in self.E.values():
            if E.count:
                need[E.key] = E.count
            for k in E.dsems:
                if self.totals[k]:
                    need[k] = self.totals[k]
        for E in self.E.values():
            n2 = {k: v for k, v in need.items() if k != E.key}
            self._emit_waits(E, n2)

    def emit(self):
        sems = self.sems

        def replay(E, h):
            regs = {}
            for it in E.prog:
                if it[0] == "w":
                    h.wait_ge(sems[it[1]], it[2])
                elif it[0] == "o":
                    it[1](h).then_inc(sems[it[2]], 1)
                elif it[0] == "i":
                    oo = None if it[3] is None else bass.IndirectOffsetOnAxis(ap=it[3], axis=0)
                    io = None if it[4] is None else bass.IndirectOffsetOnAxis(ap=it[4], axis=0)
                    if it[6] is None:
                        h.indirect_dma_start(out=it[1], out_offset=oo, in_=it[2], in_offset=io).then_inc(sems[it[5]], 16)
                    else:
                        if it[6] not in regs:
                            regs[it[6]] = h.to_reg(it[6])
                        h.indirect_dma_start(out=it[1], out_offset=oo, in_=it[2], in_offset=io, bounds_check=regs[it[6]], oob_is_err=False).then_inc(sems[it[5]], 16)
                else:
                    h.dma_start(out=it[1], in_=it[2]).then_inc(sems[it[3]], 16)

        with self.nc.Block() as block:
            @block.tensor
            def _(h):
                replay(self.E["pe"], h)

            @block.scalar
            def _(h):
                replay(self.E["act"], h)

            @block.vector
            def _(h):
                replay(self.E["dve"], h)

            @block.gpsimd
            def _(h):
                replay(self.E["pool"], h)

            @block.sync
            def _(h):
                replay(self.E["sp"], h)


def _bf(a):
    return np.ascontiguousarray(a.astype(np.float32)).astype(ml_dtypes.bfloat16)


_CONST = None


def host_consts():
    global _CONST
    if _CONST is not None:
        return _CONST
    N = NFFT
    c = {}
    c["ident"] = np.eye(128, dtype=np.float32)
    c["identb"] = _bf(np.eye(128))
    c["ones"] = np.ones((128, 128), np.float32)
    n1 = np.arange(128)[:, None].astype(np.float64)
    k1 = np.arange(128)[None, :].astype(np.float64)
    th = -2 * np.pi * n1 * (2 * k1 + 1) / 256.0
    c["F1"] = _bf(np.concatenate([np.cos(th), np.sin(th)], axis=1))
    n2 = np.arange(128)[:, None, None].astype(np.float64)
    kk1 = np.arange(128)[None, :, None].astype(np.float64)
    kk2 = np.arange(64)[None, None, :].astype(np.float64)
    ph = -2 * np.pi * n2 * (kk1 + 0.5 + 128 * kk2) / N
    Gre, Gim = np.cos(ph), np.sin(ph)
    c["L1"] = _bf(np.concatenate([Gre, Gim], axis=2).reshape(128, 128 * 128))
    c["L2"] = _bf(np.concatenate([-Gim, Gre], axis=2).reshape(128, 128 * 128))
    k2 = np.arange(64)[:, None].astype(np.float64)
    nn2 = np.arange(128)[None, :].astype(np.float64)
    cp = 2 * np.pi * k2 * nn2 / 128.0
    Cre, Cim = np.cos(cp), np.sin(cp)
    c["R1"] = _bf(np.concatenate([np.concatenate([Cre, Cim], 1), np.concatenate([-Cim, Cre], 1)], 0))
    c["R2"] = _bf(np.concatenate([np.concatenate([-Cim, Cre], 1), np.concatenate([Cre, Cim], 1)], 0))
    hk1 = np.arange(128)[:, None, None].astype(np.float64)
    hn2 = np.arange(128)[None, :, None].astype(np.float64)
    hn1 = np.arange(TOWN // 128)[None, None, :].astype(np.float64)
    hp = 2 * np.pi * (hk1 + 0.5) * (128 * hn1 + hn2) / N
    c["HR"] = _bf((2.0 / N) * np.cos(hp).reshape(128, 128 * (TOWN // 128)))
    c["HI"] = _bf(-(2.0 / N) * np.sin(hp).reshape(128, 128 * (TOWN // 128)))
    PA = np.zeros((128, 128)); PC = np.zeros((128, 128))
    for m in range(64):
        PA[m, m] = 1; PA[m, m + 64] = 1
        PC[64 + m, m] = 1; PC[64 + m, 64 + m] = -1
    c["PA"] = _bf(PA); c["PC"] = _bf(PC)
    n = np.arange(N)
    s = np.where(n <= 8192, n, N - n).astype(np.int64)
    s = np.where(n == 8192, 0, s)
    sf = s.astype(np.float32)
    t_norm = (sf / np.float32(L - 1)).astype(np.float32)
    bands = np.linspace(1e-4, 15, 16, dtype=np.float32)
    ang = (np.float32(2.0 * np.pi / L) * sf[:, None] * bands[None, :]).astype(np.float32)
    rows = L // 64
    row_lag = (s // 64).astype(np.float32) / np.float32(rows)
    colb = np.arange(1, 9, dtype=np.float32)
    cang = (np.float32(2.0 * np.pi / 64) * (s % 64).astype(np.float32)[:, None] * colb[None, :]).astype(np.float32)
    feats = np.concatenate([t_norm[:, None], np.cos(ang), np.sin(ang), row_lag[:, None],
                            np.cos(cang), np.sin(cang)], axis=-1).astype(np.float32)
    c["featsT"] = np.ascontiguousarray(feats.T)
    tn = t_norm.copy()
    tn[8192] = 1e4
    c["ntn2"] = np.ascontiguousarray((-tn).reshape(128, 128))
    mxd = np.log(1e-2) / 0.3
    mnd = np.log(1e-2) / 1.5
    deltas = np.abs(np.linspace(mnd, mxd, 1024, dtype=np.float32))
    c["DLT"] = np.ascontiguousarray(np.broadcast_to(deltas[None, :], (128, 1024))).astype(np.float32)
    dec = np.exp(-tn.astype(np.float32)[:, None] * deltas[None, :]).astype(np.float32)
    dec = dec.reshape(128, 128, 32, 32).transpose(2, 0, 1, 3).reshape(32, 128, 4096)
    c["DEC"] = _bf(dec)
    c["tri"] = np.triu(np.ones((128, 128), np.float32), k=1)
    pp = np.arange(128, dtype=np.float32)[:, None]
    c["pkw"] = np.ascontiguousarray(pp + 128.0 * np.arange(8, dtype=np.float32)[None, :])
    c["pkw2"] = np.ascontiguousarray(pp + 128.0 * np.arange(4, dtype=np.float32)[None, :])
    c["jbs"] = np.ascontiguousarray(np.broadcast_to((MOE_BS * np.arange(MOE_NB, dtype=np.float32))[None, :], (128, MOE_NB)))
    _CONST = c
    return c


def fm(v, nch=None):
    v = np.asarray(v, np.float32)
    return np.ascontiguousarray(v.reshape(-1, 128).T)


def build(debug=(), stop_after=None):
    nc = bass.Bass("TRN2", target_bir_lowering=False)
    P = Planner(nc)
    dbg = set(debug)

    def din(name, shape, dt=F32):
        return nc.dram_tensor(name, list(shape), dt, kind="ExternalInput").ap()

    def dscr(name, shape, dt):
        kind = "ExternalOutput" if name in dbg else "Internal"
        return nc.dram_tensor(name, list(shape), dt, kind=kind).ap()

    x_d = din("x", [L, D])
    ctx_d = din("ctx", [CTX, D])
    cT_d = din("cT", [128, NCH, 2])
    adaw_d = din("ada_w", [D, 6 * D])
    adab_d = din("ada_bT", [128, 48])
    g1n_d = din("g1nT", [128, NCH])
    g2n_d = din("g2nT", [128, NCH])
    fg_d = din("final_g", [1, D])
    win_d = din("w_in", [D, 7 * D])
    rgcw_d = din("rg_cwT", [128, NCH, 5])
    rgcb_d = din("rg_cbT", [128, NCH])
    rgbd_d = din("rg_bd", [128, 4, NCH, 128])
    rgbias_d = din("rg_biasT", [128, 4, NCH])
    rglam_d = din("rg_lamT", [128, 2, NCH])
    rgp_d = din("rg_proj", [D, D])
    hycw_d = din("hy_cw96", [96, 11, 3, 3])
    hycb_d = din("hy_cb96", [96, 11, 3])
    hyw1_d = din("hy_w1", [50, 64])
    hyb1_d = din("hy_b1T", [64, 1])
    hyw2_d = din("hy_w2", [64, 64])
    hyb2_d = din("hy_b2T", [64, 1])
    hyfr_d = din("hy_frT", [64, 1])
    hyw3_d = din("hy_w3", [64, 2048])
    hyw3z_d = din("hy_w3z", [64, 1024])
    hysk_d = din("hy_sk96", [96, 11])
    hyp_d = din("hy_proj", [D, D])
    wout_d = din("w_out", [D, D])
    wge_d = din("moe_wge", [D, 36])
    bge_d = din("moe_bge", [1, 36])
    mw1_d = din("moe_w1", [NE, D, DE])
    mw3_d = din("moe_w3", [NE, D, DE])
    mw2_d = din("moe_w2", [NE, DE, D])
    C = {}
    hc = host_consts()
    for nm, arr in hc.items():
        C[nm] = din("k_" + nm, arr.shape, BF16 if arr.dtype == ml_dtypes.bfloat16 else F32)
    out_d = nc.dram_tensor("out", [TOWN, D], F32, kind="ExternalOutput").ap()

    PT_d = dscr("PT", [7 * D, L], BF16)
    YRG_d = dscr("YRG", [D, TOWN], BF16)
    YHY_d = dscr("YHY", [D, TOWN], BF16)
    X1_d = dscr("X1", [TOWN, D], F32)
    KAC_d = dscr("KAC", [32, 128, 2, 4096], BF16)
    tPT, tYRG, tYHY, tX1, tKAC = T(), T(), T(), T(), T()

    banks = []
    for i in range(8):
        banks.append((nc.alloc_psum_tensor(f"ps{i}", [128, 512], F32)[:, :], T()))
    bstate = [0]

    def bank():
        b = banks[bstate[0] % 8]
        bstate[0] += 1
        return b

    ARENA = 196608 - 2048
    arena = nc.alloc_sbuf_tensor("arena", [128, ARENA // 2], BF16)
    apos = [0]

    def alloc(shape, dt):
        esz = 4 if dt in (F32, I32) else 2
        n = int(np.prod(shape[1:]))
        nbytes = (n * esz + 63) // 64 * 64
        off = apos[0]
        apos[0] += nbytes
        assert apos[0] <= ARENA, f"SBUF overflow {apos[0]}"
        ap = arena[0:shape[0], off // 2: off // 2 + n * esz // 2]
        if esz == 4:
            ap = ap.bitcast(dt)
        if len(shape) > 2:
            names = " ".join(f"a{i}" for i in range(len(shape) - 1))
            kw = {f"a{i}": int(shape[i + 1]) for i in range(len(shape) - 1)}
            ap = ap.rearrange(f"p ({names}) -> p {names}", **kw)
        return ap

    def reset_arena(to=0):
        P.barrier()
        apos[0] = to

    def act(out, in_, func, r, w, bias=None, scale=None, accum=None, eng="act"):
        kw = {}
        if bias is not None:
            kw["bias"] = bias
        if scale is not None:
            kw["scale"] = scale
        if accum is not None:
            kw["accum_out"] = accum
        P.op("act", lambda h: h.activation(out=out, in_=in_, func=func, **kw), r, w)

    def ts(eng, out, in0, s1, s2, op0, op1, r, w):
        if op1 is None:
            P.op(eng, lambda h: h.tensor_scalar(out=out, in0=in0, scalar1=s1, scalar2=None, op0=op0), r, w)
        else:
            P.op(eng, lambda h: h.tensor_scalar(out=out, in0=in0, scalar1=s1, scalar2=s2, op0=op0, op1=op1), r, w)

    def stt(out, in0, sc, in1, op0, op1, r, w):
        P.op("dve", lambda h: h.scalar_tensor_tensor(out=out, in0=in0, scalar=sc, in1=in1, op0=op0, op1=op1), r, w)

    def tt(eng, out, in0, in1, op, r, w):
        P.op(eng, lambda h: h.tensor_tensor(out=out, in0=in0, in1=in1, op=op), r, w)

    def cp(eng, out, in_, r, w):
        if eng == "act":
            P.op("act", lambda h: h.activation(out=out, in_=in_, func=AF.Copy), r, w)
        else:
            P.op(eng, lambda h: h.tensor_copy(out=out, in_=in_), r, w)

    def mm(out, lhsT, rhs, start, stop, r, w):
        P.op("pe", lambda h: h.matmul(out, lhsT, rhs, start=start, stop=stop), r, w)

    def tr(out, in_, ident, r, w):
        P.op("pe", lambda h: h.transpose(out, in_, ident), r, w)

    def memset(eng, ap, val, w):
        P.op(eng, lambda h: h.memset(ap, val), (), w)

    tC = T()
    ident = alloc([128, 128], F32)
    identb = alloc([128, 128], BF16)
    ones = alloc([128, 128], F32)
    modT = alloc([128, 48, 2], F32)
    A1 = alloc([128, NCH], F32); B1 = alloc([128, NCH], F32)
    A1c = alloc([128, NCH], F32); B1c = alloc([128, NCH], F32)
    A2 = alloc([128, NCH], F32); B2 = alloc([128, NCH], F32)
    pass
    rgcw = alloc([128, NCH, 5], F32); rgcb = alloc([128, NCH], F32)
    rgbias = alloc([128, 4, NCH], F32); nrgbias = alloc([128, 4, NCH], F32)
    rglam = alloc([128, 2, NCH], F32)
    sa1 = alloc([128, 2, NCH], F32); sa2 = alloc([128, 2, NCH], F32)
    hycw = alloc([96, 11, 3, 3], F32); hycb = alloc([96, 11, 3], F32); hysk = alloc([96, 11], F32)
    H0 = alloc([128, 2, NCH], F32)
    bgeB = alloc([128, 36], F32)
    PERSIST = apos[0]

    P.dma("sp", ident, C["ident"], (), (tC,))
    P.dma("sp", identb, C["identb"], (), (tC,))
    P.dma("sp", ones, C["ones"], (), (tC,))
    for dst, src in ((rgcw, rgcw_d), (rgcb, rgcb_d), (rgbias, rgbias_d), (rglam, rglam_d),
                     (hycw, hycw_d), (hycb, hycb_d), (hysk, hysk_d)):
        P.dma("sp", dst, src, (), (tC,))
    P.dma("sp", bgeB, bge_d.partition_broadcast(128), (), (tC,))

    def phaseA():
        cT = alloc([128, NCH, 2], F32)
        scT = alloc([128, NCH, 2], F32)
        adab = alloc([128, 48], F32)
        g1n = alloc([128, NCH], F32); g2n = alloc([128, NCH], F32)
        tmp = alloc([128, NCH], F32)
        dg = alloc([128, 128], F32)
        wbuf = [alloc([128, 1536], F32) for _ in range(3)]
        tw = [T() for _ in range(3)]
        tS = T()
        P.dma("sp", cT, cT_d, (), (tS,))
        P.dma("sp", adab, adab_d, (), (tS,))
        P.dma("sp", g1n, g1n_d, (), (tS,))
        P.dma("sp", g2n, g2n_d, (), (tS,))
        act(scT, cT, AF.Silu, (tS,), (tS,))
        pb, tb = bank()
        i = 0
        for k in range(NCH):
            for q in range(4):
                wb, twb = wbuf[i % 3], tw[i % 3]
                i += 1
                P.dma("sp", wb, adaw_d[k * 128:(k + 1) * 128, q * 1536:(q + 1) * 1536], (), (twb,))
                for jj in range(12):
                    j = q * 12 + jj
                    mm(pb[:, 2 * j:2 * j + 2], wb[:, jj * 128:(jj + 1) * 128], scT[:, k, :],
                       (k == 0 and j == 0), (k == NCH - 1 and j == 47), (twb, tS), (tb,))
        for col in range(2):
            tt("dve", modT[:, :, col], pb[:, col:96:2], adab, ALU.add, (tb, tS), (tC,))
        for (Ad, Bd, gn, sci, shi, col) in ((A1, B1, g1n, 1, 0, 0), (A1c, B1c, g1n, 1, 0, 1), (A2, B2, g2n, 4, 3, 0)):
            ts("dve", tmp, modT[:, sci * 8:(sci + 1) * 8, col], 1.0, None, ALU.add, None, (tC,), (tS,))
            tt("dve", Ad, tmp, gn, ALU.mult, (tS,), (tC,))
            cp("dve", Bd, modT[:, shi * 8:(shi + 1) * 8, col], (tC,), (tC,))
        act(sa1, rglam, AF.Exp, (tC,), (tC,), scale=-1.0)
        act(sa1, sa1, AF.Ln, (tC,), (tC,), bias=1.0)
        ts("dve", sa2, sa1, -16.0, None, ALU.mult, None, (tC,), (tC,))
        ts("dve", sa1, sa1, -8.0, None, ALU.mult, None, (tC,), (tC,))
        ts("dve", nrgbias, rgbias, -1.0, None, ALU.mult, None, (tC,), (tC,))

    phaseA()
    reset_arena(PERSIST)

    def phaseB(src_d, ntok, W, ccs_all, ccs_fn, Asc, Bsc, dst_d, tdst, sig_from=40):
        nsub = W // 128
        ncol = len(ccs_all)
        pos = {cc: i for i, cc in enumerate(ccs_all)}
        winb = alloc([128, NCH, ncol * 128], BF16)
        tWs = [T() for _ in range((ncol + 7) // 8)]
        c0 = ccs_all[0] * 128
        for blk in range(0, ncol * 128, 1024):
            wd = min(1024, ncol * 128 - blk)
            for k in range(NCH):
                P.dma("pool", winb[:, k, blk:blk + wd], win_d[k * 128:(k + 1) * 128, c0 + blk:c0 + blk + wd], (), (tWs[blk // 1024],))
        xts = [alloc([128, nsub, D], F32) for _ in range(2)]
        txs = [T(), T()]
        junk = alloc([128, D], BF16); tj = T()
        ss = [alloc([128, nsub], F32) for _ in range(2)]
        hxs = [alloc([128, NCH, W], BF16) for _ in range(2)]
        ths = [T(), T()]
        stg = [alloc([128, 4, W], BF16) for _ in range(3)]
        tst = [(T(), T()) for _ in range(3)]
        si = 0
        for ti in range(ntok // W):
            ccs = ccs_fn(ti)
            xt, tx, s_, hx, th = xts[ti % 2], txs[ti % 2], ss[ti % 2], hxs[ti % 2], ths[ti % 2]
            P.dma("sp", xt, src_d[ti * W:(ti + 1) * W, :].rearrange("(a p) d -> p a d", p=128), (), (tx,))
            for a in range(nsub):
                act(junk, xt[:, a, :], AF.Square, (tx,), (tj, tx), accum=s_[:, a:a + 1])
            act(s_, s_, AF.Sqrt, (tx,), (tx,), scale=1.0 / D, bias=EPS)
            P.op("dve", lambda h, s_=s_: h.reciprocal(out=s_, in_=s_), (tx,), (tx,))
            for a in range(nsub):
                ts("dve" if a % 2 == 0 else "pool", xt[:, a, :], xt[:, a, :], s_[:, a:a + 1], None, ALU.mult, None, (tx,), (tx,))
            for dc in range(NCH):
                pb, tb = bank()
                for a in range(nsub):
                    tr(pb[:, a * 128:(a + 1) * 128], xt[:, a, dc * 128:(dc + 1) * 128], ident, (tx, tC), (tb,))
                act(hx[:, dc, :], pb[:, 0:W], AF.Identity, (tb, tC), (th,), bias=Bsc[:, dc:dc + 1], scale=Asc[:, dc:dc + 1])
            for ci, cc in enumerate(ccs):
                pb, tb = bank()
                wi = pos[cc]
                for k in range(NCH):
                    mm(pb[:, 0:W], winb[:, k, wi * 128:(wi + 1) * 128], hx[:, k, :], k == 0, k == NCH - 1, (tWs[wi // 8], th), (tb,))
                sg, tsg = stg[si % 3], tst[si % 3]
                if cc >= sig_from:
                    act(sg[:, ci % 4, :], pb[:, 0:W], AF.Sigmoid, (tb,), (tsg[0],))
                elif ci % 2 == 0:
                    cp("act", sg[:, ci % 4, :], pb[:, 0:W], (tb,), (tsg[0],))
                else:
                    cp("dve", sg[:, ci % 4, :], pb[:, 0:W], (tb,), (tsg[1],))
                if ci % 4 == 3:
                    r0 = ccs[ci - 3] * 128
                    P.dma("sp", dst_d[r0:r0 + 512, ti * W:(ti + 1) * W].rearrange("(a p) t -> p a t", p=128), sg, tsg, (tdst,))
                    si += 1

    NOWN5 = TOWN // 512
    CC_ALL = list(range(56))
    CC_REST = list(range(0, 8)) + list(range(24, 40))
    CC_HALO = CC_REST + list(range(16, 24))

    def ccs_main(ti):
        if ti < NOWN5:
            return CC_ALL
        if ti == NOWN5:
            return CC_HALO
        return CC_REST

    phaseB(x_d, L, 512, CC_ALL, ccs_main, A1, B1, PT_d, tPT)
    reset_arena(PERSIST)
    if stop_after == "B":
        return finish(nc, P, out_d, dbg)

    PTC_d = dscr("PTC", [D, CTX], BF16)
    tPTC = T()
    phaseB(ctx_d, CTX, 256, list(range(8)), lambda ti: list(range(8)), A1c, B1c, PTC_d, tPTC)
    reset_arena(PERSIST)

    def phaseC(src_d, tsrc, Lt, Lown, TW, is_ctx):
        ntile = Lt // TW
        nown = Lown // TW
        nq = TW // 512 if TW >= 512 else 1
        QW = min(512, TW)
        wbd = alloc([128, 4, NCH, 128], BF16); tWb = T()
        P.dma("pool", wbd.rearrange("p a b c -> p (a b c)"), rgbd_d.rearrange("p a b c -> p (a b c)"), (), (tWb,))
        PX = alloc([128, Lt + 4], BF16); tPX = T()
        xc = alloc([128, Lt], BF16); txc = T()
        if not is_ctx:
            PRG = alloc([128, Lown], BF16); tPRG = T()
            HB = alloc([128, Lown], BF16); tHB = T()
            g1 = alloc([128, TW], F32); g2 = alloc([128, TW], F32); tg = T()
            yo = [alloc([128, TW], BF16) for _ in range(2)]; tyo = [T(), T()]
        NG = 2
        rts = [alloc([128, TW], F32) for _ in range(NG)]
        ats = [alloc([128, TW], F32) for _ in range(NG)]
        its = [alloc([128, TW], F32) for _ in range(NG)]
        tgts = [T() for _ in range(NG)]
        hts = [alloc([128, TW], F32) for _ in range(2)]; tht = [T(), T()]
        memset("dve", PX[:, 0:2], 0.0, (tPX,))
        memset("dve", PX[:, Lt + 2:Lt + 4], 0.0, (tPX,))
        hcount = 0
        yi = 0
        gi_ = 0
        for cc in range(NCH):
            P.dma("sp", PX[:, 2:Lt + 2], src_d[cc * 128:(cc + 1) * 128, 0:Lt], (tsrc,), (tPX,))
            if not is_ctx:
                P.dma("sp", PRG, src_d[D + cc * 128:D + (cc + 1) * 128, 0:Lown], (tsrc,), (tPRG,))
            act(xc, PX[:, 0:Lt], AF.Identity, (tPX, tC), (txc,), bias=rgcb[:, cc:cc + 1], scale=rgcw[:, cc, 0:1])
            for j in range(1, 5):
                stt(xc, PX[:, j:j + Lt], rgcw[:, cc, j:j + 1], xc, ALU.mult, ALU.add, (tPX, tC), (txc,))
            for d in (1, 0):
                order = range(ntile - 1, -1, -1) if d == 1 else range(nown)
                first = True
                for ti in order:
                    sl = slice(ti * TW, (ti + 1) * TW)
                    rt, at, it, tgt = rts[gi_ % NG], ats[gi_ % NG], its[gi_ % NG], tgts[gi_ % NG]
                    gi_ += 1
                    prs, pis = [], []
                    for q in range(nq):
                        pr, tr_ = bank(); pi, ti_ = bank()
                        mm(pr[:, 0:QW], wbd[:, 2 * d, cc, :], xc[:, ti * TW + q * QW: ti * TW + (q + 1) * QW], True, True, (tWb, txc), (tr_,))
                        mm(pi[:, 0:QW], wbd[:, 2 * d + 1, cc, :], xc[:, ti * TW + q * QW: ti * TW + (q + 1) * QW], True, True, (tWb, txc), (ti_,))
                        prs.append((pr, tr_)); pis.append((pi, ti_))
                    for q in range(nq):
                        act(rt[:, q * QW:(q + 1) * QW], prs[q][0][:, 0:QW], AF.Sigmoid, (prs[q][1], tC), (tgt,), bias=rgbias[:, 2 * d, cc:cc + 1])
                    for q in range(nq):
                        act(it[:, q * QW:(q + 1) * QW], pis[q][0][:, 0:QW], AF.Sigmoid, (pis[q][1], tC), (tgt,), bias=rgbias[:, 2 * d + 1, cc:cc + 1])
                    act(at, rt, AF.Exp, (tgt, tC), (tgt,), scale=sa1[:, d, cc:cc + 1])
                    act(rt, rt, AF.Exp, (tgt, tC), (tgt,), scale=sa2[:, d, cc:cc + 1])
                    act(rt, rt, AF.Sqrt, (tgt,), (tgt,), scale=-1.0, bias=1.0)
                    tt("pool", it, it, xc[:, sl], ALU.mult, (tgt, txc), (tgt,))
                    tt("dve", rt, rt, it, ALU.mult, (tgt,), (tgt,))
                    ht, th_ = hts[hcount % 2], tht[hcount % 2]
                    hp, thp = hts[(hcount + 1) % 2], tht[(hcount + 1) % 2]
                    hcount += 1
                    if first:
                        init = 0.0 if is_ctx else H0[:, d, cc:cc + 1]
                        rd = (tgt,) if is_ctx else (tgt, tC)
                    else:
                        init = hp[:, 0:1] if d == 1 else hp[:, TW - 1:TW]
                        rd = (tgt, thp)
                    first = False
                    if d == 1:
                        P.op("dve", lambda h, ht=ht, init=init, at=at, rt=rt: h.tensor_tensor_scan(out=ht[:, ::-1], data0=at[:, ::-1], data1=rt[:, ::-1], initial=init, op0=ALU.mult, op1=ALU.add), rd, (th_,))
                    else:
                        P.op("dve", lambda h, ht=ht, init=init, at=at, rt=rt: h.tensor_tensor_scan(out=ht, data0=at, data1=rt, initial=init, op0=ALU.mult, op1=ALU.add), rd, (th_,))
                    if is_ctx:
                        last = (ti == 0) if d == 1 else (ti == ntile - 1)
                        if last:
                            col = ht[:, 0:1] if d == 1 else ht[:, TW - 1:TW]
                            cp("dve", H0[:, d, cc:cc + 1], col, (th_,), (tC,))
                        continue
                    if d == 1:
                        if ti < nown:
                            cp("act", HB[:, sl], ht, (th_,), (tHB,))
                    else:
                        xg = PRG[:, sl]
                        tt("pool", g1, xg, xg, ALU.mult, (tPRG,), (tg,))
                        ts("pool", g1, g1, 0.044715, 1.0, ALU.mult, ALU.add, (tg,), (tg,))
                        tt("pool", g1, g1, xg, ALU.mult, (tg, tPRG), (tg,))
                        act(g1, g1, AF.Sigmoid, (tg,), (tg,), scale=1.5957691216057308)
                        tt("pool", g1, g1, xg, ALU.mult, (tg, tPRG), (tg,))
                        tt("dve", g2, ht, HB[:, sl], ALU.add, (th_, tHB), (tg,))
                        y, ty = yo[yi % 2], tyo[yi % 2]
                        yi += 1
                        tt("dve", y, g2, g1, ALU.mult, (tg,), (ty,))
                        P.dma("sp", YRG_d[cc * 128:(cc + 1) * 128, sl], y, (ty,), (tYRG,))

    phaseC(PTC_d, tPTC, CTX, CTX, 256, True)
    reset_arena(PERSIST)
    phaseC(PT_d, tPT, L, TOWN, 2048, False)
    reset_arena(PERSIST)
    if stop_after == "C":
        return finish(nc, P, out_d, dbg)

    def load_fft_tables():
        tF = T()
        L1 = alloc([128, 16384], BF16); L2 = alloc([128, 16384], BF16)
        for q in range(4):
            P.dma("sp", L1[:, q * 4096:(q + 1) * 4096], C["L1"][:, q * 4096:(q + 1) * 4096], (), (tF,))
            P.dma("sp", L2[:, q * 4096:(q + 1) * 4096], C["L2"][:, q * 4096:(q + 1) * 4096], (), (tF,))
        F1 = alloc([128, 256], BF16)
        P.dma("sp", F1, C["F1"], (), (tF,))
        return tF, L1, L2, F1

    def fft_fwd(src, Krows, A, tA, F1, tF, tsrc, ev):
        for c in range(0, 32, 2):
            pb, tb = bank()
            for h_ in range(2):
                mm(pb[:, h_ * 256:(h_ + 1) * 256], src[0:Krows, :, c + h_], F1[0:Krows, :], True, True, tuple(tsrc) + (tF,), (tb,))
            cp("act" if (ev[0] % 2 == 0) else "dve", A[:, c:c + 2, :, :].rearrange("p c a k -> p (c a k)"), pb, (tb,), (tA[ev[0] % 2],))
            ev[0] += 1

    def fft_s2(A, tA, L1, L2, tF, g):
        pb, tb = bank()
        for j in range(16):
            k1 = g * 16 + j
            mm(pb[:, j * 32:(j + 1) * 32], L1[:, k1 * 128:(k1 + 1) * 128], A[:, :, 0, k1], True, False, (tF,) + tuple(tA), (tb,))
            mm(pb[:, j * 32:(j + 1) * 32], L2[:, k1 * 128:(k1 + 1) * 128], A[:, :, 1, k1], False, True, (tF,) + tuple(tA), (tb,))
        return pb, tb

    def phaseD1():
        tF, L1, L2, F1 = load_fft_tables()
        PA = alloc([128, 128], BF16); PC = alloc([128, 128], BF16)
        P.dma("sp", PA, C["PA"], (), (tF,)); P.dma("sp", PC, C["PC"], (), (tF,))
        z2T = alloc([64, NFFT], BF16); tz = T()
        w3b = alloc([64, 2048], BF16); w3z = alloc([64, 1024], BF16)
        DLT = alloc([128, 1024], F32); ntn2 = alloc([128, 128], F32)
        P.dma("sp", DLT, C["DLT"], (), (tF,)); P.dma("sp", ntn2, C["ntn2"], (), (tF,))
        P.dma("pool", w3b, hyw3_d, (), (tF,))
        P.dma("pool", w3z, hyw3z_d, (), (tF,))
        ts("dve", w3b[:, 1024:2048], w3b[:, 1024:2048], -1.0, None, ALU.mult, None, (tF,), (tF,))
        mark = apos[0]
        w1 = alloc([50, 64], F32); w2 = alloc([64, 64], F32)
        b1 = alloc([64, 1], F32); b2 = alloc([64, 1], F32); fr = alloc([64, 1], F32)
        s1c = alloc([64, 1], F32); o1c = alloc([64, 1], F32); o2c = alloc([64, 1], F32)
        tM = T()
        for dst, src in ((w1, hyw1_d), (w2, hyw2_d), (b1, hyb1_d), (b2, hyb2_d), (fr, hyfr_d)):
            P.dma("sp", dst, src, (), (tM,))
        TWO_PI = 2.0 * np.pi
        ts("dve", s1c, fr, 1.0 / TWO_PI, None, ALU.mult, None, (tM,), (tM,))
        tt("dve", o1c, s1c, b1, ALU.mult, (tM,), (tM,))
        ts("dve", o1c, o1c, 8.0, None, ALU.add, None, (tM,), (tM,))
        tt("dve", o2c, s1c, b2, ALU.mult, (tM,), (tM,))
        ts("dve", o2c, o2c, 8.0, None, ALU.add, None, (tM,), (tM,))
        fts = [alloc([50, 512], F32) for _ in range(2)]; tft = [T(), T()]
        q_ = alloc([64, 512], F32); qi = alloc([64, 512], I32); qf = alloc([64, 512], F32); z1 = alloc([64, 512], F32)
        tq = T()

        def sin_layer(pb, tb, oc, out, tout):
            ts("dve", q_, pb[0:64, :], s1c, oc, ALU.mult, ALU.add, (tb, tM), (tq,))
            cp("dve", qi, q_, (tq,), (tq,))
            cp("dve", qf, qi, (tq,), (tq,))
            tt("dve", q_, q_, qf, ALU.subtract, (tq,), (tq,))
            act(out, q_, AF.Sin, (tq,), (tout,), scale=TWO_PI)

        for i in range(NFFT // 512):
            ft, tf_ = fts[i % 2], tft[i % 2]
            P.dma("sp", ft, C["featsT"][:, i * 512:(i + 1) * 512], (), (tf_,))
            pb, tb = bank()
            mm(pb[0:64, :], w1, ft, True, True, (tM, tf_), (tb,))
            sin_layer(pb, tb, o1c, z1, tq)
            pb2, tb2 = bank()
            mm(pb2[0:64, :], w2, z1, True, True, (tM, tq), (tb2,))
            sin_layer(pb2, tb2, o2c, z2T[:, i * 512:(i + 1) * 512], tz)
        P.barrier()
        apos[0] = mark
        kT = alloc([128, 128, 32], BF16); tk = T()
        A = alloc([128, 32, 2, 128], BF16); tA = (T(), T())
        Kpk = alloc([128, 4096], BF16); tK = T()
        KA = alloc([128, 4096], BF16); KC = alloc([128, 4096], BF16); tKA = (T(), T())
        decs = [alloc([128, 4096], BF16) for _ in range(2)]; tdec = [T(), T()]
        ev = [0]
        for sc in range(32):
            c0 = sc * 32
            dec, tdec_ = decs[sc % 2], tdec[sc % 2]
            P.dma("sp", dec, C["DEC"][sc], (), (tdec_,))
            for g in range(8):
                pb, tb = bank()
                for j in range(16):
                    n2 = g * 16 + j
                    mm(pb[0:64, j * 32:(j + 1) * 32], z2T[:, n2:8192:128], w3b[:, c0:c0 + 32], True, True, (tz, tF), (tb,))
                    mm(pb[64:128, j * 32:(j + 1) * 32], z2T[:, 8192 + n2:NFFT:128], w3b[:, 1024 + c0:1024 + c0 + 32], True, True, (tz, tF), (tb,))
                tt("dve", kT[:, g * 16:(g + 1) * 16, :].rearrange("p a b -> p (a b)"), pb, dec[:, g * 512:(g + 1) * 512], ALU.mult, (tb, tdec_), (tk,))
            pbz, tbz = bank()
            mm(pbz[0:1, 0:32], z2T[:, 0:1], w3z[:, c0:c0 + 32], True, True, (tz, tF), (tbz,))
            cp("dve", kT[0:1, 0, :], pbz[0:1, 0:32], (tbz,), (tk,))
            fft_fwd(kT, 128, A, tA, F1, tF, (tk,), ev)
            for g in range(8):
                pb, tb = fft_s2(A, tA, L1, L2, tF, g)
                cp("act", Kpk[:, g * 512:(g + 1) * 512], pb, (tb,), (tK,))
            for g in range(8):
                pa, ta = bank(); pc, tc_ = bank()
                mm(pa, PA, Kpk[:, g * 512:(g + 1) * 512], True, True, (tF, tK), (ta,))
                mm(pc, PC, Kpk[:, g * 512:(g + 1) * 512], True, True, (tF, tK), (tc_,))
                cp("act", KA[:, g * 512:(g + 1) * 512], pa, (ta,), (tKA[0],))
                cp("dve", KC[:, g * 512:(g + 1) * 512], pc, (tc_,), (tKA[1],))
            P.dma("sp", KAC_d[sc, :, 0, :], KA, (tKA[0],), (tKAC,))
            P.dma("sp", KAC_d[sc, :, 1, :], KC, (tKA[1],), (tKAC,))

    phaseD1()
    reset_arena(PERSIST)
    if stop_after == "D1":
        return finish(nc, P, out_d, dbg)

    def phaseD2():
        NN1 = TOWN // 128
        tF, L1, L2, F1 = load_fft_tables()
        R1 = alloc([128, 256], BF16); R2 = alloc([128, 256], BF16)
        P.dma("sp", R1, C["R1"], (), (tF,)); P.dma("sp", R2, C["R2"], (), (tF,))
        HR = alloc([128, 128, NN1], BF16); HI = alloc([128, 128, NN1], BF16)
        P.dma("sp", HR.rearrange("p a b -> p (a b)"), C["HR"], (), (tF,))
        P.dma("sp", HI.rearrange("p a b -> p (a b)"), C["HI"], (), (tF,))
        U = alloc([128, L], BF16); tU = T()
        X0 = alloc([128, TOWN], BF16); tX0 = T()
        Ys = alloc([128, 128, NN1], BF16); tYs = T()
        mark = apos[0]
        ev = [0]
        NPB = 512 // NN1
        chunks = [(96 * i, 96) for i in range(10)] + [(960, 64)]
        for ci_, (row0, nr) in enumerate(chunks):
            apos[0] = mark
            PH = alloc([128, L + 2], BF16); tPH = T()
            TM = alloc([128, L], BF16); tTM = T()
            memset("dve", PH[0:nr, 0:1], 0.0, (tPH,))
            memset("dve", PH[0:nr, L + 1:L + 2], 0.0, (tPH,))
            dgb = alloc([128, 9, 96], BF16); tdg_ = T()
            for gi in range(3):
                for j in range(3):
                    ts("dve", dgb[0:nr, gi * 3 + j, 0:nr], identb[0:nr, 0:nr], hycw[0:nr, ci_, gi, j:j + 1], None, ALU.mult, None, (tC,), (tdg_,))
            cvi = 0
            for gi, (dst, tdst_, Lg) in enumerate(((X0, tX0, TOWN), (TM, tTM, L), (U, tU, L))):
                r_ = (2 + gi) * D + row0
                Lld = min(L, Lg + 1)
                P.dma("sp", PH[0:nr, 1:Lld + 1], PT_d[r_:r_ + nr, 0:Lld], (tPT,), (tPH,))
                for t5 in range(Lg // 512):
                    pb, tb = bank()
                    for j in range(3):
                        mm(pb[0:nr, :], dgb[0:nr, gi * 3 + j, 0:nr], PH[0:nr, t5 * 512 + j:t5 * 512 + j + 512], j == 0, j == 2, (tdg_, tPH), (tb,))
                    sl5 = slice(t5 * 512, (t5 + 1) * 512)
                    if gi < 2:
                        if cvi % 3 == 2:
                            ts("dve", dst[0:nr, sl5], pb[0:nr, :], hycb[0:nr, ci_, gi:gi + 1], None, ALU.add, None, (tb, tC), (tdst_,))
                        else:
                            act(dst[0:nr, sl5], pb[0:nr, :], AF.Identity, (tb, tC), (tdst_,), bias=hycb[0:nr, ci_, gi:gi + 1])
                        cvi += 1
                    else:
                        stt(U[0:nr, sl5], pb[0:nr, :], hycb[0:nr, ci_, gi:gi + 1], TM[0:nr, sl5], ALU.add, ALU.mult, (tb, tC, tTM), (tU,))
            P.barrier()
            apos[0] = mark
            UT = alloc([64, 128, 32], BF16); tUT = (T(), T())
            A = alloc([128, 32, 2, 128], BF16); tA = (T(), T())
            P1 = alloc([128, 128, 32], BF16); P2 = alloc([128, 128, 32], BF16); tP = T()
            KA = alloc([128, 128, 32], BF16); KC = alloc([128, 128, 32], BF16); tKA = T()
            Bv = alloc([128, 32, 2, 128], BF16); tB = (T(), T())

            def st_ka(s):
                sc = (row0 + 32 * s) // 32
                P.dma("sp", KA.rearrange("p a b -> p (a b)"), KAC_d[sc, :, 0, :], (tKAC,), (tKA,))
                P.dma("sp", KC.rearrange("p a b -> p (a b)"), KAC_d[sc, :, 1, :], (tKAC,), (tKA,))

            def st_trs1(s):
                for g in range(4):
                    pb, tb = bank()
                    pbb = pb.bitcast(BF16)
                    for j in range(32):
                        n2 = g * 32 + j
                        tr(pbb[0:64, j * 32:(j + 1) * 32], U[32 * s:32 * s + 32, n2:L:128], identb[32 * s:32 * s + 32, 32 * s:32 * s + 32], (tU, tC), (tb,))
                    cp("act" if g % 2 == 0 else "dve", UT[:, g * 32:(g + 1) * 32, :], pbb[0:64, :].rearrange("p (a b) -> p a b", a=32), (tb,), (tUT[g % 2],))
                fft_fwd(UT, 64, A, tA, F1, tF, tUT, ev)

            def st_s2(s):
                for g in range(8):
                    pb, tb = fft_s2(A, tA, L1, L2, tF, g)
                    pv = pb.rearrange("p (a b) -> p a b", a=16)
                    tt("dve", P1[:, g * 16:(g + 1) * 16, :], pv, KA[:, g * 16:(g + 1) * 16, :], ALU.mult, (tb, tKA), (tP,))
                    tt("dve", P2[:, g * 16:(g + 1) * 16, :], pv, KC[:, g * 16:(g + 1) * 16, :], ALU.mult, (tb, tKA), (tP,))

            def st_s1p(s):
                for c in range(0, 32, 2):
                    pb, tb = bank()
                    for h_ in range(2):
                        mm(pb[:, h_ * 256:(h_ + 1) * 256], P1[:, :, c + h_], R1, True, False, (tP, tF), (tb,))
                        mm(pb[:, h_ * 256:(h_ + 1) * 256], P2[:, :, c + h_], R2, False, True, (tP, tF), (tb,))
                    cp("act" if (ev[0] % 2 == 0) else "dve", Bv[:, c:c + 2, :, :].rearrange("p c a k -> p (c a k)"), pb, (tb,), (tB[ev[0] % 2],))
                    ev[0] += 1

            def st_s2p(s):
                rows = slice(32 * s, 32 * s + 32)
                for g in range(128 // NPB):
                    pb, tb = bank()
                    for j in range(NPB):
                        n2 = g * NPB + j
                        mm(pb[rows, j * NN1:(j + 1) * NN1], Bv[:, :, 0, n2], HR[:, n2, :], True, False, tB + (tF,), (tb,))
                        mm(pb[rows, j * NN1:(j + 1) * NN1], Bv[:, :, 1, n2], HI[:, n2, :], False, True, tB + (tF,), (tb,))
                    cp("act", Ys[rows, g * NPB:(g + 1) * NPB, :].rearrange("p a b -> p (a b)"), pb[rows, :], (tb,), (tYs,))

            ns_ = nr // 32
            st_ka(0); st_trs1(0); st_s2(0)
            for s in range(ns_):
                if s + 1 < ns_:
                    st_ka(s + 1)
                    st_trs1(s + 1)
                st_s1p(s)
                st_s2p(s)
                if s + 1 < ns_:
                    st_s2(s + 1)
            uo = U[0:nr, 0:TOWN]
            stt(uo.rearrange("p (n1 n2) -> p n1 n2", n2=128), uo.rearrange("p (n1 n2) -> p n1 n2", n2=128), hysk[0:nr, ci_:ci_ + 1],
                Ys[0:nr, :, :].rearrange("p n2 n1 -> p n1 n2"), ALU.mult, ALU.add, (tYs, tC, tU), (tU,))
            tt("dve", X0[0:nr, :], X0[0:nr, :], uo, ALU.mult, (tU, tX0), (tX0,))
            P.dma("sp", YHY_d[row0:row0 + nr, :], X0[0:nr, :], (tX0,), (tYHY,))
            P.barrier()

    phaseD2()
    reset_arena(PERSIST)
    if stop_after == "D2":
        return finish(nc, P, out_d, dbg)

    G1B = alloc([128, D], F32); G2B = alloc([128, D], F32); FGB = alloc([128, D], F32)
    P.dma("sp", FGB, fg_d.partition_broadcast(128), (), (tC,))
    dg = alloc([128, 128], F32); tdg = T()
    for (dst, gi) in ((G1B, 2), (G2B, 5)):
        for dc in range(NCH):
            ts("dve", dg, ident, modT[:, gi * 8 + dc, 0:1], None, ALU.mult, None, (tC,), (tdg,))
            pb2, tb2 = bank()
            mm(pb2[:, 0:128], ones, dg, True, True, (tC, tdg), (tb2,))
            cp("act", dst[:, dc * 128:(dc + 1) * 128], pb2[:, 0:128], (tb2,), (tC,))
    PERSIST2 = apos[0]

    def load_w(dram, rows_k, cols):
        w = alloc([128, rows_k, cols], BF16); tw = T()
        for k in range(rows_k):
            for c0 in range(0, cols, 1024):
                wd = min(1024, cols - c0)
                P.dma("pool", w[:, k, c0:c0 + wd], dram[k * 128:(k + 1) * 128, c0:c0 + wd], (), (tw,))
        return w, tw

    def phaseE():
        wrg, twrg = load_w(rgp_d, NCH, D)
        why, twhy = load_w(hyp_d, NCH, D)
        wo, two = load_w(wout_d, NCH, D)
        W = 512
        ins = [[alloc([128, NCH, W], BF16) for _ in range(4)] for _ in range(2)]
        tin = [T(), T()]
        mg = alloc([128, NCH, W], BF16); tmg = T()
        m1 = alloc([128, W], F32); m2 = alloc([128, W], F32); tm = T()
        xts = [alloc([128, D], F32) for _ in range(2)]; txs = [T(), T()]
        t1 = alloc([128, D], F32); tt1 = T()
        xi = 0
        for ti in range(TOWN // W):
            bufs, tb_in = ins[ti % 2], tin[ti % 2]
            tsl = slice(ti * W, (ti + 1) * W)
            for bi, (src, tsrc) in enumerate(((YRG_d, tYRG), (YHY_d, tYHY))):
                P.dma("sp", bufs[bi], src[:, tsl].rearrange("(k p) t -> p k t", p=128), (tsrc,), (tb_in,))
            for bi, g in ((2, 5), (3, 6)):
                P.dma("sp", bufs[bi], PT_d[g * D:(g + 1) * D, tsl].rearrange("(k p) t -> p k t", p=128), (tPT,), (tb_in,))
            for dc in range(NCH):
                pa, ta = bank(); ph, th_ = bank()
                for k in range(NCH):
                    mm(pa, wrg[:, k, dc * 128:(dc + 1) * 128], bufs[0][:, k, :], k == 0, k == NCH - 1, (twrg, tb_in), (ta,))
                for k in range(NCH):
                    mm(ph, why[:, k, dc * 128:(dc + 1) * 128], bufs[1][:, k, :], k == 0, k == NCH - 1, (twhy, tb_in), (th_,))
                tt("dve", m1, pa, bufs[2][:, dc, :], ALU.mult, (ta, tb_in), (tm,))
                tt("dve", m2, ph, bufs[3][:, dc, :], ALU.mult, (th_, tb_in), (tm,))
                tt("pool", mg[:, dc, :], m1, m2, ALU.add, (tm,), (tmg,))
            for a in range(W // 128):
                xt, tx = xts[xi % 2], txs[xi % 2]
                xi += 1
                r0 = ti * W + a * 128
                P.dma("sp", xt, x_d[r0:r0 + 128, :], (), (tx,))
                for half in range(2):
                    pb, tb = bank()
                    for k in range(NCH):
                        mm(pb, mg[:, k, a * 128:(a + 1) * 128], wo[:, k, half * 512:(half + 1) * 512], k == 0, k == NCH - 1, (tmg, two), (tb,))
                    tt("dve", t1[:, half * 512:(half + 1) * 512], pb, G1B[:, half * 512:(half + 1) * 512], ALU.mult, (tb, tC), (tt1,))
                tt("pool", xt, xt, t1, ALU.add, (tt1, tx), (tx,))
                P.dma("sp", X1_d[r0:r0 + 128, :], xt, (tx,), (tX1,))

    phaseE()
    reset_arena(PERSIST2)
    if stop_after == "E":
        return finish(nc, P, out_d, dbg)

    def phaseF():
        NT = TOWN // 128
        BS, NB = MOE_BS, MOE_NB
        PTOT = NB * BS
        XS_d = dscr("XS", [PTOT, D], BF16); YS_d = dscr("YS", [PTOT, D], BF16)
        tXS, tYS = T(), T()
        W1f = mw1_d.rearrange("e k f -> (e k) f"); W3f = mw3_d.rearrange("e k f -> (e k) f"); W2f = mw2_d.rearrange("e f d -> (e f) d")
        wge, twge = load_w(wge_d, NCH, 36)
        tK = T()
        tri = alloc([128, 128], F32); pkw = alloc([128, 8], F32); pkw2 = alloc([128, 4], F32); jbs = alloc([128, NB], F32)
        for dst, nm in ((tri, "tri"), (pkw, "pkw"), (pkw2, "pkw2"), (jbs, "jbs")):
            P.dma("sp", dst, C[nm], (), (tK,))
        A2B = alloc([128, D], F32); B2B = alloc([128, D], F32)
        dg2 = alloc([128, 128], F32); tdg2 = T()
        for (dst, col) in ((A2B, A2), (B2B, B2)):
            for dc in range(NCH):
                ts("dve", dg2, ident, col[:, dc:dc + 1], None, ALU.mult, None, (tC,), (tdg2,))
                pb2, tb2 = bank()
                mm(pb2[:, 0:128], ones, dg2, True, True, (tC, tdg2), (tb2,))
                cp("act", dst[:, dc * 128:(dc + 1) * 128], pb2[:, 0:128], (tb2,), (tK,))
        OH = alloc([128, NT, 64], F32); tOH = T()
        RK = alloc([128, NT, 2], F32); WTS = alloc([128, NT, 2], F32); tRK = T()
        DSTf = alloc([128, NT, 2], F32); DSTi = alloc([128, NT, 2], I32); tDST = T()
        S = alloc([128, 64], F32); tS = T()
        CNT = alloc([128, 64], F32); BASE = alloc([128, 64], F32); PEND = alloc([128, 32], F32); PADD = alloc([128, 32], F32)
        Z32 = alloc([128, 32], F32); tmp64 = alloc([128, 64], F32); ttmp = T()
        EB = alloc([128, NB], F32); EBs = alloc([128, NB], F32)
        IDX1f = alloc([128, NB, 8], F32); IDX1 = alloc([128, NB, 8], I32); IDX2f = alloc([128, NB, 4], F32); IDX2 = alloc([128, NB, 4], I32)
        tIDX = T()
        xts = [alloc([128, D], F32) for _ in range(2)]; txs = [T(), T()]
        junk = alloc([128, D], BF16); tj = T()
        sm = alloc([128, 96], F32); tsm = T()
        ss = alloc([128, 2], F32)
        hxT = alloc([128, NCH, 128], BF16); thx = T()
        tm32 = alloc([128, D], F32); ttm = T()
        mark = apos[0]
        HXTM = alloc([128, NT, D], BF16); tHX = T()
        memset("dve", S, 0.0, (tS,))
        memset("dve", Z32, 0.0, (ttmp,))
        for a in range(NT):
            xt, tx = xts[a % 2], txs[a % 2]
            r0 = a * 128
            P.dma("sp", xt, X1_d[r0:r0 + 128, :], (tX1,), (tx,))
            act(junk, xt, AF.Square, (tx,), (tj, tx), accum=ss[:, 0:1])
            act(ss[:, 0:1], ss[:, 0:1], AF.Sqrt, (tx,), (tx,), scale=1.0 / D, bias=EPS)
            P.op("dve", lambda h: h.reciprocal(out=ss[:, 0:1], in_=ss[:, 0:1]), (tx,), (tx,))
            ts("dve", xt, xt, ss[:, 0:1], None, ALU.mult, None, (tx,), (tx,))
            tt("pool", tm32, xt, A2B, ALU.mult, (tx, tK), (ttm,))
            tt("pool", HXTM[:, a, :], tm32, B2B, ALU.add, (ttm, tK), (tHX,))
            for dc in range(0, NCH, 4):
                pb, tb = bank()
                for j in range(4):
                    tr(pb[:, j * 128:(j + 1) * 128], xt[:, (dc + j) * 128:(dc + j + 1) * 128], ident, (tx, tC), (tb,))
                for j in range(4):
                    act(hxT[:, dc + j, :], pb[:, j * 128:(j + 1) * 128], AF.Identity, (tb, tC), (thx,),
                        bias=B2[:, dc + j:dc + j + 1], scale=A2[:, dc + j:dc + j + 1])
            pb, tb = bank()
            for k in range(NCH):
                mm(pb[:, 0:36], hxT[:, k, :], wge[:, k, :], k == 0, k == NCH - 1, (thx, twge), (tb,))
            lg = sm[:, 0:36]
            tt("dve", lg, pb[:, 0:36], bgeB, ALU.add, (tb, tC), (tsm,))
            gmax = sm[:, 36:37]; sg = sm[:, 37:38]; oh = sm[:, 38:42]; ein = sm[:, 42:50]; m8 = sm[:, 50:58]
            e4 = sm[:, 58:62]; ngm = sm[:, 62:63]; dd = sm[:, 63:64]; mk1 = sm[:, 64:72]; mk2 = sm[:, 72:80]
            P.op("dve", lambda h: h.tensor_reduce(out=gmax, in_=lg[:, 0:4], axis=AX.X, op=ALU.max), (tsm,), (tsm,))
            ts("dve", ngm, gmax, -1.0, None, ALU.mult, None, (tsm,), (tsm,))
            act(e4, lg[:, 0:4], AF.Exp, (tsm,), (tsm,), bias=ngm, accum=sg)
            P.op("dve", lambda h: h.reciprocal(out=sg, in_=sg), (tsm,), (tsm,))
            ts("dve", oh, lg[:, 0:4], gmax, None, ALU.is_equal, None, (tsm,), (tsm,))
            ts("dve", ein, lg[:, 4:12], oh[:, 0:1], None, ALU.mult, None, (tsm,), (tsm,))
            for g in range(1, 4):
                stt(ein, lg[:, 4 + 8 * g:12 + 8 * g], oh[:, g:g + 1], ein, ALU.mult, ALU.add, (tsm,), (tsm,))
            P.op("dve", lambda h: h.max(out=m8, in_=ein), (tsm,), (tsm,))
            tt("dve", dd, m8[:, 1:2], m8[:, 0:1], ALU.subtract, (tsm,), (tsm,))
            act(dd, dd, AF.Exp, (tsm,), (tsm,))
            ts("dve", e4[:, 0:1], dd, 1.0, None, ALU.add, None, (tsm,), (tsm,))
            P.op("dve", lambda h: h.reciprocal(out=e4[:, 0:1], in_=e4[:, 0:1]), (tsm,), (tsm,))
            tt("dve", e4[:, 1:2], dd, e4[:, 0:1], ALU.mult, (tsm,), (tsm,))
            tt("dve", WTS[:, a, 0:1], e4[:, 0:1], sg, ALU.mult, (tsm,), (tRK,))
            tt("dve", WTS[:, a, 1:2], e4[:, 1:2], sg, ALU.mult, (tsm,), (tRK,))
            ts("dve", mk1, ein, m8[:, 0:1], None, ALU.is_equal, None, (tsm,), (tsm,))
            ts("dve", mk2, ein, m8[:, 1:2], None, ALU.is_equal, None, (tsm,), (tsm,))
            for g in range(4):
                ts("dve", OH[:, a, g * 8:(g + 1) * 8], mk1, oh[:, g:g + 1], None, ALU.mult, None, (tsm,), (tOH,))
                ts("dve", OH[:, a, 32 + g * 8:32 + (g + 1) * 8], mk2, oh[:, g:g + 1], None, ALU.mult, None, (tsm,), (tOH,))
            pR, tR = bank()
            mm(pR[:, 0:64], tri, OH[:, a, :], True, False, (tK, tOH), (tR,))
            mm(pR[:, 0:64], ones, S, False, True, (tC, tS), (tR,))
            tt("dve", tmp64, pR[:, 0:64], OH[:, a, :], ALU.mult, (tR, tOH), (ttmp,))
            P.op("dve", lambda h, a=a: h.tensor_reduce(out=RK[:, a, :], in_=tmp64.rearrange("p (a b) -> p a b", a=2), axis=AX.X, op=ALU.add), (ttmp,), (tRK,))
            tt("pool", S, S, OH[:, a, :], ALU.add, (tOH, tS), (tS,))
        pT, tT_ = bank()
        mm(pT[:, 0:64], ones, S, True, True, (tC, tS), (tT_,))
        cp("dve", CNT, pT[:, 0:64], (tT_,), (ttmp,))
        tt("dve", PADD, CNT[:, 0:32], CNT[:, 32:64], ALU.add, (ttmp,), (ttmp,))
        ts("dve", PADD, PADD, 1.0 / BS, (BS - 1.0) / BS - 0.5 + 0.5 / BS, ALU.mult, ALU.add, (ttmp,), (ttmp,))
        cp("dve", DSTi[:, 0:16, :].rearrange("p a b -> p (a b)"), PADD, (ttmp,), (tDST,))
        cp("dve", PADD, DSTi[:, 0:16, :].rearrange("p a b -> p (a b)"), (tDST,), (ttmp,))
        ts("dve", PADD, PADD, float(BS), None, ALU.mult, None, (ttmp,), (ttmp,))
        P.op("dve", lambda h: h.tensor_tensor_scan(out=PEND, data0=PADD, data1=Z32, initial=0.0, op0=ALU.add, op1=ALU.add), (ttmp,), (ttmp,))
        tt("dve", BASE[:, 0:32], PEND, PADD, ALU.subtract, (ttmp,), (ttmp,))
        tt("dve", BASE[:, 32:64], BASE[:, 0:32], CNT[:, 0:32], ALU.add, (ttmp,), (ttmp,))
        for a in range(NT):
            tt("dve", tmp64, OH[:, a, :], BASE, ALU.mult, (tOH, ttmp), (ttmp,))
            P.op("dve", lambda h, a=a: h.tensor_reduce(out=DSTf[:, a, :], in_=tmp64.rearrange("p (a b) -> p a b", a=2), axis=AX.X, op=ALU.add), (ttmp,), (tDST,))
        tt("dve", DSTf.rearrange("p a b -> p (a b)"), DSTf.rearrange("p a b -> p (a b)"), RK.rearrange("p a b -> p (a b)"), ALU.add, (tDST, tRK), (tDST,))
        cp("dve", DSTi.rearrange("p a b -> p (a b)"), DSTf.rearrange("p a b -> p (a b)"), (tDST,), (tDST,))
        memset("dve", EB, 0.0, (tIDX,))
        for e in range(NE):
            stt(EB, jbs, PEND[:, e:e + 1], EB, ALU.is_ge, ALU.add, (tK, ttmp, tIDX), (tIDX,))
        ts("dve", EB, EB, float(NE - 1), None, ALU.min, None, (tIDX,), (tIDX,))
        ts("dve", EBs, EB, 1024.0, None, ALU.mult, None, (tIDX,), (tIDX,))
        for j in range(NB):
            ts("dve", IDX1f[:, j, :], pkw, EBs[:, j:j + 1], None, ALU.add, None, (tK, tIDX), (tIDX,))
        ts("dve", EBs, EB, 512.0, None, ALU.mult, None, (tIDX,), (tIDX,))
        for j in range(NB):
            ts("dve", IDX2f[:, j, :], pkw2, EBs[:, j:j + 1], None, ALU.add, None, (tK, tIDX), (tIDX,))
        cp("dve", IDX1.rearrange("p a b -> p (a b)"), IDX1f.rearrange("p a b -> p (a b)"), (tIDX,), (tIDX,))
        cp("dve", IDX2.rearrange("p a b -> p (a b)"), IDX2f.rearrange("p a b -> p (a b)"), (tIDX,), (tIDX,))
        for a in range(NT):
            for k in range(2):
                P.idma("pool", XS_d, HXTM[:, a, :], DSTi[:, a, k:k + 1], None, (tHX, tDST), (tXS,))
        P.barrier()
        apos[0] = mark
        w1s = [alloc([128, NCH, DE], BF16) for _ in range(2)]
        w3s = [alloc([128, NCH, DE], BF16) for _ in range(2)]
        w2s = [alloc([128, 4, D], BF16) for _ in range(2)]
        tws = [(T(), T()), (T(), T())]
        wstg = [alloc([128, DE], F32) for _ in range(12)]; twstg = [T() for _ in range(12)]
        wstg2 = [alloc([128, D], F32) for _ in range(4)]; twstg2 = [T() for _ in range(4)]
        wi2 = [0]
        wi = [0]
        mark3 = apos[0]
        NR = 3
        xss = [alloc([128, D], BF16) for _ in range(NR)]; txss = [T() for _ in range(NR)]
        xTs = [alloc([128, NCH, 128], BF16) for _ in range(NR)]; txT = [T() for _ in range(NR)]
        hids = [alloc([128, DE], BF16) for _ in range(NR)]; thid = [T() for _ in range(NR)]
        hTs = [alloc([128, 4, 128], BF16) for _ in range(NR)]; thT = [T() for _ in range(NR)]
        ybs = [alloc([128, D], BF16) for _ in range(NR)]; tyb = [(T(), T()) for _ in range(NR)]
        s1s = [alloc([128, DE], F32) for _ in range(2)]; ts1s = [T(), T()]

        def wpieces(jb, grp):
            w1, w3, w2, tw = w1s[jb % 2], w3s[jb % 2], w2s[jb % 2], tws[jb % 2]
            pcs = []
            for k in range(NCH):
                pcs.append((w1[:, k, :], W1f, IDX1[:, jb, k:k + 1], False))
                pcs.append((w3[:, k, :], W3f, IDX1[:, jb, k:k + 1], False))
            for k in range(4):
                pcs.append((w2[:, k, :], W2f, IDX2[:, jb, k:k + 1], True))
            for (dst, src, idx, big) in pcs[grp * 5:(grp + 1) * 5]:
                if big:
                    sg_, tsg_ = wstg2[wi2[0] % len(wstg2)], twstg2[wi2[0] % len(wstg2)]
                    wi2[0] += 1
                else:
                    sg_, tsg_ = wstg[wi[0] % len(wstg)], twstg[wi[0] % len(wstg)]
                wi[0] += 1
                P.idma("pool", sg_, src, None, idx, (tIDX,), (tsg_,))
                cp("act" if wi[0] % 2 == 0 else "dve", dst, sg_, (tsg_,), (tw[wi[0] % 2],))

        def stageA(s):
            jb, sb = s // 4, s % 4
            w1, w3, tw = w1s[jb % 2], w3s[jb % 2], tws[jb % 2]
            xs, txs_ = xss[s % NR], txss[s % NR]
            xT, txT_ = xTs[s % NR], txT[s % NR]
            hid, thid_ = hids[s % NR], thid[s % NR]
            s1, ts1 = s1s[s % 2], ts1s[s % 2]
            r0 = jb * BS + sb * 128
            P.dma("sp", xs, XS_d[r0:r0 + 128, :], (tXS,), (txs_,))
            pb, tb = bank()
            pbb = pb.bitcast(BF16)
            for k in range(NCH):
                tr(pbb[:, k * 128:(k + 1) * 128], xs[:, k * 128:(k + 1) * 128], identb, (txs_, tC), (tb,))
            cp("dve", xT.rearrange("p a b -> p (a b)"), pbb, (tb,), (txT_,))
            p1, tp1 = bank(); p3, tp3 = bank()
            for k in range(NCH):
                mm(p1, xT[:, k, :], w1[:, k, :], k == 0, k == NCH - 1, (txT_,) + tw, (tp1,))
            for k in range(NCH):
                mm(p3, xT[:, k, :], w3[:, k, :], k == 0, k == NCH - 1, (txT_,) + tw, (tp3,))
            act(s1, p1, AF.Silu, (tp1,), (ts1,))
            tt("dve", hid, s1, p3, ALU.mult, (ts1, tp3), (thid_,))

        def stageB(s):
            jb, sb = s // 4, s % 4
            w2, tw = w2s[jb % 2], tws[jb % 2]
            hid, thid_ = hids[s % NR], thid[s % NR]
            hT, thT_ = hTs[s % NR], thT[s % NR]
            yb, tyb_ = ybs[s % NR], tyb[s % NR]
            r0 = jb * BS + sb * 128
            pb2, tb2 = bank()
            pbb2 = pb2.bitcast(BF16)
            for f in range(4):
                tr(pbb2[:, f * 128:(f + 1) * 128], hid[:, f * 128:(f + 1) * 128], identb, (thid_, tC), (tb2,))
            cp("act", hT.rearrange("p a b -> p (a b)"), pbb2[:, 0:512], (tb2,), (thT_,))
            for half in range(2):
                py, tpy = bank()
                for f in range(4):
                    mm(py, hT[:, f, :], w2[:, f, half * 512:(half + 1) * 512], f == 0, f == 3, (thT_,) + tw, (tpy,))
                cp("act" if half == 0 else "dve", yb[:, half * 512:(half + 1) * 512], py, (tpy,), (tyb_[half],))
            P.dma("sp", YS_d[r0:r0 + 128, :], yb, tyb_, (tYS,))

        NS = NB * (BS // 128)
        for g in range(4):
            wpieces(0, g)
        for t in range(NS + 1):
            if t < NS:
                stageA(t)
            if t >= 1:
                stageB(t - 1)
            if t < NS:
                jn = t // 4 + 1
                if jn < NB:
                    wpieces(jn, t % 4)
        P.barrier()
        apos[0] = mark3
        y1s = [alloc([128, D], BF16) for _ in range(2)]; y2s = [alloc([128, D], BF16) for _ in range(2)]; tys = [T(), T()]
        mo = alloc([128, D], F32); tmo = T()
        for a in range(NT):
            y1, y2, ty = y1s[a % 2], y2s[a % 2], tys[a % 2]
            xt, tx = xts[a % 2], txs[a % 2]
            r0 = a * 128
            P.idma("pool", y1, YS_d, None, DSTi[:, a, 0:1], (tYS, tDST), (ty,))
            P.idma("pool", y2, YS_d, None, DSTi[:, a, 1:2], (tYS, tDST), (ty,))
            P.dma("sp", xt, X1_d[r0:r0 + 128, :], (tX1,), (tx,))
            ts("dve", mo, y1, WTS[:, a, 0:1], None, ALU.mult, None, (ty, tRK), (tmo,))
            stt(mo, y2, WTS[:, a, 1:2], mo, ALU.mult, ALU.add, (ty, tRK), (tmo,))
            tt("pool", mo, mo, G2B, ALU.mult, (tC,), (tmo,))
            tt("pool", xt, xt, mo, ALU.add, (tmo,), (tx,))
            act(junk, xt, AF.Square, (tx,), (tj, tx), accum=ss[:, 1:2])
            act(ss[:, 1:2], ss[:, 1:2], AF.Sqrt, (tx,), (tx,), scale=1.0 / D, bias=EPS)
            P.op("dve", lambda h: h.reciprocal(out=ss[:, 1:2], in_=ss[:, 1:2]), (tx,), (tx,))
            stt(xt, xt, ss[:, 1:2], FGB, ALU.mult, ALU.mult, (tx, tC), (tx,))
            P.dma("sp", out_d[r0:r0 + 128, :], xt, (tx,), (tX1,))

    phaseF()
    return finish(nc, P, out_d, dbg)


def finish(nc, P, out_d, dbg):
    P.barrier()
    P.emit()
    return nc


def make_inputs(inp, core):
    b, rev = core // 2, (core % 2 == 1)
    f = lambda a: np.ascontiguousarray(np.asarray(a, np.float32))
    m = {}
    xs = np.asarray(inp["x"][b], np.float32)
    cs = np.asarray(inp["ctx"][b], np.float32)
    m["x"] = f(xs[::-1] if rev else xs)
    m["ctx"] = f(cs[::-1] if rev else cs)
    cT = np.stack([fm(inp["c"][b]), fm(inp["c_ctx"])], axis=-1)
    m["cT"] = f(cT)
    m["ada_w"] = f(inp["ada_w"][0])
    m["ada_bT"] = fm(inp["ada_b"][0])
    m["g1nT"] = fm(inp["norm1_g"][0])
    m["g2nT"] = fm(inp["norm2_g"][0])
    m["final_g"] = f(inp["final_g"]).reshape(1, D)
    m["w_in"] = f(inp["w_in"][0])
    rw = np.asarray(inp["rg_conv_w"][0], np.float32)
    zero = np.zeros((D,), np.float32)
    taps5 = [zero, rw[3], rw[2], rw[1], rw[0]] if rev else [rw[0], rw[1], rw[2], rw[3], zero]
    m["rg_cwT"] = f(np.stack([fm(tp) for tp in taps5], axis=-1))
    m["rg_cbT"] = fm(inp["rg_conv_b"][0])
    bd = np.zeros((128, 4, NCH, 128), np.float32)
    gnames = ("rg_wa_b", "rg_wx_b", "rg_wa_f", "rg_wx_f") if rev else ("rg_wa_f", "rg_wx_f", "rg_wa_b", "rg_wx_b")
    bnames = ("rg_ba_b", "rg_bx_b", "rg_ba_f", "rg_bx_f") if rev else ("rg_ba_f", "rg_bx_f", "rg_ba_b", "rg_bx_b")
    lnames = ("rg_lam_b", "rg_lam_f") if rev else ("rg_lam_f", "rg_lam_b")
    for gi, nm in enumerate(gnames):
        w = np.asarray(inp[nm][0], np.float32)
        for cc in range(NCH):
            bd[0:64, gi, cc, 0:64] = w[2 * cc]
            bd[64:128, gi, cc, 64:128] = w[2 * cc + 1]
    m["rg_bd"] = bd
    m["rg_biasT"] = f(np.stack([fm(inp[nm][0]) for nm in bnames], axis=1))
    m["rg_lamT"] = f(np.stack([fm(inp[nm][0]) for nm in lnames], axis=1))
    m["rg_proj"] = f(inp["rg_proj"][0])
    def c96(v):
        o = np.zeros((11 * 96,), np.float32); o[:1024] = np.asarray(v, np.float32)
        return np.ascontiguousarray(o.reshape(11, 96).T)
    hw = np.asarray(inp["hy_conv_w"][0], np.float32); hb = np.asarray(inp["hy_conv_b"][0], np.float32)
    jt = (2, 1, 0) if rev else (0, 1, 2)
    m["hy_cw96"] = f(np.stack([np.stack([c96(hw[j, g * 1024:(g + 1) * 1024]) for j in jt], axis=-1) for g in range(3)], axis=2))
    m["hy_cb96"] = f(np.stack([c96(hb[g * 1024:(g + 1) * 1024]) for g in range(3)], axis=-1))
    m["hy_w1"] = f(inp["hy_pos_w1"][0])
    m["hy_b1T"] = f(inp["hy_pos_b1"][0]).reshape(64, 1)
    m["hy_w2"] = f(inp["hy_pos_w2"][0])
    m["hy_b2T"] = f(inp["hy_pos_b2"][0]).reshape(64, 1)
    m["hy_frT"] = f(inp["hy_freq"][0]).reshape(64, 1)
    w3 = np.asarray(inp["hy_pos_w3"][0], np.float32)
    m["hy_w3"] = f(np.concatenate([w3[:, 1024:], w3[:, :1024]], axis=1) if rev else w3)
    m["hy_w3z"] = f(w3[:, :1024])
    m["hy_sk96"] = c96(inp["hy_skip"][0])
    m["hy_proj"] = f(inp["hy_proj"][0])
    m["w_out"] = f(inp["w_out"][0])
    m["moe_wge"] = f(np.concatenate([inp["moe_wg"][0], inp["moe_we"][0]], axis=1))
    m["moe_bge"] = f(np.concatenate([inp["moe_bg"][0], inp["moe_be"][0]])).reshape(1, 36)
    m["moe_w1"] = f(inp["moe_w1"][0])
    m["moe_w3"] = f(inp["moe_w3"][0])
    m["moe_w2"] = f(inp["moe_w2"][0])
    for nm, arr in host_consts().items():
        m["k_" + nm] = arr
    return m


def kernel(**inputs):
    nc = build()
    in_maps = [make_inputs(inputs, c) for c in range(NCORES)]
    res = run_bass_kernel_spmd(nc, in_maps, core_ids=list(range(NCORES)))
    B = NCORES // 2
    out = np.empty((B, L, D), np.float32)
    for c in range(NCORES):
        r = np.asarray(res.results[c]["out"], np.float32)
        if c % 2 == 0:
            out[c // 2, 0:TOWN] = r
        else:
            out[c // 2, TOWN:L] = r[::-1]
    return out
```

```python
import numpy as np
import ml_dtypes
import concourse.bass as bass
import concourse.mybir as mybir
from concourse.bass_utils import run_bass_kernel_spmd

F32 = mybir.dt.float32
BF16 = mybir.dt.bfloat16
I32 = mybir.dt.int32
ALU = mybir.AluOpType
AF = mybir.ActivationFunctionType
AX = mybir.AxisListType

L = 8192
D = 1024
NCH = 8
NTT = L // 128
CTX = 256
NE = 32
DE = 512
NFFT = 16384
EPS = 1e-6
NCORES = 8
MOE_BS = 512
MOE_NB = 2 * 4096 // MOE_BS + 32
TOWN = 4096


class T:
    __slots__ = ("w", "r")

    def __init__(self):
        self.w = {}
        self.r = {}


class Eng:
    def __init__(self, name, key, sem):
        self.name = name
        self.key = key
        self.sem = sem
        self.count = 0
        self.waited = {}
        self.prog = []
        self.dsems = []
        self.dnext = 0


class Planner:
    def __init__(self, nc, ndma=8):
        self.nc = nc
        self.sems = []
        self.totals = []
        self.E = {}
        for name in ("pe", "act", "dve", "pool", "sp"):
            k = self._newsem(name)
            self.E[name] = Eng(name, k, self.sems[k])
        for q in ("sp", "pool", "act"):
            for i in range({"sp": 16, "pool": 24, "act": 2}[q]):
                self.E[q].dsems.append(self._newsem(f"d_{q}{i}"))

    def _newsem(self, name):
        self.sems.append(self.nc.alloc_semaphore(name=name))
        self.totals.append(0)
        return len(self.sems) - 1

    def _need(self, reads, writes):
        need = {}
        for t in reads:
            for k, v in t.w.items():
                if need.get(k, 0) < v:
                    need[k] = v
        for t in writes:
            for k, v in t.w.items():
                if need.get(k, 0) < v:
                    need[k] = v
            for k, v in t.r.items():
                if need.get(k, 0) < v:
                    need[k] = v
        return need

    def _emit_waits(self, E, need, skip_self=False):
        for k, v in need.items():
            if skip_self and k == E.key:
                continue
            if E.waited.get(k, 0) >= v:
                continue
            E.waited[k] = v
            E.prog.append(("w", k, v))

    def op(self, ename, fn, reads=(), writes=()):
        E = self.E[ename]
        need = self._need(reads, writes)
        self._emit_waits(E, need, skip_self=(ename == "pe"))
        E.count += 1
        E.prog.append(("o", fn, E.key))
        for t in reads:
            t.r[E.key] = E.count
        for t in writes:
            t.w[E.key] = E.count

    def dma(self, q, out, in_, reads=(), writes=()):
        E = self.E[q]
        need = self._need(reads, writes)
        k = E.dsems[E.dnext]
        E.dnext = (E.dnext + 1) % len(E.dsems)
        if self.totals[k] > 0:
            need[k] = max(need.get(k, 0), self.totals[k])
        self._emit_waits(E, need)
        self.totals[k] += 16
        E.prog.append(("d", out, in_, k))
        for t in reads:
            t.r[k] = self.totals[k]
        for t in writes:
            t.w[k] = self.totals[k]

    def idma(self, q, out, in_, out_off, in_off, reads=(), writes=(), bound=None):
        E = self.E[q]
        need = self._need(reads, writes)
        k = E.dsems[E.dnext]
        E.dnext = (E.dnext + 1) % len(E.dsems)
        if self.totals[k] > 0:
            need[k] = max(need.get(k, 0), self.totals[k])
        self._emit_waits(E, need)
        self.totals[k] += 16
        E.prog.append(("i", out, in_, out_off, in_off, k, bound))
        for t in reads:
            t.r[k] = self.totals[k]
        for t in writes:
            t.w[k] = self.totals[k]

    def barrier(self):
        need = {}
        for E in self.E.values():
            if E.count:
                need[E.key] = E.count
            for k in E.dsems:
                if self.totals[k]:
                    need[k] = self.totals[k]
        for E in self.E.values():
            n2 = {k: v for k, v in need.items() if k != E.key}
            self._emit_waits(E, n2)

    def emit(self):
        sems = self.sems

        def replay(E, h):
            regs = {}
            for it in E.prog:
                if it[0] == "w":
                    h.wait_ge(sems[it[1]], it[2])
                elif it[0] == "o":
                    it[1](h).then_inc(sems[it[2]], 1)
                elif it[0] == "i":
                    oo = None if it[3] is None else bass.IndirectOffsetOnAxis(ap=it[3], axis=0)
                    io = None if it[4] is None else bass.IndirectOffsetOnAxis(ap=it[4], axis=0)
                    if it[6] is None:
                        h.indirect_dma_start(out=it[1], out_offset=oo, in_=it[2], in_offset=io).then_inc(sems[it[5]], 16)
                    else:
                        if it[6] not in regs:
                            regs[it[6]] = h.to_reg(it[6])
                        h.indirect_dma_start(out=it[1], out_offset=oo, in_=it[2], in_offset=io, bounds_check=regs[it[6]], oob_is_err=False).then_inc(sems[it[5]], 16)
                else:
                    h.dma_start(out=it[1], in_=it[2]).then_inc(sems[it[3]], 16)

        with self.nc.Block() as block:
            @block.tensor
            def _(h):
                replay(self.E["pe"], h)

            @block.scalar
            def _(h):
                replay(self.E["act"], h)

            @block.vector
            def _(h):
                replay(self.E["dve"], h)

            @block.gpsimd
            def _(h):
                replay(self.E["pool"], h)

            @block.sync
            def _(h):
                replay(self.E["sp"], h)


def _bf(a):
    return np.ascontiguousarray(a.astype(np.float32)).astype(ml_dtypes.bfloat16)


_CONST = None


def host_consts():
    global _CONST
    if _CONST is not None:
        return _CONST
    N = NFFT
    c = {}
    c["ident"] = np.eye(128, dtype=np.float32)
    c["identb"] = _bf(np.eye(128))
    c["ones"] = np.ones((128, 128), np.float32)
    n1 = np.arange(128)[:, None].astype(np.float64)
    k1 = np.arange(128)[None, :].astype(np.float64)
    th = -2 * np.pi * n1 * (2 * k1 + 1) / 256.0
    c["F1"] = _bf(np.concatenate([np.cos(th), np.sin(th)], axis=1))
    n2 = np.arange(128)[:, None, None].astype(np.float64)
    kk1 = np.arange(128)[None, :, None].astype(np.float64)
    kk2 = np.arange(64)[None, None, :].astype(np.float64)
    ph = -2 * np.pi * n2 * (kk1 + 0.5 + 128 * kk2) / N
    Gre, Gim = np.cos(ph), np.sin(ph)
    c["L1"] = _bf(np.concatenate([Gre, Gim], axis=2).reshape(128, 128 * 128))
    c["L2"] = _bf(np.concatenate([-Gim, Gre], axis=2).reshape(128, 128 * 128))
    k2 = np.arange(64)[:, None].astype(np.float64)
    nn2 = np.arange(128)[None, :].astype(np.float64)
    cp = 2 * np.pi * k2 * nn2 / 128.0
    Cre, Cim = np.cos(cp), np.sin(cp)
    c["R1"] = _bf(np.concatenate([np.concatenate([Cre, Cim], 1), np.concatenate([-Cim, Cre], 1)], 0))
    c["R2"] = _bf(np.concatenate([np.concatenate([-Cim, Cre], 1), np.concatenate([Cre, Cim], 1)], 0))
    hk1 = np.arange(128)[:, None, None].astype(np.float64)
    hn2 = np.arange(128)[None, :, None].astype(np.float64)
    hn1 = np.arange(TOWN // 128)[None, None, :].astype(np.float64)
    hp = 2 * np.pi * (hk1 + 0.5) * (128 * hn1 + hn2) / N
    c["HR"] = _bf((2.0 / N) * np.cos(hp).reshape(128, 128 * (TOWN // 128)))
    c["HI"] = _bf(-(2.0 / N) * np.sin(hp).reshape(128, 128 * (TOWN // 128)))
    PA = np.zeros((128, 128)); PC = np.zeros((128, 128))
    for m in range(64):
        PA[m, m] = 1; PA[m, m + 64] = 1
        PC[64 + m, m] = 1; PC[64 + m, 64 + m] = -1
    c["PA"] = _bf(PA); c["PC"] = _bf(PC)
    n = np.arange(N)
    s = np.where(n <= 8192, n, N - n).astype(np.int64)
    s = np.where(n == 8192, 0, s)
    sf = s.astype(np.float32)
    t_norm = (sf / np.float32(L - 1)).astype(np.float32)
    bands = np.linspace(1e-4, 15, 16, dtype=np.float32)
    ang = (np.float32(2.0 * np.pi / L) * sf[:, None] * bands[None, :]).astype(np.float32)
    rows = L // 64
    row_lag = (s // 64).astype(np.float32) / np.float32(rows)
    colb = np.arange(1, 9, dtype=np.float32)
    cang = (np.float32(2.0 * np.pi / 64) * (s % 64).astype(np.float32)[:, None] * colb[None, :]).astype(np.float32)
    feats = np.concatenate([t_norm[:, None], np.cos(ang), np.sin(ang), row_lag[:, None],
                            np.cos(cang), np.sin(cang)], axis=-1).astype(np.float32)
    c["featsT"] = np.ascontiguousarray(feats.T)
    tn = t_norm.copy()
    tn[8192] = 1e4
    c["ntn2"] = np.ascontiguousarray((-tn).reshape(128, 128))
    mxd = np.log(1e-2) / 0.3
    mnd = np.log(1e-2) / 1.5
    deltas = np.abs(np.linspace(mnd, mxd, 1024, dtype=np.float32))
    c["DLT"] = np.ascontiguousarray(np.broadcast_to(deltas[None, :], (128, 1024))).astype(np.float32)
    dec = np.exp(-tn.astype(np.float32)[:, None] * deltas[None, :]).astype(np.float32)
    dec = dec.reshape(128, 128, 32, 32).transpose(2, 0, 1, 3).reshape(32, 128, 4096)
    c["DEC"] = _bf(dec)
    c["tri"] = np.triu(np.ones((128, 128), np.float32), k=1)
    pp = np.arange(128, dtype=np.float32)[:, None]
    c["pkw"] = np.ascontiguousarray(pp + 128.0 * np.arange(8, dtype=np.float32)[None, :])
    c["pkw2"] = np.ascontiguousarray(pp + 128.0 * np.arange(4, dtype=np.float32)[None, :])
    c["jbs"] = np.ascontiguousarray(np.broadcast_to((MOE_BS * np.arange(MOE_NB, dtype=np.float32))[None, :], (128, MOE_NB)))
    _CONST = c
    return c


def fm(v, nch=None):
    v = np.asarray(v, np.float32)
    return np.ascontiguousarray(v.reshape(-1, 128).T)


def build(debug=(), stop_after=None):
    nc = bass.Bass("TRN2", target_bir_lowering=False)
    P = Planner(nc)
    dbg = set(debug)

    def din(name, shape, dt=F32):
        return nc.dram_tensor(name, list(shape), dt, kind="ExternalInput").ap()

    def dscr(name, shape, dt):
        kind = "ExternalOutput" if name in dbg else "Internal"
        return nc.dram_tensor(name, list(shape), dt, kind=kind).ap()

    x_d = din("x", [L, D])
    ctx_d = din("ctx", [CTX, D])
    cT_d = din("cT", [128, NCH, 2])
    adaw_d = din("ada_w", [D, 6 * D])
    adab_d = din("ada_bT", [128, 48])
    g1n_d = din("g1nT", [128, NCH])
    g2n_d = din("g2nT", [128, NCH])
    fg_d = din("final_g", [1, D])
    win_d = din("w_in", [D, 7 * D])
    rgcw_d = din("rg_cwT", [128, NCH, 5])
    rgcb_d = din("rg_cbT", [128, NCH])
    rgbd_d = din("rg_bd", [128, 4, NCH, 128])
    rgbias_d = din("rg_biasT", [128, 4, NCH])
    rglam_d = din("rg_lamT", [128, 2, NCH])
    rgp_d = din("rg_proj", [D, D])
    hycw_d = din("hy_cw96", [96, 11, 3, 3])
    hycb_d = din("hy_cb96", [96, 11, 3])
    hyw1_d = din("hy_w1", [50, 64])
    hyb1_d = din("hy_b1T", [64, 1])
    hyw2_d = din("hy_w2", [64, 64])
    hyb2_d = din("hy_b2T", [64, 1])
    hyfr_d = din("hy_frT", [64, 1])
    hyw3_d = din("hy_w3", [64, 2048])
    hyw3z_d = din("hy_w3z", [64, 1024])
    hysk_d = din("hy_sk96", [96, 11])
    hyp_d = din("hy_proj", [D, D])
    wout_d = din("w_out", [D, D])
    wge_d = din("moe_wge", [D, 36])
    bge_d = din("moe_bge", [1, 36])
    mw1_d = din("moe_w1", [NE, D, DE])
    mw3_d = din("moe_w3", [NE, D, DE])
    mw2_d = din("moe_w2", [NE, DE, D])
    C = {}
    hc = host_consts()
    for nm, arr in hc.items():
        C[nm] = din("k_" + nm, arr.shape, BF16 if arr.dtype == ml_dtypes.bfloat16 else F32)
    out_d = nc.dram_tensor("out", [TOWN, D], F32, kind="ExternalOutput").ap()

    PT_d = dscr("PT", [7 * D, L], BF16)
    YRG_d = dscr("YRG", [D, TOWN], BF16)
    YHY_d = dscr("YHY", [D, TOWN], BF16)
    X1_d = dscr("X1", [TOWN, D], F32)
    KAC_d = dscr("KAC", [32, 128, 2, 4096], BF16)
    tPT, tYRG, tYHY, tX1, tKAC = T(), T(), T(), T(), T()

    banks = []
    for i in range(8):
        banks.append((nc.alloc_psum_tensor(f"ps{i}", [128, 512], F32)[:, :], T()))
    bstate = [0]

    def bank():
        b = banks[bstate[0] % 8]
        bstate[0] += 1
        return b

    ARENA = 196608 - 2048
    arena = nc.alloc_sbuf_tensor("arena", [128, ARENA // 2], BF16)
    apos = [0]

    def alloc(shape, dt):
        esz = 4 if dt in (F32, I32) else 2
        n = int(np.prod(shape[1:]))
        nbytes = (n * esz + 63) // 64 * 64
        off = apos[0]
        apos[0] += nbytes
        assert apos[0] <= ARENA, f"SBUF overflow {apos[0]}"
        ap = arena[0:shape[0], off // 2: off // 2 + n * esz // 2]
        if esz == 4:
            ap = ap.bitcast(dt)
        if len(shape) > 2:
            names = " ".join(f"a{i}" for i in range(len(shape) - 1))
            kw = {f"a{i}": int(shape[i + 1]) for i in range(len(shape) - 1)}
            ap = ap.rearrange(f"p ({names}) -> p {names}", **kw)
        return ap

    def reset_arena(to=0):
        P.barrier()
        apos[0] = to

    def act(out, in_, func, r, w, bias=None, scale=None, accum=None, eng="act"):
        kw = {}
        if bias is not None:
            kw["bias"] = bias
        if scale is not None:
            kw["scale"] = scale
        if accum is not None:
            kw["accum_out"] = accum
        P.op("act", lambda h: h.activation(out=out, in_=in_, func=func, **kw), r, w)

    def ts(eng, out, in0, s1, s2, op0, op1, r, w):
        if op1 is None:
            P.op(eng, lambda h: h.tensor_scalar(out=out, in0=in0, scalar1=s1, scalar2=None, op0=op0), r, w)
        else:
            P.op(eng, lambda h: h.tensor_scalar(out=out, in0=in0, scalar1=s1, scalar2=s2, op0=op0, op1=op1), r, w)

    def stt(out, in0, sc, in1, op0, op1, r, w):
        P.op("dve", lambda h: h.scalar_tensor_tensor(out=out, in0=in0, scalar=sc, in1=in1, op0=op0, op1=op1), r, w)

    def tt(eng, out, in0, in1, op, r, w):
        P.op(eng, lambda h: h.tensor_tensor(out=out, in0=in0, in1=in1, op=op), r, w)

    def cp(eng, out, in_, r, w):
        if eng == "act":
            P.op("act", lambda h: h.activation(out=out, in_=in_, func=AF.Copy), r, w)
        else:
            P.op(eng, lambda h: h.tensor_copy(out=out, in_=in_), r, w)

    def mm(out, lhsT, rhs, start, stop, r, w):
        P.op("pe", lambda h: h.matmul(out, lhsT, rhs, start=start, stop=stop), r, w)

    def tr(out, in_, ident, r, w):
        P.op("pe", lambda h: h.transpose(out, in_, ident), r, w)

    def memset(eng, ap, val, w):
        P.op(eng, lambda h: h.memset(ap, val), (), w)

    tC = T()
    ident = alloc([128, 128], F32)
    identb = alloc([128, 128], BF16)
    ones = alloc([128, 128], F32)
    modT = alloc([128, 48, 2], F32)
    A1 = alloc([128, NCH], F32); B1 = alloc([128, NCH], F32)
    A1c = alloc([128, NCH], F32); B1c = alloc([128, NCH], F32)
    A2 = alloc([128, NCH], F32); B2 = alloc([128, NCH], F32)
    pass
    rgcw = alloc([128, NCH, 5], F32); rgcb = alloc([128, NCH], F32)
    rgbias = alloc([128, 4, NCH], F32); nrgbias = alloc([128, 4, NCH], F32)
    rglam = alloc([128, 2, NCH], F32)
    sa1 = alloc([128, 2, NCH], F32); sa2 = alloc([128, 2, NCH], F32)
    hycw = alloc([96, 11, 3, 3], F32); hycb = alloc([96, 11, 3], F32); hysk = alloc([96, 11], F32)
    H0 = alloc([128, 2, NCH], F32)
    bgeB = alloc([128, 36], F32)
    PERSIST = apos[0]

    P.dma("sp", ident, C["ident"], (), (tC,))
    P.dma("sp", identb, C["identb"], (), (tC,))
    P.dma("sp", ones, C["ones"], (), (tC,))
    for dst, src in ((rgcw, rgcw_d), (rgcb, rgcb_d), (rgbias, rgbias_d), (rglam, rglam_d),
                     (hycw, hycw_d), (hycb, hycb_d), (hysk, hysk_d)):
        P.dma("sp", dst, src, (), (tC,))
    P.dma("sp", bgeB, bge_d.partition_broadcast(128), (), (tC,))

    def phaseA():
        cT = alloc([128, NCH, 2], F32)
        scT = alloc([128, NCH, 2], F32)
        adab = alloc([128, 48], F32)
        g1n = alloc([128, NCH], F32); g2n = alloc([128, NCH], F32)
        tmp = alloc([128, NCH], F32)
        dg = alloc([128, 128], F32)
        wbuf = [alloc([128, 1536], F32) for _ in range(3)]
        tw = [T() for _ in range(3)]
        tS = T()
        P.dma("sp", cT, cT_d, (), (tS,))
        P.dma("sp", adab, adab_d, (), (tS,))
        P.dma("sp", g1n, g1n_d, (), (tS,))
        P.dma("sp", g2n, g2n_d, (), (tS,))
        act(scT, cT, AF.Silu, (tS,), (tS,))
        pb, tb = bank()
        i = 0
        for k in range(NCH):
            for q in range(4):
                wb, twb = wbuf[i % 3], tw[i % 3]
                i += 1
                P.dma("sp", wb, adaw_d[k * 128:(k + 1) * 128, q * 1536:(q + 1) * 1536], (), (twb,))
                for jj in range(12):
                    j = q * 12 + jj
                    mm(pb[:, 2 * j:2 * j + 2], wb[:, jj * 128:(jj + 1) * 128], scT[:, k, :],
                       (k == 0 and j == 0), (k == NCH - 1 and j == 47), (twb, tS), (tb,))
        for col in range(2):
            tt("dve", modT[:, :, col], pb[:, col:96:2], adab, ALU.add, (tb, tS), (tC,))
        for (Ad, Bd, gn, sci, shi, col) in ((A1, B1, g1n, 1, 0, 0), (A1c, B1c, g1n, 1, 0, 1), (A2, B2, g2n, 4, 3, 0)):
            ts("dve", tmp, modT[:, sci * 8:(sci + 1) * 8, col], 1.0, None, ALU.add, None, (tC,), (tS,))
            tt("dve", Ad, tmp, gn, ALU.mult, (tS,), (tC,))
            cp("dve", Bd, modT[:, shi * 8:(shi + 1) * 8, col], (tC,), (tC,))
        act(sa1, rglam, AF.Exp, (tC,), (tC,), scale=-1.0)
        act(sa1, sa1, AF.Ln, (tC,), (tC,), bias=1.0)
        ts("dve", sa2, sa1, -16.0, None, ALU.mult, None, (tC,), (tC,))
        ts("dve", sa1, sa1, -8.0, None, ALU.mult, None, (tC,), (tC,))
        ts("dve", nrgbias, rgbias, -1.0, None, ALU.mult, None, (tC,), (tC,))

    phaseA()
    reset_arena(PERSIST)

    def phaseB(src_d, ntok, W, ccs_all, ccs_fn, Asc, Bsc, dst_d, tdst, sig_from=40):
        nsub = W // 128
        ncol = len(ccs_all)
        pos = {cc: i for i, cc in enumerate(ccs_all)}
        winb = alloc([128, NCH, ncol * 128], BF16)
        tWs = [T() for _ in range((ncol + 7) // 8)]
        c0 = ccs_all[0] * 128
        for blk in range(0, ncol * 128, 1024):
            wd = min(1024, ncol * 128 - blk)
            for k in range(NCH):
                P.dma("pool", winb[:, k, blk:blk + wd], win_d[k * 128:(k + 1) * 128, c0 + blk:c0 + blk + wd], (), (tWs[blk // 1024],))
        xts = [alloc([128, nsub, D], F32) for _ in range(2)]
        txs = [T(), T()]
        junk = alloc([128, D], BF16); tj = T()
        ss = [alloc([128, nsub], F32) for _ in range(2)]
        hxs = [alloc([128, NCH, W], BF16) for _ in range(2)]
        ths = [T(), T()]
        stg = [alloc([128, 4, W], BF16) for _ in range(3)]
        tst = [(T(), T()) for _ in range(3)]
        si = 0
        for ti in range(ntok // W):
            ccs = ccs_fn(ti)
            xt, tx, s_, hx, th = xts[ti % 2], txs[ti % 2], ss[ti % 2], hxs[ti % 2], ths[ti % 2]
            P.dma("sp", xt, src_d[ti * W:(ti + 1) * W, :].rearrange("(a p) d -> p a d", p=128), (), (tx,))
            for a in range(nsub):
                act(junk, xt[:, a, :], AF.Square, (tx,), (tj, tx), accum=s_[:, a:a + 1])
            act(s_, s_, AF.Sqrt, (tx,), (tx,), scale=1.0 / D, bias=EPS)
            P.op("dve", lambda h, s_=s_: h.reciprocal(out=s_, in_=s_), (tx,), (tx,))
            for a in range(nsub):
                ts("dve" if a % 2 == 0 else "pool", xt[:, a, :], xt[:, a, :], s_[:, a:a + 1], None, ALU.mult, None, (tx,), (tx,))
            for dc in range(NCH):
                pb, tb = bank()
                for a in range(nsub):
                    tr(pb[:, a * 128:(a + 1) * 128], xt[:, a, dc * 128:(dc + 1) * 128], ident, (tx, tC), (tb,))
                act(hx[:, dc, :], pb[:, 0:W], AF.Identity, (tb, tC), (th,), bias=Bsc[:, dc:dc + 1], scale=Asc[:, dc:dc + 1])
            for ci, cc in enumerate(ccs):
                pb, tb = bank()
                wi = pos[cc]
                for k in range(NCH):
                    mm(pb[:, 0:W], winb[:, k, wi * 128:(wi + 1) * 128], hx[:, k, :], k == 0, k == NCH - 1, (tWs[wi // 8], th), (tb,))
                sg, tsg = stg[si % 3], tst[si % 3]
                if cc >= sig_from:
                    act(sg[:, ci % 4, :], pb[:, 0:W], AF.Sigmoid, (tb,), (tsg[0],))
                elif ci % 2 == 0:
                    cp("act", sg[:, ci % 4, :], pb[:, 0:W], (tb,), (tsg[0],))
                else:
                    cp("dve", sg[:, ci % 4, :], pb[:, 0:W], (tb,), (tsg[1],))
                if ci % 4 == 3:
                    r0 = ccs[ci - 3] * 128
                    P.dma("sp", dst_d[r0:r0 + 512, ti * W:(ti + 1) * W].rearrange("(a p) t -> p a t", p=128), sg, tsg, (tdst,))
                    si += 1

    NOWN5 = TOWN // 512
    CC_ALL = list(range(56))
    CC_REST = list(range(0, 8)) + list(range(24, 40))
    CC_HALO = CC_REST + list(range(16, 24))

    def ccs_main(ti):
        if ti < NOWN5:
            return CC_ALL
        if ti == NOWN5:
            return CC_HALO
        return CC_REST

    phaseB(x_d, L, 512, CC_ALL, ccs_main, A1, B1, PT_d, tPT)
    reset_arena(PERSIST)
    if stop_after == "B":
        return finish(nc, P, out_d, dbg)

    PTC_d = dscr("PTC", [D, CTX], BF16)
    tPTC = T()
    phaseB(ctx_d, CTX, 256, list(range(8)), lambda ti: list(range(8)), A1c, B1c, PTC_d, tPTC)
    reset_arena(PERSIST)

    def phaseC(src_d, tsrc, Lt, Lown, TW, is_ctx):
        ntile = Lt // TW
        nown = Lown // TW
        nq = TW // 512 if TW >= 512 else 1
        QW = min(512, TW)
        wbd = alloc([128, 4, NCH, 128], BF16); tWb = T()
        P.dma("pool", wbd.rearrange("p a b c -> p (a b c)"), rgbd_d.rearrange("p a b c -> p (a b c)"), (), (tWb,))
        PX = alloc([128, Lt + 4], BF16); tPX = T()
        xc = alloc([128, Lt], BF16); txc = T()
        dgc = alloc([128, 5, 128], BF16); tdgc = T()
        if not is_ctx:
            PRG = alloc([128, Lown], BF16); tPRG = T()
            HB = alloc([128, Lown], BF16); tHB = T()
            g1 = alloc([128, TW], F32); g2 = alloc([128, TW], F32); tg = T()
            yo = [alloc([128, TW], BF16) for _ in range(2)]; tyo = [T(), T()]
        NG = 2
        rts = [alloc([128, TW], F32) for _ in range(NG)]
        ats = [alloc([128, TW], F32) for _ in range(NG)]
        its = [alloc([128, TW], F32) for _ in range(NG)]
        tgts = [T() for _ in range(NG)]
        hts = [alloc([128, TW], F32) for _ in range(2)]; tht = [T(), T()]
        memset("dve", PX[:, 0:2], 0.0, (tPX,))
        memset("dve", PX[:, Lt + 2:Lt + 4], 0.0, (tPX,))
        hcount = 0
        yi = 0
        gi_ = 0
        for cc in range(NCH):
            P.dma("sp", PX[:, 2:Lt + 2], src_d[cc * 128:(cc + 1) * 128, 0:Lt], (tsrc,), (tPX,))
            if not is_ctx:
                P.dma("sp", PRG, src_d[D + cc * 128:D + (cc + 1) * 128, 0:Lown], (tsrc,), (tPRG,))
            for j in range(5):
                ts("pool", dgc[:, j, :], identb, rgcw[:, cc, j:j + 1], None, ALU.mult, None, (tC,), (tdgc,))
            CW = min(512, Lt)
            for t5 in range(Lt // CW):
                pbc, tbc = bank()
                for j in range(5):
                    mm(pbc[:, 0:CW], dgc[:, j, :], PX[:, t5 * CW + j:t5 * CW + j + CW], j == 0, j == 4, (tdgc, tPX), (tbc,))
                ts("dve", xc[:, t5 * CW:(t5 + 1) * CW], pbc[:, 0:CW], rgcb[:, cc:cc + 1], None, ALU.add, None, (tbc, tC), (txc,))
            for d in (1, 0):
                order = range(ntile - 1, -1, -1) if d == 1 else range(nown)
                first = True
                for ti in order:
                    sl = slice(ti * TW, (ti + 1) * TW)
                    rt, at, it, tgt = rts[gi_ % NG], ats[gi_ % NG], its[gi_ % NG], tgts[gi_ % NG]
                    gi_ += 1
                    prs, pis = [], []
                    for q in range(nq):
                        pr, tr_ = bank(); pi, ti_ = bank()
                        mm(pr[:, 0:QW], wbd[:, 2 * d, cc, :], xc[:, ti * TW + q * QW: ti * TW + (q + 1) * QW], True, True, (tWb, txc), (tr_,))
                        mm(pi[:, 0:QW], wbd[:, 2 * d + 1, cc, :], xc[:, ti * TW + q * QW: ti * TW + (q + 1) * QW], True, True, (tWb, txc), (ti_,))
                        prs.append((pr, tr_)); pis.append((pi, ti_))
                    for q in range(nq):
                        act(rt[:, q * QW:(q + 1) * QW], prs[q][0][:, 0:QW], AF.Sigmoid, (prs[q][1], tC), (tgt,), bias=rgbias[:, 2 * d, cc:cc + 1])
                    for q in range(nq):
                        act(it[:, q * QW:(q + 1) * QW], pis[q][0][:, 0:QW], AF.Sigmoid, (pis[q][1], tC), (tgt,), bias=rgbias[:, 2 * d + 1, cc:cc + 1])
                    act(at, rt, AF.Exp, (tgt, tC), (tgt,), scale=sa1[:, d, cc:cc + 1])
                    act(rt, rt, AF.Exp, (tgt, tC), (tgt,), scale=sa2[:, d, cc:cc + 1])
                    act(rt, rt, AF.Sqrt, (tgt,), (tgt,), scale=-1.0, bias=1.0)
                    tt("pool", it, it, xc[:, sl], ALU.mult, (tgt, txc), (tgt,))
                    tt("dve", rt, rt, it, ALU.mult, (tgt,), (tgt,))
                    ht, th_ = hts[hcount % 2], tht[hcount % 2]
                    hp, thp = hts[(hcount + 1) % 2], tht[(hcount + 1) % 2]
                    hcount += 1
                    if first:
                        init = 0.0 if is_ctx else H0[:, d, cc:cc + 1]
                        rd = (tgt,) if is_ctx else (tgt, tC)
                    else:
                        init = hp[:, 0:1] if d == 1 else hp[:, TW - 1:TW]
                        rd = (tgt, thp)
                    first = False
                    if d == 1:
                        P.op("dve", lambda h, ht=ht, init=init, at=at, rt=rt: h.tensor_tensor_scan(out=ht[:, ::-1], data0=at[:, ::-1], data1=rt[:, ::-1], initial=init, op0=ALU.mult, op1=ALU.add), rd, (th_,))
                    else:
                        P.op("dve", lambda h, ht=ht, init=init, at=at, rt=rt: h.tensor_tensor_scan(out=ht, data0=at, data1=rt, initial=init, op0=ALU.mult, op1=ALU.add), rd, (th_,))
                    if is_ctx:
                        last = (ti == 0) if d == 1 else (ti == ntile - 1)
                        if last:
                            col = ht[:, 0:1] if d == 1 else ht[:, TW - 1:TW]
                            cp("dve", H0[:, d, cc:cc + 1], col, (th_,), (tC,))
                        continue
                    if d == 1:
                        if ti < nown:
                            cp("act", HB[:, sl], ht, (th_,), (tHB,))
                    else:
                        xg = PRG[:, sl]
                        tt("pool", g1, xg, xg, ALU.mult, (tPRG,), (tg,))
                        ts("pool", g1, g1, 0.044715, 1.0, ALU.mult, ALU.add, (tg,), (tg,))
                        tt("pool", g1, g1, xg, ALU.mult, (tg, tPRG), (tg,))
                        act(g1, g1, AF.Sigmoid, (tg,), (tg,), scale=1.5957691216057308)
                        tt("pool", g1, g1, xg, ALU.mult, (tg, tPRG), (tg,))
                        tt("dve", g2, ht, HB[:, sl], ALU.add, (th_, tHB), (tg,))
                        y, ty = yo[yi % 2], tyo[yi % 2]
                        yi += 1
                        tt("dve", y, g2, g1, ALU.mult, (tg,), (ty,))
                        P.dma("sp", YRG_d[cc * 128:(cc + 1) * 128, sl], y, (ty,), (tYRG,))

    phaseC(PTC_d, tPTC, CTX, CTX, 256, True)
    reset_arena(PERSIST)
    phaseC(PT_d, tPT, L, TOWN, 2048, False)
    reset_arena(PERSIST)
    if stop_after == "C":
        return finish(nc, P, out_d, dbg)

    def load_fft_tables():
        tF = T()
        L1 = alloc([128, 16384], BF16); L2 = alloc([128, 16384], BF16)
        for q in range(4):
            P.dma("sp", L1[:, q * 4096:(q + 1) * 4096], C["L1"][:, q * 4096:(q + 1) * 4096], (), (tF,))
            P.dma("sp", L2[:, q * 4096:(q + 1) * 4096], C["L2"][:, q * 4096:(q + 1) * 4096], (), (tF,))
        F1 = alloc([128, 256], BF16)
        P.dma("sp", F1, C["F1"], (), (tF,))
        return tF, L1, L2, F1

    def fft_fwd(src, Krows, A, tA, F1, tF, tsrc, ev):
        for c in range(0, 32, 2):
            pb, tb = bank()
            for h_ in range(2):
                mm(pb[:, h_ * 256:(h_ + 1) * 256], src[0:Krows, :, c + h_], F1[0:Krows, :], True, True, tuple(tsrc) + (tF,), (tb,))
            cp("act" if (ev[0] % 2 == 0) else "dve", A[:, c:c + 2, :, :].rearrange("p c a k -> p (c a k)"), pb, (tb,), (tA[ev[0] % 2],))
            ev[0] += 1

    def fft_s2(A, tA, L1, L2, tF, g):
        pb, tb = bank()
        for j in range(16):
            k1 = g * 16 + j
            mm(pb[:, j * 32:(j + 1) * 32], L1[:, k1 * 128:(k1 + 1) * 128], A[:, :, 0, k1], True, False, (tF,) + tuple(tA), (tb,))
            mm(pb[:, j * 32:(j + 1) * 32], L2[:, k1 * 128:(k1 + 1) * 128], A[:, :, 1, k1], False, True, (tF,) + tuple(tA), (tb,))
        return pb, tb

    def phaseD1():
        tF, L1, L2, F1 = load_fft_tables()
        PA = alloc([128, 128], BF16); PC = alloc([128, 128], BF16)
        P.dma("sp", PA, C["PA"], (), (tF,)); P.dma("sp", PC, C["PC"], (), (tF,))
        z2T = alloc([64, NFFT], BF16); tz = T()
        w3b = alloc([64, 2048], BF16); w3z = alloc([64, 1024], BF16)
        DLT = alloc([128, 1024], F32); ntn2 = alloc([128, 128], F32)
        P.dma("sp", DLT, C["DLT"], (), (tF,)); P.dma("sp", ntn2, C["ntn2"], (), (tF,))
        P.dma("pool", w3b, hyw3_d, (), (tF,))
        P.dma("pool", w3z, hyw3z_d, (), (tF,))
        ts("dve", w3b[:, 1024:2048], w3b[:, 1024:2048], -1.0, None, ALU.mult, None, (tF,), (tF,))
        mark = apos[0]
        w1 = alloc([50, 64], F32); w2 = alloc([64, 64], F32)
        b1 = alloc([64, 1], F32); b2 = alloc([64, 1], F32); fr = alloc([64, 1], F32)
        s1c = alloc([64, 1], F32); o1c = alloc([64, 1], F32); o2c = alloc([64, 1], F32)
        tM = T()
        for dst, src in ((w1, hyw1_d), (w2, hyw2_d), (b1, hyb1_d), (b2, hyb2_d), (fr, hyfr_d)):
            P.dma("sp", dst, src, (), (tM,))
        TWO_PI = 2.0 * np.pi
        ts("dve", s1c, fr, 1.0 / TWO_PI, None, ALU.mult, None, (tM,), (tM,))
        tt("dve", o1c, s1c, b1, ALU.mult, (tM,), (tM,))
        ts("dve", o1c, o1c, 8.0, None, ALU.add, None, (tM,), (tM,))
        tt("dve", o2c, s1c, b2, ALU.mult, (tM,), (tM,))
        ts("dve", o2c, o2c, 8.0, None, ALU.add, None, (tM,), (tM,))
        fts = [alloc([50, 512], F32) for _ in range(2)]; tft = [T(), T()]
        q_ = alloc([64, 512], F32); qi = alloc([64, 512], I32); qf = alloc([64, 512], F32); z1 = alloc([64, 512], F32)
        tq = T()

        def sin_layer(pb, tb, oc, out, tout):
            ts("dve", q_, pb[0:64, :], s1c, oc, ALU.mult, ALU.add, (tb, tM), (tq,))
            cp("dve", qi, q_, (tq,), (tq,))
            cp("dve", qf, qi, (tq,), (tq,))
            tt("dve", q_, q_, qf, ALU.subtract, (tq,), (tq,))
            act(out, q_, AF.Sin, (tq,), (tout,), scale=TWO_PI)

        for i in range(NFFT // 512):
            ft, tf_ = fts[i % 2], tft[i % 2]
            P.dma("sp", ft, C["featsT"][:, i * 512:(i + 1) * 512], (), (tf_,))
            pb, tb = bank()
            mm(pb[0:64, :], w1, ft, True, True, (tM, tf_), (tb,))
            sin_layer(pb, tb, o1c, z1, tq)
            pb2, tb2 = bank()
            mm(pb2[0:64, :], w2, z1, True, True, (tM, tq), (tb2,))
            sin_layer(pb2, tb2, o2c, z2T[:, i * 512:(i + 1) * 512], tz)
        P.barrier()
        apos[0] = mark
        kT = alloc([128, 128, 32], BF16); tk = T()
        A = alloc([128, 32, 2, 128], BF16); tA = (T(), T())
        Kpk = alloc([128, 4096], BF16); tK = T()
        KA = alloc([128, 4096], BF16); KC = alloc([128, 4096], BF16); tKA = (T(), T())
        decs = [alloc([128, 4096], BF16) for _ in range(2)]; tdec = [T(), T()]
        ev = [0]
        for sc in range(32):
            c0 = sc * 32
            dec, tdec_ = decs[sc % 2], tdec[sc % 2]
            P.dma("sp", dec, C["DEC"][sc], (), (tdec_,))
            for g in range(8):
                pb, tb = bank()
                for j in range(16):
                    n2 = g * 16 + j
                    mm(pb[0:64, j * 32:(j + 1) * 32], z2T[:, n2:8192:128], w3b[:, c0:c0 + 32], True, True, (tz, tF), (tb,))
                    mm(pb[64:128, j * 32:(j + 1) * 32], z2T[:, 8192 + n2:NFFT:128], w3b[:, 1024 + c0:1024 + c0 + 32], True, True, (tz, tF), (tb,))
                tt("dve", kT[:, g * 16:(g + 1) * 16, :].rearrange("p a b -> p (a b)"), pb, dec[:, g * 512:(g + 1) * 512], ALU.mult, (tb, tdec_), (tk,))
            pbz, tbz = bank()
            mm(pbz[0:1, 0:32], z2T[:, 0:1], w3z[:, c0:c0 + 32], True, True, (tz, tF), (tbz,))
            cp("dve", kT[0:1, 0, :], pbz[0:1, 0:32], (tbz,), (tk,))
            fft_fwd(kT, 128, A, tA, F1, tF, (tk,), ev)
            for g in range(8):
                pb, tb = fft_s2(A, tA, L1, L2, tF, g)
                cp("act", Kpk[:, g * 512:(g + 1) * 512], pb, (tb,), (tK,))
            for g in range(8):
                pa, ta = bank(); pc, tc_ = bank()
                mm(pa, PA, Kpk[:, g * 512:(g + 1) * 512], True, True, (tF, tK), (ta,))
                mm(pc, PC, Kpk[:, g * 512:(g + 1) * 512], True, True, (tF, tK), (tc_,))
                cp("act", KA[:, g * 512:(g + 1) * 512], pa, (ta,), (tKA[0],))
                cp("dve", KC[:, g * 512:(g + 1) * 512], pc, (tc_,), (tKA[1],))
            P.dma("sp", KAC_d[sc, :, 0, :], KA, (tKA[0],), (tKAC,))
            P.dma("sp", KAC_d[sc, :, 1, :], KC, (tKA[1],), (tKAC,))

    phaseD1()
    reset_arena(PERSIST)
    if stop_after == "D1":
        return finish(nc, P, out_d, dbg)

    def phaseD2():
        NN1 = TOWN // 128
        tF, L1, L2, F1 = load_fft_tables()
        R1 = alloc([128, 256], BF16); R2 = alloc([128, 256], BF16)
        P.dma("sp", R1, C["R1"], (), (tF,)); P.dma("sp", R2, C["R2"], (), (tF,))
        HR = alloc([128, 128, NN1], BF16); HI = alloc([128, 128, NN1], BF16)
        P.dma("sp", HR.rearrange("p a b -> p (a b)"), C["HR"], (), (tF,))
        P.dma("sp", HI.rearrange("p a b -> p (a b)"), C["HI"], (), (tF,))
        U = alloc([128, L], BF16); tU = T()
        X0 = alloc([128, TOWN], BF16); tX0 = T()
        Ys = alloc([128, 128, NN1], BF16); tYs = T()
        mark = apos[0]
        ev = [0]
        NPB = 512 // NN1
        chunks = [(96 * i, 96) for i in range(10)] + [(960, 64)]
        for ci_, (row0, nr) in enumerate(chunks):
            apos[0] = mark
            PH = alloc([128, L + 2], BF16); tPH = T()
            TM = alloc([128, L], BF16); tTM = T()
            memset("dve", PH[0:nr, 0:1], 0.0, (tPH,))
            memset("dve", PH[0:nr, L + 1:L + 2], 0.0, (tPH,))
            dgb = alloc([128, 9, 96], BF16); tdg_ = T()
            for gi in range(3):
                for j in range(3):
                    ts("dve", dgb[0:nr, gi * 3 + j, 0:nr], identb[0:nr, 0:nr], hycw[0:nr, ci_, gi, j:j + 1], None, ALU.mult, None, (tC,), (tdg_,))
            cvi = 0
            for gi, (dst, tdst_, Lg) in enumerate(((X0, tX0, TOWN), (TM, tTM, L), (U, tU, L))):
                r_ = (2 + gi) * D + row0
                Lld = min(L, Lg + 1)
                P.dma("sp", PH[0:nr, 1:Lld + 1], PT_d[r_:r_ + nr, 0:Lld], (tPT,), (tPH,))
                for t5 in range(Lg // 512):
                    pb, tb = bank()
                    for j in range(3):
                        mm(pb[0:nr, :], dgb[0:nr, gi * 3 + j, 0:nr], PH[0:nr, t5 * 512 + j:t5 * 512 + j + 512], j == 0, j == 2, (tdg_, tPH), (tb,))
                    sl5 = slice(t5 * 512, (t5 + 1) * 512)
                    if gi < 2:
                        if cvi % 3 == 2:
                            ts("dve", dst[0:nr, sl5], pb[0:nr, :], hycb[0:nr, ci_, gi:gi + 1], None, ALU.add, None, (tb, tC), (tdst_,))
                        else:
                            act(dst[0:nr, sl5], pb[0:nr, :], AF.Identity, (tb, tC), (tdst_,), bias=hycb[0:nr, ci_, gi:gi + 1])
                        cvi += 1
                    else:
                        stt(U[0:nr, sl5], pb[0:nr, :], hycb[0:nr, ci_, gi:gi + 1], TM[0:nr, sl5], ALU.add, ALU.mult, (tb, tC, tTM), (tU,))
            P.barrier()
            apos[0] = mark
            UT = alloc([64, 128, 32], BF16); tUT = (T(), T())
            A = alloc([128, 32, 2, 128], BF16); tA = (T(), T())
            P1 = alloc([128, 128, 32], BF16); P2 = alloc([128, 128, 32], BF16); tP = T()
            KA = alloc([128, 128, 32], BF16); KC = alloc([128, 128, 32], BF16); tKA = T()
            Bv = alloc([128, 32, 2, 128], BF16); tB = (T(), T())

            def st_ka(s):
                sc = (row0 + 32 * s) // 32
                P.dma("sp", KA.rearrange("p a b -> p (a b)"), KAC_d[sc, :, 0, :], (tKAC,), (tKA,))
                P.dma("sp", KC.rearrange("p a b -> p (a b)"), KAC_d[sc, :, 1, :], (tKAC,), (tKA,))

            def st_trs1(s):
                for g in range(4):
                    pb, tb = bank()
                    pbb = pb.bitcast(BF16)
                    for j in range(32):
                        n2 = g * 32 + j
                        tr(pbb[0:64, j * 32:(j + 1) * 32], U[32 * s:32 * s + 32, n2:L:128], identb[32 * s:32 * s + 32, 32 * s:32 * s + 32], (tU, tC), (tb,))
                    cp("act" if g % 2 == 0 else "dve", UT[:, g * 32:(g + 1) * 32, :], pbb[0:64, :].rearrange("p (a b) -> p a b", a=32), (tb,), (tUT[g % 2],))
                fft_fwd(UT, 64, A, tA, F1, tF, tUT, ev)

            def st_s2(s):
                for g in range(8):
                    pb, tb = fft_s2(A, tA, L1, L2, tF, g)
                    pv = pb.rearrange("p (a b) -> p a b", a=16)
                    tt("dve", P1[:, g * 16:(g + 1) * 16, :], pv, KA[:, g * 16:(g + 1) * 16, :], ALU.mult, (tb, tKA), (tP,))
                    tt("dve", P2[:, g * 16:(g + 1) * 16, :], pv, KC[:, g * 16:(g + 1) * 16, :], ALU.mult, (tb, tKA), (tP,))

            def st_s1p(s):
                for c in range(0, 32, 2):
                    pb, tb = bank()
                    for h_ in range(2):
                        mm(pb[:, h_ * 256:(h_ + 1) * 256], P1[:, :, c + h_], R1, True, False, (tP, tF), (tb,))
                        mm(pb[:, h_ * 256:(h_ + 1) * 256], P2[:, :, c + h_], R2, False, True, (tP, tF), (tb,))
                    cp("act" if (ev[0] % 2 == 0) else "dve", Bv[:, c:c + 2, :, :].rearrange("p c a k -> p (c a k)"), pb, (tb,), (tB[ev[0] % 2],))
                    ev[0] += 1

            def st_s2p(s):
                rows = slice(32 * s, 32 * s + 32)
                for g in range(128 // NPB):
                    pb, tb = bank()
                    for j in range(NPB):
                        n2 = g * NPB + j
                        mm(pb[rows, j * NN1:(j + 1) * NN1], Bv[:, :, 0, n2], HR[:, n2, :], True, False, tB + (tF,), (tb,))
                        mm(pb[rows, j * NN1:(j + 1) * NN1], Bv[:, :, 1, n2], HI[:, n2, :], False, True, tB + (tF,), (tb,))
                    cp("act", Ys[rows, g * NPB:(g + 1) * NPB, :].rearrange("p a b -> p (a b)"), pb[rows, :], (tb,), (tYs,))

            ns_ = nr // 32
            st_ka(0); st_trs1(0); st_s2(0)
            for s in range(ns_):
                if s + 1 < ns_:
                    st_ka(s + 1)
                    st_trs1(s + 1)
                st_s1p(s)
                st_s2p(s)
                if s + 1 < ns_:
                    st_s2(s + 1)
            uo = U[0:nr, 0:TOWN]
            stt(uo.rearrange("p (n1 n2) -> p n1 n2", n2=128), uo.rearrange("p (n1 n2) -> p n1 n2", n2=128), hysk[0:nr, ci_:ci_ + 1],
                Ys[0:nr, :, :].rearrange("p n2 n1 -> p n1 n2"), ALU.mult, ALU.add, (tYs, tC, tU), (tU,))
            tt("dve", X0[0:nr, :], X0[0:nr, :], uo, ALU.mult, (tU, tX0), (tX0,))
            P.dma("sp", YHY_d[row0:row0 + nr, :], X0[0:nr, :], (tX0,), (tYHY,))
            P.barrier()

    phaseD2()
    reset_arena(PERSIST)
    if stop_after == "D2":
        return finish(nc, P, out_d, dbg)

    G1B = alloc([128, D], F32); G2B = alloc([128, D], F32); FGB = alloc([128, D], F32)
    P.dma("sp", FGB, fg_d.partition_broadcast(128), (), (tC,))
    dg = alloc([128, 128], F32); tdg = T()
    for (dst, gi) in ((G1B, 2), (G2B, 5)):
        for dc in range(NCH):
            ts("dve", dg, ident, modT[:, gi * 8 + dc, 0:1], None, ALU.mult, None, (tC,), (tdg,))
            pb2, tb2 = bank()
            mm(pb2[:, 0:128], ones, dg, True, True, (tC, tdg), (tb2,))
            cp("act", dst[:, dc * 128:(dc + 1) * 128], pb2[:, 0:128], (tb2,), (tC,))
    PERSIST2 = apos[0]

    def load_w(dram, rows_k, cols):
        w = alloc([128, rows_k, cols], BF16); tw = T()
        for k in range(rows_k):
            for c0 in range(0, cols, 1024):
                wd = min(1024, cols - c0)
                P.dma("pool", w[:, k, c0:c0 + wd], dram[k * 128:(k + 1) * 128, c0:c0 + wd], (), (tw,))
        return w, tw

    def phaseE():
        wrg, twrg = load_w(rgp_d, NCH, D)
        why, twhy = load_w(hyp_d, NCH, D)
        wo, two = load_w(wout_d, NCH, D)
        W = 512
        ins = [[alloc([128, NCH, W], BF16) for _ in range(4)] for _ in range(2)]
        tin = [T(), T()]
        mg = alloc([128, NCH, W], BF16); tmg = T()
        m1 = alloc([128, W], F32); m2 = alloc([128, W], F32); tm = T()
        xts = [alloc([128, D], F32) for _ in range(2)]; txs = [T(), T()]
        t1 = alloc([128, D], F32); tt1 = T()
        xi = 0
        for ti in range(TOWN // W):
            bufs, tb_in = ins[ti % 2], tin[ti % 2]
            tsl = slice(ti * W, (ti + 1) * W)
            for bi, (src, tsrc) in enumerate(((YRG_d, tYRG), (YHY_d, tYHY))):
                P.dma("sp", bufs[bi], src[:, tsl].rearrange("(k p) t -> p k t", p=128), (tsrc,), (tb_in,))
            for bi, g in ((2, 5), (3, 6)):
                P.dma("sp", bufs[bi], PT_d[g * D:(g + 1) * D, tsl].rearrange("(k p) t -> p k t", p=128), (tPT,), (tb_in,))
            for dc in range(NCH):
                pa, ta = bank(); ph, th_ = bank()
                for k in range(NCH):
                    mm(pa, wrg[:, k, dc * 128:(dc + 1) * 128], bufs[0][:, k, :], k == 0, k == NCH - 1, (twrg, tb_in), (ta,))
                for k in range(NCH):
                    mm(ph, why[:, k, dc * 128:(dc + 1) * 128], bufs[1][:, k, :], k == 0, k == NCH - 1, (twhy, tb_in), (th_,))
                tt("dve", m1, pa, bufs[2][:, dc, :], ALU.mult, (ta, tb_in), (tm,))
                tt("dve", m2, ph, bufs[3][:, dc, :], ALU.mult, (th_, tb_in), (tm,))
                tt("pool", mg[:, dc, :], m1, m2, ALU.add, (tm,), (tmg,))
            for a in range(W // 128):
                xt, tx = xts[xi % 2], txs[xi % 2]
                xi += 1
                r0 = ti * W + a * 128
                P.dma("sp", xt, x_d[r0:r0 + 128, :], (), (tx,))
                for half in range(2):
                    pb, tb = bank()
                    for k in range(NCH):
                        mm(pb, mg[:, k, a * 128:(a + 1) * 128], wo[:, k, half * 512:(half + 1) * 512], k == 0, k == NCH - 1, (tmg, two), (tb,))
                    tt("dve", t1[:, half * 512:(half + 1) * 512], pb, G1B[:, half * 512:(half + 1) * 512], ALU.mult, (tb, tC), (tt1,))
                tt("pool", xt, xt, t1, ALU.add, (tt1, tx), (tx,))
                P.dma("sp", X1_d[r0:r0 + 128, :], xt, (tx,), (tX1,))

    phaseE()
    reset_arena(PERSIST2)
    if stop_after == "E":
        return finish(nc, P, out_d, dbg)

    def phaseF():
        NT = TOWN // 128
        BS, NB = MOE_BS, MOE_NB
        PTOT = NB * BS
        XS_d = dscr("XS", [PTOT, D], BF16); YS_d = dscr("YS", [PTOT, D], BF16)
        tXS, tYS = T(), T()
        W1f = mw1_d.rearrange("e k f -> (e k) f"); W3f = mw3_d.rearrange("e k f -> (e k) f"); W2f = mw2_d.rearrange("e f d -> (e f) d")
        wge, twge = load_w(wge_d, NCH, 36)
        tK = T()
        tri = alloc([128, 128], F32); pkw = alloc([128, 8], F32); pkw2 = alloc([128, 4], F32); jbs = alloc([128, NB], F32)
        for dst, nm in ((tri, "tri"), (pkw, "pkw"), (pkw2, "pkw2"), (jbs, "jbs")):
            P.dma("sp", dst, C[nm], (), (tK,))
        A2B = alloc([128, D], F32); B2B = alloc([128, D], F32)
        dg2 = alloc([128, 128], F32); tdg2 = T()
        for (dst, col) in ((A2B, A2), (B2B, B2)):
            for dc in range(NCH):
                ts("dve", dg2, ident, col[:, dc:dc + 1], None, ALU.mult, None, (tC,), (tdg2,))
                pb2, tb2 = bank()
                mm(pb2[:, 0:128], ones, dg2, True, True, (tC, tdg2), (tb2,))
                cp("act", dst[:, dc * 128:(dc + 1) * 128], pb2[:, 0:128], (tb2,), (tK,))
        OH = alloc([128, NT, 64], F32); tOH = T()
        RK = alloc([128, NT, 2], F32); WTS = alloc([128, NT, 2], F32); tRK = T()
        DSTf = alloc([128, NT, 2], F32); DSTi = alloc([128, NT, 2], I32); tDST = T()
        S = alloc([128, 64], F32); tS = T()
        CNT = alloc([128, 64], F32); BASE = alloc([128, 64], F32); PEND = alloc([128, 32], F32); PADD = alloc([128, 32], F32)
        Z32 = alloc([128, 32], F32); tmp64 = alloc([128, 64], F32); ttmp = T()
        EB = alloc([128, NB], F32); EBs = alloc([128, NB], F32)
        IDX1f = alloc([128, NB, 8], F32); IDX1 = alloc([128, NB, 8], I32); IDX2f = alloc([128, NB, 4], F32); IDX2 = alloc([128, NB, 4], I32)
        tIDX = T()
        xts = [alloc([128, D], F32) for _ in range(2)]; txs = [T(), T()]
        junk = alloc([128, D], BF16); tj = T()
        sm = alloc([128, 96], F32); tsm = T()
        ss = alloc([128, 2], F32)
        hxT = alloc([128, NCH, 128], BF16); thx = T()
        tm32 = alloc([128, D], F32); ttm = T()
        mark = apos[0]
        HXTM = alloc([128, NT, D], BF16); tHX = T()
        memset("dve", S, 0.0, (tS,))
        memset("dve", Z32, 0.0, (ttmp,))
        for a in range(NT):
            xt, tx = xts[a % 2], txs[a % 2]
            r0 = a * 128
            P.dma("sp", xt, X1_d[r0:r0 + 128, :], (tX1,), (tx,))
            act(junk, xt, AF.Square, (tx,), (tj, tx), accum=ss[:, 0:1])
            act(ss[:, 0:1], ss[:, 0:1], AF.Sqrt, (tx,), (tx,), scale=1.0 / D, bias=EPS)
            P.op("dve", lambda h: h.reciprocal(out=ss[:, 0:1], in_=ss[:, 0:1]), (tx,), (tx,))
            ts("dve", xt, xt, ss[:, 0:1], None, ALU.mult, None, (tx,), (tx,))
            tt("pool", tm32, xt, A2B, ALU.mult, (tx, tK), (ttm,))
            tt("pool", HXTM[:, a, :], tm32, B2B, ALU.add, (ttm, tK), (tHX,))
            for dc in range(0, NCH, 4):
                pb, tb = bank()
                for j in range(4):
                    tr(pb[:, j * 128:(j + 1) * 128], xt[:, (dc + j) * 128:(dc + j + 1) * 128], ident, (tx, tC), (tb,))
                for j in range(4):
                    act(hxT[:, dc + j, :], pb[:, j * 128:(j + 1) * 128], AF.Identity, (tb, tC), (thx,),
                        bias=B2[:, dc + j:dc + j + 1], scale=A2[:, dc + j:dc + j + 1])
            pb, tb = bank()
            for k in range(NCH):
                mm(pb[:, 0:36], hxT[:, k, :], wge[:, k, :], k == 0, k == NCH - 1, (thx, twge), (tb,))
            lg = sm[:, 0:36]
            tt("dve", lg, pb[:, 0:36], bgeB, ALU.add, (tb, tC), (tsm,))
            gmax = sm[:, 36:37]; sg = sm[:, 37:38]; oh = sm[:, 38:42]; ein = sm[:, 42:50]; m8 = sm[:, 50:58]
            e4 = sm[:, 58:62]; ngm = sm[:, 62:63]; dd = sm[:, 63:64]; mk1 = sm[:, 64:72]; mk2 = sm[:, 72:80]
            P.op("dve", lambda h: h.tensor_reduce(out=gmax, in_=lg[:, 0:4], axis=AX.X, op=ALU.max), (tsm,), (tsm,))
            ts("dve", ngm, gmax, -1.0, None, ALU.mult, None, (tsm,), (tsm,))
            act(e4, lg[:, 0:4], AF.Exp, (tsm,), (tsm,), bias=ngm, accum=sg)
            P.op("dve", lambda h: h.reciprocal(out=sg, in_=sg), (tsm,), (tsm,))
            ts("dve", oh, lg[:, 0:4], gmax, None, ALU.is_equal, None, (tsm,), (tsm,))
            ts("dve", ein, lg[:, 4:12], oh[:, 0:1], None, ALU.mult, None, (tsm,), (tsm,))
            for g in range(1, 4):
                stt(ein, lg[:, 4 + 8 * g:12 + 8 * g], oh[:, g:g + 1], ein, ALU.mult, ALU.add, (tsm,), (tsm,))
            P.op("dve", lambda h: h.max(out=m8, in_=ein), (tsm,), (tsm,))
            tt("dve", dd, m8[:, 1:2], m8[:, 0:1], ALU.subtract, (tsm,), (tsm,))
            act(dd, dd, AF.Exp, (tsm,), (tsm,))
            ts("dve", e4[:, 0:1], dd, 1.0, None, ALU.add, None, (tsm,), (tsm,))
            P.op("dve", lambda h: h.reciprocal(out=e4[:, 0:1], in_=e4[:, 0:1]), (tsm,), (tsm,))
            tt("dve", e4[:, 1:2], dd, e4[:, 0:1], ALU.mult, (tsm,), (tsm,))
            tt("dve", WTS[:, a, 0:1], e4[:, 0:1], sg, ALU.mult, (tsm,), (tRK,))
            tt("dve", WTS[:, a, 1:2], e4[:, 1:2], sg, ALU.mult, (tsm,), (tRK,))
            ts("dve", mk1, ein, m8[:, 0:1], None, ALU.is_equal, None, (tsm,), (tsm,))
            ts("dve", mk2, ein, m8[:, 1:2], None, ALU.is_equal, None, (tsm,), (tsm,))
            for g in range(4):
                ts("dve", OH[:, a, g * 8:(g + 1) * 8], mk1, oh[:, g:g + 1], None, ALU.mult, None, (tsm,), (tOH,))
                ts("dve", OH[:, a, 32 + g * 8:32 + (g + 1) * 8], mk2, oh[:, g:g + 1], None, ALU.mult, None, (tsm,), (tOH,))
            pR, tR = bank()
            mm(pR[:, 0:64], tri, OH[:, a, :], True, False, (tK, tOH), (tR,))
            mm(pR[:, 0:64], ones, S, False, True, (tC, tS), (tR,))
            tt("dve", tmp64, pR[:, 0:64], OH[:, a, :], ALU.mult, (tR, tOH), (ttmp,))
            P.op("dve", lambda h, a=a: h.tensor_reduce(out=RK[:, a, :], in_=tmp64.rearrange("p (a b) -> p a b", a=2), axis=AX.X, op=ALU.add), (ttmp,), (tRK,))
            tt("pool", S, S, OH[:, a, :], ALU.add, (tOH, tS), (tS,))
        pT, tT_ = bank()
        mm(pT[:, 0:64], ones, S, True, True, (tC, tS), (tT_,))
        cp("dve", CNT, pT[:, 0:64], (tT_,), (ttmp,))
        tt("dve", PADD, CNT[:, 0:32], CNT[:, 32:64], ALU.add, (ttmp,), (ttmp,))
        ts("dve", PADD, PADD, 1.0 / BS, (BS - 1.0) / BS - 0.5 + 0.5 / BS, ALU.mult, ALU.add, (ttmp,), (ttmp,))
        cp("dve", DSTi[:, 0:16, :].rearrange("p a b -> p (a b)"), PADD, (ttmp,), (tDST,))
        cp("dve", PADD, DSTi[:, 0:16, :].rearrange("p a b -> p (a b)"), (tDST,), (ttmp,))
        ts("dve", PADD, PADD, float(BS), None, ALU.mult, None, (ttmp,), (ttmp,))
        P.op("dve", lambda h: h.tensor_tensor_scan(out=PEND, data0=PADD, data1=Z32, initial=0.0, op0=ALU.add, op1=ALU.add), (ttmp,), (ttmp,))
        tt("dve", BASE[:, 0:32], PEND, PADD, ALU.subtract, (ttmp,), (ttmp,))
        tt("dve", BASE[:, 32:64], BASE[:, 0:32], CNT[:, 0:32], ALU.add, (ttmp,), (ttmp,))
        for a in range(NT):
            tt("dve", tmp64, OH[:, a, :], BASE, ALU.mult, (tOH, ttmp), (ttmp,))
            P.op("dve", lambda h, a=a: h.tensor_reduce(out=DSTf[:, a, :], in_=tmp64.rearrange("p (a b) -> p a b", a=2), axis=AX.X, op=ALU.add), (ttmp,), (tDST,))
        tt("dve", DSTf.rearrange("p a b -> p (a b)"), DSTf.rearrange("p a b -> p (a b)"), RK.rearrange("p a b -> p (a b)"), ALU.add, (tDST, tRK), (tDST,))
        cp("dve", DSTi.rearrange("p a b -> p (a b)"), DSTf.rearrange("p a b -> p (a b)"), (tDST,), (tDST,))
        memset("dve", EB, 0.0, (tIDX,))
        for e in range(NE):
            stt(EB, jbs, PEND[:, e:e + 1], EB, ALU.is_ge, ALU.add, (tK, ttmp, tIDX), (tIDX,))
        ts("dve", EB, EB, float(NE - 1), None, ALU.min, None, (tIDX,), (tIDX,))
        ts("dve", EBs, EB, 1024.0, None, ALU.mult, None, (tIDX,), (tIDX,))
        for j in range(NB):
            ts("dve", IDX1f[:, j, :], pkw, EBs[:, j:j + 1], None, ALU.add, None, (tK, tIDX), (tIDX,))
        ts("dve", EBs, EB, 512.0, None, ALU.mult, None, (tIDX,), (tIDX,))
        for j in range(NB):
            ts("dve", IDX2f[:, j, :], pkw2, EBs[:, j:j + 1], None, ALU.add, None, (tK, tIDX), (tIDX,))
        cp("dve", IDX1.rearrange("p a b -> p (a b)"), IDX1f.rearrange("p a b -> p (a b)"), (tIDX,), (tIDX,))
        cp("dve", IDX2.rearrange("p a b -> p (a b)"), IDX2f.rearrange("p a b -> p (a b)"), (tIDX,), (tIDX,))
        for a in range(NT):
            for k in range(2):
                P.idma("pool", XS_d, HXTM[:, a, :], DSTi[:, a, k:k + 1], None, (tHX, tDST), (tXS,))
        P.barrier()
        apos[0] = mark
        w1s = [alloc([128, NCH, DE], BF16) for _ in range(2)]
        w3s = [alloc([128, NCH, DE], BF16) for _ in range(2)]
        w2s = [alloc([128, 4, D], BF16) for _ in range(2)]
        tws = [(T(), T()), (T(), T())]
        wstg = [alloc([128, DE], F32) for _ in range(12)]; twstg = [T() for _ in range(12)]
        wstg2 = [alloc([128, D], F32) for _ in range(4)]; twstg2 = [T() for _ in range(4)]
        wi2 = [0]
        wi = [0]
        mark3 = apos[0]
        NR = 3
        xss = [alloc([128, D], BF16) for _ in range(NR)]; txss = [T() for _ in range(NR)]
        xTs = [alloc([128, NCH, 128], BF16) for _ in range(NR)]; txT = [T() for _ in range(NR)]
        hids = [alloc([128, DE], BF16) for _ in range(NR)]; thid = [T() for _ in range(NR)]
        hTs = [alloc([128, 4, 128], BF16) for _ in range(NR)]; thT = [T() for _ in range(NR)]
        ybs = [alloc([128, D], BF16) for _ in range(NR)]; tyb = [(T(), T()) for _ in range(NR)]
        s1s = [alloc([128, DE], F32) for _ in range(2)]; ts1s = [T(), T()]

        def wpieces(jb, grp):
            w1, w3, w2, tw = w1s[jb % 2], w3s[jb % 2], w2s[jb % 2], tws[jb % 2]
            pcs = []
            for k in range(NCH):
                pcs.append((w1[:, k, :], W1f, IDX1[:, jb, k:k + 1], False))
                pcs.append((w3[:, k, :], W3f, IDX1[:, jb, k:k + 1], False))
            for k in range(4):
                pcs.append((w2[:, k, :], W2f, IDX2[:, jb, k:k + 1], True))
            for (dst, src, idx, big) in pcs[grp * 5:(grp + 1) * 5]:
                if big:
                    sg_, tsg_ = wstg2[wi2[0] % len(wstg2)], twstg2[wi2[0] % len(wstg2)]
                    wi2[0] += 1
                else:
                    sg_, tsg_ = wstg[wi[0] % len(wstg)], twstg[wi[0] % len(wstg)]
                wi[0] += 1
                P.idma("pool", sg_, src, None, idx, (tIDX,), (tsg_,))
                cp("act" if wi[0] % 2 == 0 else "dve", dst, sg_, (tsg_,), (tw[wi[0] % 2],))

        def stageA(s):
            jb, sb = s // 4, s % 4
            w1, w3, tw = w1s[jb % 2], w3s[jb % 2], tws[jb % 2]
            xs, txs_ = xss[s % NR], txss[s % NR]
            xT, txT_ = xTs[s % NR], txT[s % NR]
            hid, thid_ = hids[s % NR], thid[s % NR]
            s1, ts1 = s1s[s % 2], ts1s[s % 2]
            r0 = jb * BS + sb * 128
            P.dma("sp", xs, XS_d[r0:r0 + 128, :], (tXS,), (txs_,))
            pb, tb = bank()
            pbb = pb.bitcast(BF16)
            for k in range(NCH):
                tr(pbb[:, k * 128:(k + 1) * 128], xs[:, k * 128:(k + 1) * 128], identb, (txs_, tC), (tb,))
            cp("dve", xT.rearrange("p a b -> p (a b)"), pbb, (tb,), (txT_,))
            p1, tp1 = bank(); p3, tp3 = bank()
            for k in range(NCH):
                mm(p1, xT[:, k, :], w1[:, k, :], k == 0, k == NCH - 1, (txT_,) + tw, (tp1,))
            for k in range(NCH):
                mm(p3, xT[:, k, :], w3[:, k, :], k == 0, k == NCH - 1, (txT_,) + tw, (tp3,))
            act(s1, p1, AF.Silu, (tp1,), (ts1,))
            tt("dve", hid, s1, p3, ALU.mult, (ts1, tp3), (thid_,))

        def stageB(s):
            jb, sb = s // 4, s % 4
            w2, tw = w2s[jb % 2], tws[jb % 2]
            hid, thid_ = hids[s % NR], thid[s % NR]
            hT, thT_ = hTs[s % NR], thT[s % NR]
            yb, tyb_ = ybs[s % NR], tyb[s % NR]
            r0 = jb * BS + sb * 128
            pb2, tb2 = bank()
            pbb2 = pb2.bitcast(BF16)
            for f in range(4):
                tr(pbb2[:, f * 128:(f + 1) * 128], hid[:, f * 128:(f + 1) * 128], identb, (thid_, tC), (tb2,))
            cp("act", hT.rearrange("p a b -> p (a b)"), pbb2[:, 0:512], (tb2,), (thT_,))
            for half in range(2):
                py, tpy = bank()
                for f in range(4):
                    mm(py, hT[:, f, :], w2[:, f, half * 512:(half + 1) * 512], f == 0, f == 3, (thT_,) + tw, (tpy,))
                cp("act" if half == 0 else "dve", yb[:, half * 512:(half + 1) * 512], py, (tpy,), (tyb_[half],))
            P.dma("sp", YS_d[r0:r0 + 128, :], yb, tyb_, (tYS,))

        NS = NB * (BS // 128)
        for g in range(4):
            wpieces(0, g)
        for t in range(NS + 1):
            if t < NS:
                stageA(t)
            if t >= 1:
                stageB(t - 1)
            if t < NS:
                jn = t // 4 + 1
                if jn < NB:
                    wpieces(jn, t % 4)
        P.barrier()
        apos[0] = mark3
        y1s = [alloc([128, D], BF16) for _ in range(2)]; y2s = [alloc([128, D], BF16) for _ in range(2)]; tys = [T(), T()]
        mo = alloc([128, D], F32); tmo = T()
        for a in range(NT):
            y1, y2, ty = y1s[a % 2], y2s[a % 2], tys[a % 2]
            xt, tx = xts[a % 2], txs[a % 2]
            r0 = a * 128
            P.idma("pool", y1, YS_d, None, DSTi[:, a, 0:1], (tYS, tDST), (ty,))
            P.idma("pool", y2, YS_d, None, DSTi[:, a, 1:2], (tYS, tDST), (ty,))
            P.dma("sp", xt, X1_d[r0:r0 + 128, :], (tX1,), (tx,))
            ts("dve", mo, y1, WTS[:, a, 0:1], None, ALU.mult, None, (ty, tRK), (tmo,))
            stt(mo, y2, WTS[:, a, 1:2], mo, ALU.mult, ALU.add, (ty, tRK), (tmo,))
            tt("pool", mo, mo, G2B, ALU.mult, (tC,), (tmo,))
            tt("pool", xt, xt, mo, ALU.add, (tmo,), (tx,))
            act(junk, xt, AF.Square, (tx,), (tj, tx), accum=ss[:, 1:2])
            act(ss[:, 1:2], ss[:, 1:2], AF.Sqrt, (tx,), (tx,), scale=1.0 / D, bias=EPS)
            P.op("dve", lambda h: h.reciprocal(out=ss[:, 1:2], in_=ss[:, 1:2]), (tx,), (tx,))
            stt(xt, xt, ss[:, 1:2], FGB, ALU.mult, ALU.mult, (tx, tC), (tx,))
            P.dma("sp", out_d[r0:r0 + 128, :], xt, (tx,), (tX1,))

    phaseF()
    return finish(nc, P, out_d, dbg)


def finish(nc, P, out_d, dbg):
    P.barrier()
    P.emit()
    return nc


def make_inputs(inp, core):
    b, rev = core // 2, (core % 2 == 1)
    f = lambda a: np.ascontiguousarray(np.asarray(a, np.float32))
    m = {}
    xs = np.asarray(inp["x"][b], np.float32)
    cs = np.asarray(inp["ctx"][b], np.float32)
    m["x"] = f(xs[::-1] if rev else xs)
    m["ctx"] = f(cs[::-1] if rev else cs)
    cT = np.stack([fm(inp["c"][b]), fm(inp["c_ctx"])], axis=-1)
    m["cT"] = f(cT)
    m["ada_w"] = f(inp["ada_w"][0])
    m["ada_bT"] = fm(inp["ada_b"][0])
    m["g1nT"] = fm(inp["norm1_g"][0])
    m["g2nT"] = fm(inp["norm2_g"][0])
    m["final_g"] = f(inp["final_g"]).reshape(1, D)
    m["w_in"] = f(inp["w_in"][0])
    rw = np.asarray(inp["rg_conv_w"][0], np.float32)
    zero = np.zeros((D,), np.float32)
    taps5 = [zero, rw[3], rw[2], rw[1], rw[0]] if rev else [rw[0], rw[1], rw[2], rw[3], zero]
    m["rg_cwT"] = f(np.stack([fm(tp) for tp in taps5], axis=-1))
    m["rg_cbT"] = fm(inp["rg_conv_b"][0])
    bd = np.zeros((128, 4, NCH, 128), np.float32)
    gnames = ("rg_wa_b", "rg_wx_b", "rg_wa_f", "rg_wx_f") if rev else ("rg_wa_f", "rg_wx_f", "rg_wa_b", "rg_wx_b")
    bnames = ("rg_ba_b", "rg_bx_b", "rg_ba_f", "rg_bx_f") if rev else ("rg_ba_f", "rg_bx_f", "rg_ba_b", "rg_bx_b")
    lnames = ("rg_lam_b", "rg_lam_f") if rev else ("rg_lam_f", "rg_lam_b")
    for gi, nm in enumerate(gnames):
        w = np.asarray(inp[nm][0], np.float32)
        for cc in range(NCH):
            bd[0:64, gi, cc, 0:64] = w[2 * cc]
            bd[64:128, gi, cc, 64:128] = w[2 * cc + 1]
    m["rg_bd"] = bd
    m["rg_biasT"] = f(np.stack([fm(inp[nm][0]) for nm in bnames], axis=1))
    m["rg_lamT"] = f(np.stack([fm(inp[nm][0]) for nm in lnames], axis=1))
    m["rg_proj"] = f(inp["rg_proj"][0])
    def c96(v):
        o = np.zeros((11 * 96,), np.float32); o[:1024] = np.asarray(v, np.float32)
        return np.ascontiguousarray(o.reshape(11, 96).T)
    hw = np.asarray(inp["hy_conv_w"][0], np.float32); hb = np.asarray(inp["hy_conv_b"][0], np.float32)
    jt = (2, 1, 0) if rev else (0, 1, 2)
    m["hy_cw96"] = f(np.stack([np.stack([c96(hw[j, g * 1024:(g + 1) * 1024]) for j in jt], axis=-1) for g in range(3)], axis=2))
    m["hy_cb96"] = f(np.stack([c96(hb[g * 1024:(g + 1) * 1024]) for g in range(3)], axis=-1))
    m["hy_w1"] = f(inp["hy_pos_w1"][0])
    m["hy_b1T"] = f(inp["hy_pos_b1"][0]).reshape(64, 1)
    m["hy_w2"] = f(inp["hy_pos_w2"][0])
    m["hy_b2T"] = f(inp["hy_pos_b2"][0]).reshape(64, 1)
    m["hy_frT"] = f(inp["hy_freq"][0]).reshape(64, 1)
    w3 = np.asarray(inp["hy_pos_w3"][0], np.float32)
    m["hy_w3"] = f(np.concatenate([w3[:, 1024:], w3[:, :1024]], axis=1) if rev else w3)
    m["hy_w3z"] = f(w3[:, :1024])
    m["hy_sk96"] = c96(inp["hy_skip"][0])
    m["hy_proj"] = f(inp["hy_proj"][0])
    m["w_out"] = f(inp["w_out"][0])
    m["moe_wge"] = f(np.concatenate([inp["moe_wg"][0], inp["moe_we"][0]], axis=1))
    m["moe_bge"] = f(np.concatenate([inp["moe_bg"][0], inp["moe_be"][0]])).reshape(1, 36)
    m["moe_w1"] = f(inp["moe_w1"][0])
    m["moe_w3"] = f(inp["moe_w3"][0])
    m["moe_w2"] = f(inp["moe_w2"][0])
    for nm, arr in host_consts().items():
        m["k_" + nm] = arr
    return m


def kernel(**inputs):
    nc = build()
    in_maps = [make_inputs(inputs, c) for c in range(NCORES)]
    res = run_bass_kernel_spmd(nc, in_maps, core_ids=list(range(NCORES)))
    B = NCORES // 2
    out = np.empty((B, L, D), np.float32)
    for c in range(NCORES):
        r = np.asarray(res.results[c]["out"], np.float32)
        if c % 2 == 0:
            out[c // 2, 0:TOWN] = r
        else:
            out[c // 2, TOWN:L] = r[::-1]
    return out
```
